# Optimizing a Trainium2 kernel written in Bass

```python
import math
import jax, jax.numpy as jnp
from jax import lax
import numpy as np

D_MODEL = 1024
BATCH = 8
SEQ = 2048
DEPTH = 1

CHUNK = 64
CONV_A_GROUPS = 8
CONV_A_GROUP_DIM = D_MODEL // 16
CONV_A_DIM = CONV_A_GROUPS * CONV_A_GROUP_DIM
CONV_A_WIDTH = 3
DN_HEADS = 4
DN_HEAD_DIM = D_MODEL // 8
DN_DIM = DN_HEADS * DN_HEAD_DIM
DN_CONV_WIDTH = 4
MIX_DIM = CONV_A_DIM + DN_DIM
IN_PROJ_DIM = 3 * CONV_A_DIM + 4 * DN_DIM + 2 * DN_HEADS
N_GROUPS = 4
EXPERTS_PER_GROUP = 8
N_EXPERTS = N_GROUPS * EXPERTS_PER_GROUP
TOP_K = 2
EXPERT_FF = D_MODEL // 2
MOE_BLOCK = 128
EPS = 1e-6

kernel_name = "hybrid_conv_deltanet_hmoe_block"


def rms_norm(x, g):
    xf = x.astype(jnp.float32)
    y = xf * lax.rsqrt(jnp.mean(xf * xf, axis=-1, keepdims=True) + EPS)
    return (y * g.astype(jnp.float32)).astype(x.dtype)


def causal_dwconv(x, w):
    K = w.shape[0]
    S = x.shape[1]
    xp = jnp.pad(x, ((0, 0), (K - 1, 0), (0, 0)))
    y = xp[:, 0:S] * w[0]
    for j in range(1, K):
        y = y + xp[:, j:j + S] * w[j]
    return y


def short_conv_mixer(hx, b, c, conv_w, norm_g):
    Bsz, S, _ = hx.shape
    y = b * causal_dwconv(c * hx, conv_w)
    y = y.reshape(Bsz, S, CONV_A_GROUPS, CONV_A_GROUP_DIM)
    y = rms_norm(y, norm_g.reshape(CONV_A_GROUPS, CONV_A_GROUP_DIM))
    return y.reshape(Bsz, S, CONV_A_DIM)


def chunk_gated_delta(q, k, v, beta, g):
    Bsz, H, S, dk = q.shape
    dv = v.shape[-1]
    n = S // CHUNK

    def chunks(t):
        return t.reshape((Bsz, H, n, CHUNK) + t.shape[3:])

    q, k, v, beta, g = chunks(q), chunks(k), chunks(v), chunks(beta), chunks(g)
    g = jnp.cumsum(g, axis=-1)
    causal = jnp.tril(jnp.ones((CHUNK, CHUNK), bool))
    strict = jnp.tril(jnp.ones((CHUNK, CHUNK), bool), -1)
    decay = jnp.exp(jnp.where(causal, g[..., :, None] - g[..., None, :], -jnp.inf))
    k_beta = k * beta[..., None]
    L = jnp.where(strict, jnp.einsum('bhncd,bhnjd->bhncj', k_beta, k) * decay, 0.0)
    eye = jnp.eye(CHUNK, dtype=jnp.float32)
    rhs = jnp.concatenate([v * beta[..., None], k_beta * jnp.exp(g)[..., None]], axis=-1)
    uw = lax.linalg.triangular_solve(eye + L, rhs, left_side=True, lower=True, unit_diagonal=True)
    u, w = uw[..., :dv], uw[..., dv:]
    intra = jnp.einsum('bhncd,bhnjd->bhncj', q, k) * decay
    q_dec = q * jnp.exp(g)[..., None]
    g_last = g[..., -1]
    k_dec = k * jnp.exp(g_last[..., None] - g)[..., None]

    def to_scan(t):
        return jnp.moveaxis(t, 2, 0)

    xs = (to_scan(u), to_scan(w), to_scan(q_dec), to_scan(intra), to_scan(k_dec), to_scan(g_last))

    def step(state, inp):
        u_i, w_i, qd_i, a_i, kd_i, gl_i = inp
        v_new = u_i - jnp.einsum('bhck,bhkv->bhcv', w_i, state)
        o_i = jnp.einsum('bhck,bhkv->bhcv', qd_i, state) + jnp.einsum('bhcj,bhjv->bhcv', a_i, v_new)
        state = state * jnp.exp(gl_i)[..., None, None] + jnp.einsum('bhck,bhcv->bhkv', kd_i, v_new)
        return state, o_i

    s0 = jnp.zeros((Bsz, H, dk, dv), jnp.float32)
    _, o = lax.scan(step, s0, xs)
    return jnp.moveaxis(o, 0, 2).reshape(Bsz, H, S, dv)


def gated_deltanet(q, k, v, z, beta_logit, a_logit, conv_w, a_log, dt_bias, norm_g):
    Bsz, S, _ = q.shape
    dtype = q.dtype
    qkv = jax.nn.silu(causal_dwconv(jnp.concatenate([q, k, v], axis=-1), conv_w)).astype(jnp.float32)
    q, k, v = jnp.split(qkv, 3, axis=-1)

    def heads(t):
        return t.reshape(Bsz, S, DN_HEADS, DN_HEAD_DIM).transpose(0, 2, 1, 3)

    q, k, v = heads(q), heads(k), heads(v)
    q = q * lax.rsqrt(jnp.sum(q * q, axis=-1, keepdims=True) + EPS) * (DN_HEAD_DIM ** -0.5)
    k = k * lax.rsqrt(jnp.sum(k * k, axis=-1, keepdims=True) + EPS)
    beta = jax.nn.sigmoid(beta_logit.astype(jnp.float32)).transpose(0, 2, 1)
    g = (-jnp.exp(a_log.astype(jnp.float32))
         * jax.nn.softplus(a_logit.astype(jnp.float32) + dt_bias.astype(jnp.float32))).transpose(0, 2, 1)
    o = chunk_gated_delta(q, k, v, beta, g).transpose(0, 2, 1, 3)
    zf = z.astype(jnp.float32).reshape(Bsz, S, DN_HEADS, DN_HEAD_DIM)
    o = rms_norm(o, norm_g) * jax.nn.silu(zf)
    return o.reshape(Bsz, S, DN_DIM).astype(dtype)


def hier_moe(h, router_group_w, router_expert_w, w_gate, w_up, w_down):
    N, D = h.shape
    group_logits = (h @ router_group_w).astype(jnp.float32)
    group_idx = jnp.argmax(group_logits, axis=-1)
    group_prob = jnp.take_along_axis(jax.nn.softmax(group_logits, axis=-1), group_idx[:, None], axis=1)
    exp_logits = (h @ router_expert_w).astype(jnp.float32).reshape(N, N_GROUPS, EXPERTS_PER_GROUP)
    exp_logits = jnp.take_along_axis(exp_logits, group_idx[:, None, None], axis=1)[:, 0]
    top_p, top_local = lax.top_k(jax.nn.softmax(exp_logits, axis=-1), TOP_K)
    gate = group_prob * top_p / jnp.sum(top_p, axis=-1, keepdims=True)
    expert_id = (group_idx[:, None] * EXPERTS_PER_GROUP + top_local).astype(jnp.int32)

    flat_e = expert_id.reshape(-1)
    flat_tok = jnp.repeat(jnp.arange(N, dtype=jnp.int32), TOP_K)
    flat_gate = gate.reshape(-1)
    order = jnp.argsort(flat_e)
    sorted_e = flat_e[order]
    counts = jnp.bincount(flat_e, length=N_EXPERTS)
    padded = (counts + MOE_BLOCK - 1) // MOE_BLOCK * MOE_BLOCK
    pad_end = jnp.cumsum(padded)
    pad_start = pad_end - padded
    start = jnp.cumsum(counts) - counts
    dest = pad_start[sorted_e] + jnp.arange(N * TOP_K, dtype=jnp.int32) - start[sorted_e]
    P = N * TOP_K + N_EXPERTS * MOE_BLOCK
    slot_tok = jnp.full((P,), N, jnp.int32).at[dest].set(flat_tok[order])
    slot_gate = jnp.zeros((P,), jnp.float32).at[dest].set(flat_gate[order])
    n_blocks = P // MOE_BLOCK
    block_expert = jnp.minimum(
        jnp.searchsorted(pad_end, jnp.arange(n_blocks, dtype=jnp.int32) * MOE_BLOCK, side='right'),
        N_EXPERTS - 1)
    h_pad = jnp.concatenate([h, jnp.zeros((1, D), h.dtype)], axis=0)
    xb = h_pad[slot_tok].reshape(n_blocks, MOE_BLOCK, D)

    def expert_block(args):
        xblk, e = args
        a = xblk @ w_gate[e]
        b = xblk @ w_up[e]
        return (jax.nn.silu(a) * b) @ w_down[e]

    yb = lax.map(expert_block, (xb, block_expert)).reshape(P, D)
    out = jnp.zeros((N + 1, D), jnp.float32).at[slot_tok].add(yb.astype(jnp.float32) * slot_gate[:, None])
    return out[:N].astype(h.dtype)


def setup_inputs(seed: int = 0) -> dict:
    key = jax.random.key(seed)
    ks = jax.random.split(key, 18)
    f32 = jnp.float32

    def normal(k, shape, scale):
        return jax.random.normal(k, shape, f32) * scale

    def gain(k, shape):
        return 1.0 + 0.05 * jax.random.normal(k, shape, f32)

    dt = jnp.exp(jax.random.uniform(ks[7], (DEPTH, DN_HEADS), f32, math.log(1e-3), math.log(1e-1)))
    return {
        "x": normal(ks[0], (BATCH, SEQ, D_MODEL), 1.0),
        "mix_norm_g": gain(ks[1], (DEPTH, D_MODEL)),
        "w_in": normal(ks[2], (DEPTH, D_MODEL, IN_PROJ_DIM), D_MODEL ** -0.5),
        "conv_a_w": normal(ks[3], (DEPTH, CONV_A_WIDTH, CONV_A_DIM), CONV_A_WIDTH ** -0.5),
        "conv_a_norm_g": gain(ks[4], (DEPTH, CONV_A_DIM)),
        "dn_conv_w": normal(ks[5], (DEPTH, DN_CONV_WIDTH, 3 * DN_DIM), DN_CONV_WIDTH ** -0.5),
        "dn_a_log": jnp.log(jax.random.uniform(ks[6], (DEPTH, DN_HEADS), f32, 1.0, 16.0)),
        "dn_dt_bias": dt + jnp.log(-jnp.expm1(-dt)),
        "dn_norm_g": gain(ks[8], (DEPTH, DN_HEAD_DIM)),
        "w_out": normal(ks[9], (DEPTH, MIX_DIM, D_MODEL), MIX_DIM ** -0.5),
        "ffn_norm_g": gain(ks[10], (DEPTH, D_MODEL)),
        "router_group_w": normal(ks[11], (DEPTH, D_MODEL, N_GROUPS), D_MODEL ** -0.5),
        "router_expert_w": normal(ks[12], (DEPTH, D_MODEL, N_EXPERTS), D_MODEL ** -0.5),
        "w_gate": normal(ks[13], (DEPTH, N_EXPERTS, D_MODEL, EXPERT_FF), D_MODEL ** -0.5),
        "w_up": normal(ks[14], (DEPTH, N_EXPERTS, D_MODEL, EXPERT_FF), D_MODEL ** -0.5),
        "w_down": normal(ks[15], (DEPTH, N_EXPERTS, EXPERT_FF, D_MODEL), EXPERT_FF ** -0.5),
        "final_norm_g": gain(ks[16], (D_MODEL,)),
    }


def reference(x, mix_norm_g, w_in, conv_a_w, conv_a_norm_g, dn_conv_w, dn_a_log, dn_dt_bias,
              dn_norm_g, w_out, ffn_norm_g, router_group_w, router_expert_w, w_gate, w_up, w_down,
              final_norm_g):
    Bsz, S, D = x.shape
    sizes = (CONV_A_DIM,) * 3 + (DN_DIM,) * 4 + (DN_HEADS,) * 2
    split_idx = []
    acc = 0
    for s in sizes[:-1]:
        acc += s
        split_idx.append(acc)
    for l in range(DEPTH):
        h = rms_norm(x, mix_norm_g[l])
        proj = h @ w_in[l]
        a_h, a_b, a_c, d_q, d_k, d_v, d_z, d_beta, d_alpha = jnp.split(proj, split_idx, axis=-1)
        y_a = short_conv_mixer(a_h, a_b, a_c, conv_a_w[l], conv_a_norm_g[l])
        y_b = gated_deltanet(d_q, d_k, d_v, d_z, d_beta, d_alpha, dn_conv_w[l], dn_a_log[l],
                             dn_dt_bias[l], dn_norm_g[l])
        x = x + jnp.concatenate([y_a, y_b], axis=-1) @ w_out[l]
        h = rms_norm(x, ffn_norm_g[l]).reshape(Bsz * S, D)
        x = x + hier_moe(h, router_group_w[l], router_expert_w[l], w_gate[l], w_up[l], w_down[l]).reshape(Bsz, S, D)
    return rms_norm(x, final_norm_g)
```

```python
from contextlib import ExitStack
import numpy as np
import concourse.bass as bass
import concourse.mybir as mybir
from concourse.bass_utils import run_bass_kernel_spmd

F32 = mybir.dt.float32
BF16 = mybir.dt.bfloat16
I32 = mybir.dt.int32
ALU = mybir.AluOpType
AF = mybir.ActivationFunctionType
AX = mybir.AxisListType

S = 2048
D = 1024
NT = 16
NCH = 32
NE = 32
FF = 512
CAP = 256
EPS = 1e-6
DN_DT = F32


class _Eng:
    def __init__(self, eng, sem, is_pe=False):
        self.eng = eng
        self.sem = sem
        self.count = 0
        self.waited = {}
        self.is_pe = is_pe
        self.rec = []


class Prog:
    def __init__(self, nc, n_dma_sems=10):
        self.nc = nc
        self.E = {
            "pe": _Eng(nc.tensor, nc.alloc_semaphore("s_pe"), True),
            "act": _Eng(nc.scalar, nc.alloc_semaphore("s_act")),
            "dve": _Eng(nc.vector, nc.alloc_semaphore("s_dve")),
            "pool": _Eng(nc.gpsimd, nc.alloc_semaphore("s_pool")),
            "sp": _Eng(nc.sync, nc.alloc_semaphore("s_sp")),
        }
        self.dma_sems = {}
        for q in ("sp", "pool", "act"):
            self.dma_sems[q] = [[nc.alloc_semaphore(f"d_{q}{i}"), 0] for i in range(n_dma_sems)]
        self.dma_rr = {"sp": 0, "pool": 0, "act": 0}
        self.rw = {}

    def _deps(self, reads, writes):
        deps = {}

        def add(tok):
            if tok is None:
                return
            s, v = tok
            if deps.get(s.num, (None, -1))[1] < v:
                deps[s.num] = (s, v)

        for k in reads:
            st = self.rw.get(k)
            if st is not None:
                add(st["w"])
        for k in writes:
            st = self.rw.get(k)
            if st is not None:
                add(st["w"])
                for t in st["r"].values():
                    add(t)
        return deps

    def _commit(self, tok, reads, writes):
        for k in writes:
            self.rw[k] = {"w": tok, "r": {}}
        for k in reads:
            st = self.rw.setdefault(k, {"w": None, "r": {}})
            s, v = tok
            if st["r"].get(s.num, (None, -1))[1] < v:
                st["r"][s.num] = tok

    def _wait(self, e, deps, skip_own=False):
        for num, (s, v) in deps.items():
            if skip_own and num == e.sem.num:
                continue
            if e.waited.get(num, 0) < v:
                e.rec.append(("w", s, v))
                e.waited[num] = v

    def op(self, en, fn, reads=(), writes=()):
        e = self.E[en]
        deps = self._deps(reads, writes)
        self._wait(e, deps, skip_own=e.is_pe)
        e.count += 1
        e.rec.append(("i", fn, e.sem, 1))
        tok = (e.sem, e.count)
        self._commit(tok, reads, writes)
        return tok

    def dma(self, q, fn, reads=(), writes=()):
        e = self.E[q]
        deps = self._deps(reads, writes)
        self._wait(e, deps)
        pool = self.dma_sems[q]
        i = self.dma_rr[q]
        self.dma_rr[q] = (i + 1) % len(pool)
        slot = pool[i]
        s, v = slot
        if v > 0 and e.waited.get(s.num, 0) < v:
            e.rec.append(("w", s, v))
            e.waited[s.num] = v
        e.rec.append(("i", fn, s, 16))
        slot[1] = v + 16
        tok = (s, v + 16)
        self._commit(tok, reads, writes)
        return tok

    def barrier(self):
        toks = []
        for e in self.E.values():
            if e.count > 0:
                toks.append((e.sem, e.count))
        for q in self.dma_sems.values():
            for s, v in q:
                if v > 0:
                    toks.append((s, v))
        for e in self.E.values():
            for s, v in toks:
                if s.num == e.sem.num:
                    continue
                if e.waited.get(s.num, 0) < v:
                    e.rec.append(("w", s, v))
                    e.waited[s.num] = v

    def finish(self, keys):
        e = self.E["sp"]
        self._wait(e, self._deps(keys, ()))

    def emit(self):
        nc = self.nc

        def replay(e):
            def f(eng):
                for r in e.rec:
                    if r[0] == "w":
                        eng.wait_ge(r[1], r[2])
                    else:
                        r[1](eng).then_inc(r[2], r[3])
            return f

        with nc.Block() as block:
            block.sync(replay(self.E["sp"]))
            block.scalar(replay(self.E["act"]))
            block.vector(replay(self.E["dve"]))
            block.gpsimd(replay(self.E["pool"]))
            block.tensor(replay(self.E["pe"]))


def interleave(gens):
    gens = list(gens)
    while gens:
        for g in list(gens):
            try:
                next(g)
            except StopIteration:
                gens.remove(g)


def build(stage="full"):
    nc = bass.Bass("TRN2", target_bir_lowering=False)
    P = Prog(nc)

    def din(name, shape, dt=F32):
        return nc.dram_tensor(name, list(shape), dt, kind="ExternalInput")

    x_tok = din("x_tok", [S, D]).ap()
    xT = din("xT", [D, S]).ap()
    w_in_r = din("w_in_r", [28, 128, 8, 128]).ap()
    w_ba = din("w_ba", [128, 8, 8]).ap()
    g1 = din("g1", [128, 8]).ap()
    caw = din("caw", [128, 4, 3]).ap()
    gA = din("gA", [128, 4]).ap()
    dcw = din("dcw", [128, 12, 4]).ap()
    alog_h = din("alog", [4])
    dtb_h = din("dtb", [4])
    gdn = din("gdn", [128, 1]).ap()
    w_out_r = din("w_out_r", [128, 8, D]).ap()
    g2_h = din("g2", [D])
    wr_r = din("wr_r", [128, 8, 36]).ap()
    w_gate = din("w_gate", [NE, D, FF]).ap()
    w_up = din("w_up", [NE, D, FF]).ap()
    w_down = din("w_down", [NE, FF, D]).ap()
    g3_h = din("g3", [D])
    out = nc.dram_tensor("out", [S, D], F32, kind="ExternalOutput").ap()

    DUMP = NE * CAP
    Xs = nc.dram_tensor("Xs_scr", [NE * CAP + 128, D], BF16, kind="Internal").ap()
    Ys = nc.dram_tensor("Ys_scr", [NE * CAP + 128, D], F32, kind="Internal").ap()
    stack0 = ExitStack()

    def sb(st, name, shape, dt, side=None):
        return st.enter_context(nc.sbuf_tensor(name, list(shape), dt, side=side))

    psum = nc.alloc_psum_tensor("psum", [128, 8 * 512], F32)
    bank_rr = [0]
    bank_lim = [8]

    def bank(n=1):
        b = bank_rr[0]
        if b + n > bank_lim[0]:
            b = 0
        bank_rr[0] = (b + n) % bank_lim[0]
        return b

    def psv(b, parts=128, n=512, nb=1):
        return psum[0:parts, b * 512:b * 512 + n] if nb == 1 else psum[0:parts, b * 512:(b + nb) * 512]

    def pk(b):
        return ("ps", b)

    ident_f = sb(stack0, "ident_f", [128, 128], F32)
    ident_b = sb(stack0, "ident_b", [128, 128], BF16)
    ones_f = sb(stack0, "ones_f", [128, 128], F32)
    ones_b = sb(stack0, "ones_b", [128, 128], BF16)
    bd64_b = sb(stack0, "bd64_b", [128, 128], BF16)
    bd64_f = sb(stack0, "bd64_f", [128, 128], F32)
    u64 = sb(stack0, "u64", [64, 64], F32)
    maskc8 = sb(stack0, "maskc8", [64, 8, 64], F32)
    masks8 = sb(stack0, "masks8", [64, 8, 64], F32)
    eye8 = sb(stack0, "eye8", [64, 8, 64], F32)

    P.op("pool", lambda e: e.memset(ident_f[:], 1.0), writes=["ident_f"])
    P.op("pool", lambda e: e.affine_select(out=ident_f[:], in_=ident_f[:], pattern=[[-1, 128]], compare_op=ALU.is_equal, fill=0.0, base=0, channel_multiplier=1), reads=["ident_f"], writes=["ident_f"])
    P.op("dve", lambda e: e.tensor_copy(out=ident_b[:], in_=ident_f[:]), reads=["ident_f"], writes=["ident_b"])
    P.op("pool", lambda e: e.memset(ones_f[:], 1.0), writes=["ones_f"])
    P.op("pool", lambda e: e.memset(ones_b[:], 1.0), writes=["ones_b"])
    P.op("pool", lambda e: e.memset(bd64_f[:], 1.0), writes=["bd64_f"])
    P.op("pool", lambda e: e.affine_select(out=bd64_f[:, 0:64], in_=bd64_f[:, 0:64], pattern=[[0, 64]], compare_op=ALU.is_ge, fill=0.0, base=63, channel_multiplier=-1), reads=["bd64_f"], writes=["bd64_f"])
    P.op("pool", lambda e: e.affine_select(out=bd64_f[:, 64:128], in_=bd64_f[:, 64:128], pattern=[[0, 64]], compare_op=ALU.is_ge, fill=0.0, base=-64, channel_multiplier=1), reads=["bd64_f"], writes=["bd64_f"])
    P.op("dve", lambda e: e.tensor_copy(out=bd64_b[:], in_=bd64_f[:]), reads=["bd64_f"], writes=["bd64_b"])
    P.op("pool", lambda e: e.memset(u64[:], 1.0), writes=["u64"])
    P.op("pool", lambda e: e.affine_select(out=u64[:], in_=u64[:], pattern=[[1, 64]], compare_op=ALU.is_ge, fill=0.0, base=0, channel_multiplier=-1), reads=["u64"], writes=["u64"])
    for t_, op_, nm in ((maskc8, ALU.is_ge, "maskc8"), (masks8, ALU.is_gt, "masks8"), (eye8, ALU.is_equal, "eye8")):
        P.op("pool", lambda e, t_=t_: e.memset(t_[:], 1.0), writes=[nm])
        P.op("pool", lambda e, t_=t_, op_=op_: e.affine_select(out=t_[:], in_=t_[:], pattern=[[0, 8], [-1, 64]], compare_op=op_, fill=0.0, base=0, channel_multiplier=1), reads=[nm], writes=[nm])

    g1_s = sb(stack0, "g1_s", [128, 8], F32)
    caw_s = sb(stack0, "caw_s", [128, 4, 3], F32)
    gA_s = sb(stack0, "gA_s", [128, 4], F32)
    dcw_s = sb(stack0, "dcw_s", [128, 12, 4], F32)
    gdn_s = sb(stack0, "gdn_s", [128, 1], F32)
    P.dma("sp", lambda e: e.dma_start(out=g1_s[:], in_=g1), writes=["g1_s"])
    P.dma("sp", lambda e: e.dma_start(out=caw_s[:], in_=caw), writes=["caw_s"])
    P.dma("sp", lambda e: e.dma_start(out=gA_s[:], in_=gA), writes=["gA_s"])
    P.dma("sp", lambda e: e.dma_start(out=dcw_s[:], in_=dcw), writes=["dcw_s"])
    P.dma("sp", lambda e: e.dma_start(out=gdn_s[:], in_=gdn), writes=["gdn_s"])

    stackR = ExitStack()
    yT = sb(stackR, "yT", [128, 8, S], BF16, side="right")

    stA = ExitStack()
    hT = sb(stA, "hT", [128, 8, S], BF16)
    wring = sb(stA, "wring", [128, 2, 8, 128], BF16)
    wba_s = sb(stA, "wba_s", [128, 8, 8], BF16)
    pc = sb(stA, "pc", [128, 4, S], F32)
    cvt = sb(stA, "cvt", [128, S], F32)
    sqb = sb(stA, "sqb", [128, 2, S], BF16)
    tb_ba = sb(stA, "tb_ba", [64, NCH, 8], F32)
    tb_alog = sb(stA, "tb_alog", [64, NCH, 4], F32)
    tb_dtb = sb(stA, "tb_dtb", [64, NCH, 4], F32)
    tb_beta = sb(stA, "tb_beta", [64, NCH, 4], F32)
    tb_nbeta = sb(stA, "tb_nbeta", [64, NCH, 4], F32)
    tb_t0 = sb(stA, "tb_t0", [64, NCH, 4], F32)
    tb_t1 = sb(stA, "tb_t1", [64, NCH, 4], F32)
    tb_g = sb(stA, "tb_g", [64, NCH, 4], F32)
    tb_gc = sb(stA, "tb_gc", [64, NCH, 4], F32)
    tb_gl = sb(stA, "tb_gl", [128, NCH, 4], F32)
    tb_egl = sb(stA, "tb_egl", [128, NCH, 4], F32)
    tb_kbe = sb(stA, "tb_kbe", [64, NCH, 4], F32)
    tb_kdec = sb(stA, "tb_kdec", [64, NCH, 4], F32)

    zt = sb(stA, "zt", [128, 2048], BF16)
    P.op("pool", lambda e: e.memset(zt[:], 0.0), writes=["zt"])
    xz_keys = []
    for i in range(NE):
        P.dma("sp", lambda e, i=i: e.dma_start(out=Xs[i * 256:(i + 1) * 256, :].rearrange("(p b) d -> p (b d)", b=2), in_=zt[:]), reads=["zt"], writes=[("Xz", i)])
        xz_keys.append(("Xz", i))
    P.dma("sp", lambda e: e.dma_start(out=Xs[DUMP:DUMP + 128, :], in_=zt[:, 0:D]), reads=["zt"], writes=[("Xz", NE)])
    xz_keys.append(("Xz", NE))
    P.dma("sp", lambda e: e.dma_start(out=Ys[DUMP:DUMP + 128, :], in_=zt[:].bitcast(F32)), reads=["zt"], writes=[("Yz", 0)])
    P.dma("pool", lambda e: e.dma_start(out=wba_s[:], in_=w_ba), writes=["wba_s"])
    ab_s = sb(stA, "ab_s", [64, 2, 4], F32)
    P.dma("sp", lambda e: e.dma_start(out=ab_s[:, 0, :], in_=bass.AP(alog_h, 0, [[0, 64], [1, 4]])), writes=["ab_s0"])
    P.dma("sp", lambda e: e.dma_start(out=ab_s[:, 1, :], in_=bass.AP(dtb_h, 0, [[0, 64], [1, 4]])), writes=["ab_s1"])
    P.op("dve", lambda e: e.tensor_copy(out=tb_alog[:], in_=ab_s[:, 0:1, :].to_broadcast([64, NCH, 4])), reads=["ab_s0"], writes=["tb_alog"])
    P.op("dve", lambda e: e.tensor_copy(out=tb_dtb[:], in_=ab_s[:, 1:2, :].to_broadcast([64, NCH, 4])), reads=["ab_s1"], writes=["tb_dtb"])

    stA2 = ExitStack()
    xs = sb(stA2, "xs", [128, 2, S], F32)
    rbc = sb(stA2, "rbc", [128, S], F32)
    for kc in range(8):
        sl = kc % 2
        P.dma("sp", lambda e, kc=kc, sl=sl: e.dma_start(out=xs[:, sl, :], in_=xT[kc * 128:(kc + 1) * 128, :]), writes=[("xs", sl)])
        P.op("act", lambda e, sl=sl: e.activation(out=sqb[:, sl, :], in_=xs[:, sl, :], func=AF.Square), reads=[("xs", sl)], writes=[("sqb", sl)])
        for tb in range(4):
            P.op("pe", lambda e, kc=kc, sl=sl, tb=tb: e.matmul(psv(tb), lhsT=ones_b[:], rhs=sqb[:, sl, tb * 512:(tb + 1) * 512], start=(kc == 0), stop=(kc == 7)),
                 reads=[("sqb", sl), "ones_b"], writes=[pk(tb)])
    for tb in range(4):
        P.op("act", lambda e, tb=tb: e.activation(out=rbc[:, tb * 512:(tb + 1) * 512], in_=psv(tb), func=AF.Sqrt, scale=1.0 / D, bias=EPS), reads=[pk(tb)], writes=[("rbc", tb)])
        P.op("dve", lambda e, tb=tb: e.reciprocal(out=rbc[:, tb * 512:(tb + 1) * 512], in_=rbc[:, tb * 512:(tb + 1) * 512]), reads=[("rbc", tb)], writes=[("rbc", tb)])
    for kc in range(8):
        sl = kc % 2
        P.dma("sp", lambda e, kc=kc, sl=sl: e.dma_start(out=xs[:, sl, :], in_=xT[kc * 128:(kc + 1) * 128, :]), writes=[("xs", sl)])
        P.op("dve", lambda e, kc=kc, sl=sl: e.scalar_tensor_tensor(out=hT[:, kc, :], in0=xs[:, sl, :], scalar=g1_s[:, kc:kc + 1], in1=rbc[:], op0=ALU.mult, op1=ALU.mult),
             reads=[("xs", sl), "g1_s"] + [("rbc", tb) for tb in range(4)], writes=[("hT", kc)])
    stA2.close()
    hT_keys = [("hT", kc) for kc in range(8)]

    wr_i = [0]

    def proj_chunk(c, slot):
        ws = wr_i[0] % 2
        wr_i[0] += 1
        P.dma("pool", lambda e: e.dma_start(out=wring[:, ws, :, :], in_=w_in_r[c]), writes=[("wring", ws)])
        for tb in range(4):
            b = bank()
            for kc in range(8):
                P.op("pe", lambda e, kc=kc, tb=tb, b=b: e.matmul(psv(b), lhsT=wring[:, ws, kc, :], rhs=hT[:, kc, tb * 512:(tb + 1) * 512], start=(kc == 0), stop=(kc == 7)),
                     reads=[("wring", ws), ("hT", kc)], writes=[pk(b)])
            P.op("act", lambda e, tb=tb, b=b: e.copy(out=pc[:, slot, tb * 512:(tb + 1) * 512], in_=psv(b)), reads=[pk(b)], writes=[("pc", slot, tb)])

    def pck(slot):
        return [("pc", slot, tb) for tb in range(4)]

    bb = bank()
    for c in range(NCH):
        for kc in range(8):
            P.op("pe", lambda e, c=c, kc=kc: e.matmul(psum[0:64, bb * 512 + c * 8: bb * 512 + c * 8 + 8], lhsT=hT[:, kc, c * 64:(c + 1) * 64], rhs=wba_s[:, kc, :], start=(kc == 0), stop=(kc == 7)),
                 reads=[("hT", kc), "wba_s"], writes=[pk(bb)])
    P.op("act", lambda e: e.copy(out=tb_ba[:].rearrange("p c k -> p (c k)"), in_=psum[0:64, bb * 512: bb * 512 + 256]), reads=[pk(bb)], writes=["tb_ba"])
    P.op("act", lambda e: e.activation(out=tb_beta[:], in_=tb_ba[:, :, 0:4], func=AF.Sigmoid), reads=["tb_ba"], writes=["tb_beta"])
    P.op("dve", lambda e: e.tensor_scalar(out=tb_nbeta[:], in0=tb_beta[:], scalar1=-1.0, scalar2=None, op0=ALU.mult), reads=["tb_beta"], writes=["tb_nbeta"])
    P.op("dve", lambda e: e.tensor_tensor(out=tb_t0[:], in0=tb_ba[:, :, 4:8], in1=tb_dtb[:], op=ALU.add), reads=["tb_ba", "tb_dtb"], writes=["tb_t0"])
    P.op("act", lambda e: e.activation(out=tb_t1[:], in_=tb_t0[:], func=AF.Abs), reads=["tb_t0"], writes=["tb_t1"])
    P.op("act", lambda e: e.activation(out=tb_t1[:], in_=tb_t1[:], func=AF.Exp, scale=-1.0), reads=["tb_t1"], writes=["tb_t1"])
    P.op("act", lambda e: e.activation(out=tb_t1[:], in_=tb_t1[:], func=AF.Ln, bias=1.0), reads=["tb_t1"], writes=["tb_t1"])
    P.op("dve", lambda e: e.tensor_scalar(out=tb_t0[:], in0=tb_t0[:], scalar1=0.0, scalar2=None, op0=ALU.max), reads=["tb_t0"], writes=["tb_t0"])
    P.op("dve", lambda e: e.tensor_tensor(out=tb_t0[:], in0=tb_t0[:], in1=tb_t1[:], op=ALU.add), reads=["tb_t0", "tb_t1"], writes=["tb_t0"])
    P.op("act", lambda e: e.activation(out=tb_alog[:], in_=tb_alog[:], func=AF.Exp), reads=["tb_alog"], writes=["tb_alog"])
    P.op("dve", lambda e: e.scalar_tensor_tensor(out=tb_g[:], in0=tb_t0[:], scalar=-1.0, in1=tb_alog[:], op0=ALU.mult, op1=ALU.mult), reads=["tb_t0", "tb_alog"], writes=["tb_g"])
    gflat = tb_g[:].rearrange("p c k -> p (c k)")
    b1 = bank()
    P.op("pe", lambda e: e.matmul(psum[0:64, b1 * 512:b1 * 512 + 128], lhsT=u64[:], rhs=gflat, start=True, stop=True), reads=["u64", "tb_g"], writes=[pk(b1)])
    P.op("act", lambda e: e.copy(out=tb_gc[:].rearrange("p c k -> p (c k)"), in_=psum[0:64, b1 * 512:b1 * 512 + 128]), reads=[pk(b1)], writes=["tb_gc"])
    b2 = bank()
    P.op("pe", lambda e: e.matmul(psum[0:128, b2 * 512:b2 * 512 + 128], lhsT=ones_f[0:64, :], rhs=gflat, start=True, stop=True), reads=["ones_f", "tb_g"], writes=[pk(b2)])
    P.op("act", lambda e: e.copy(out=tb_gl[:].rearrange("p c k -> p (c k)"), in_=psum[0:128, b2 * 512:b2 * 512 + 128]), reads=[pk(b2)], writes=["tb_gl"])
    P.op("act", lambda e: e.activation(out=tb_egl[:], in_=tb_gl[:], func=AF.Exp), reads=["tb_gl"], writes=["tb_egl"])
    P.op("act", lambda e: e.activation(out=tb_t1[:], in_=tb_gc[:], func=AF.Exp), reads=["tb_gc"], writes=["tb_t1"])
    P.op("dve", lambda e: e.tensor_tensor(out=tb_kbe[:], in0=tb_beta[:], in1=tb_t1[:], op=ALU.mult), reads=["tb_beta", "tb_t1"], writes=["tb_kbe"])
    P.op("dve", lambda e: e.tensor_tensor(out=tb_kdec[:], in0=tb_gl[0:64], in1=tb_gc[:], op=ALU.subtract), reads=["tb_gl", "tb_gc"], writes=["tb_kdec"])
    P.op("act", lambda e: e.activation(out=tb_kdec[:], in_=tb_kdec[:], func=AF.Exp), reads=["tb_kdec"], writes=["tb_kdec"])

    def conv(eng, dst, dkeys, src, skeys, wtile, wkey, widx, K):
        P.op("act", lambda e: e.activation(out=dst, in_=src, func=AF.Copy, scale=wtile[:, widx, K - 1:K]), reads=skeys + [wkey], writes=dkeys)
        for j in range(K - 1):
            sh = K - 1 - j
            P.op("dve", lambda e, j=j, sh=sh: e.scalar_tensor_tensor(out=dst[:, sh:], in0=src[:, 0:S - sh], scalar=wtile[:, widx, j:j + 1], in1=dst[:, sh:], op0=ALU.mult, op1=ALU.add),
                 reads=skeys + [wkey] + dkeys, writes=dkeys)

    sq_i = [0]

    def inv_rms(src, skeys, lhs, lkey, scale):
        sl = sq_i[0] % 2
        sq_i[0] += 1
        P.op("act", lambda e: e.activation(out=sqb[:, sl, :], in_=src, func=AF.Square), reads=skeys, writes=[("sqb", sl)])
        for tb in range(4):
            b = bank()
            P.op("pe", lambda e, tb=tb, b=b: e.matmul(psv(b), lhsT=lhs, rhs=sqb[:, sl, tb * 512:(tb + 1) * 512], start=True, stop=True), reads=[("sqb", sl), lkey], writes=[pk(b)])
            P.op("act", lambda e, tb=tb, b=b: e.activation(out=cvt[:, tb * 512:(tb + 1) * 512], in_=psv(b), func=AF.Ln, scale=scale, bias=EPS), reads=[pk(b)], writes=["cvt"])
        P.op("act", lambda e: e.activation(out=cvt[:], in_=cvt[:], func=AF.Exp, scale=-0.5), reads=["cvt"], writes=["cvt"])

    for j in range(4):
        proj_chunk(j, 0)
        proj_chunk(4 + j, 1)
        proj_chunk(8 + j, 2)
        P.op("dve", lambda e: e.tensor_tensor(out=pc[:, 0, :], in0=pc[:, 0, :], in1=pc[:, 2, :], op=ALU.mult), reads=pck(0) + pck(2), writes=pck(0))
        conv("pool", pc[:, 2, :], pck(2), pc[:, 0, :], pck(0), caw_s, "caw_s", j, 3)
        P.op("dve", lambda e: e.tensor_tensor(out=pc[:, 2, :], in0=pc[:, 2, :], in1=pc[:, 1, :], op=ALU.mult), reads=pck(1) + pck(2), writes=pck(2))
        inv_rms(pc[:, 2, :], pck(2), bd64_b[:], "bd64_b", 1.0 / 64)
        P.op("dve", lambda e, j=j: e.scalar_tensor_tensor(out=yT[:, j, :], in0=pc[:, 2, :], scalar=gA_s[:, j:j + 1], in1=cvt[:], op0=ALU.mult, op1=ALU.mult),
             reads=pck(2) + ["gA_s", "cvt"], writes=[("yT", j)])

    dn = ExitStack()
    GW = 8
    NSET = 2
    qb = sb(dn, "qb", [128, S], BF16)
    kb = sb(dn, "kb", [128, S], BF16)
    vb = sb(dn, "vb", [128, S], BF16)
    NPAR = 3
    ATg = sb(dn, "ATg", [64, NPAR, GW, 64], BF16)
    Kdg = sb(dn, "Kdg", [64, NPAR, GW, 128], BF16)
    Ug = sb(dn, "Ug", [64, NPAR, GW, 128], F32)
    WTg = sb(dn, "WTg", [128, NPAR, GW * 64], BF16)
    qsb = sb(dn, "qsb", [128, S], BF16)
    Sst = sb(dn, "Sst", [128, 2, 128], F32)
    Sb = sb(dn, "Sb", [128, 2, 128], BF16)
    vnew = sb(dn, "vnew", [64, 2, 128], BF16)
    Og = sb(dn, "Og", [64, 4, 128], F32)
    Ogb = sb(dn, "Ogb", [64, 4, 128], BF16)
    Osq = sb(dn, "Osq", [64, 4, 128], F32)
    oss = sb(dn, "oss", [64, 4], F32)

    SC = []
    s0 = {"id": 0}
    s0["GU"] = sb(dn, "s0_GU", [64, GW, 64], F32)[:]
    s0["E"] = sb(dn, "s0_E", [128, GW * 64], F32)[:]
    for nm in ("DS", "DC", "A", "N0", "B0", "N1", "B1", "R0", "R1"):
        s0[nm] = sb(dn, "s0_" + nm, [64, GW, 64], BF16)[:]
    s0["Kbe"] = sb(dn, "s0_Kbe", [64, GW, 128], BF16)[:]
    s0["Vb"] = sb(dn, "s0_Vb", [64, GW, 128], BF16)[:]
    SC.append(s0)

    def bfv(ap, c):
        return ap.bitcast(BF16).rearrange("p (a c) -> p a c", c=c)

    s1 = {"id": 1}
    s1["GU"] = pc[0:64, 1, 0:512].rearrange("p (a c) -> p a c", c=64)
    s1["E"] = pc[:, 1, 512:1024]
    for i_, nm in enumerate(("DS", "DC", "A", "N0")):
        s1[nm] = bfv(pc[0:64, 1, 1024 + 256 * i_:1280 + 256 * i_], 64)
    for i_, nm in enumerate(("B0", "N1", "B1", "R0", "R1")):
        s1[nm] = bfv(pc[0:64, 2, 256 * i_:256 * (i_ + 1)], 64)
    s1["Kbe"] = bfv(pc[0:64, 2, 1280:1792], 128)
    s1["Vb"] = bfv(cvt[0:64, 0:512], 128)
    SC.append(s1)

    def ops512(v):
        return v.rearrange("p a c -> p (a c)")

    def dn_phase1(h, cg, par, Sx):
        sid = Sx["id"]

        def K(nm):
            ks_ = [(nm, sid)]
            if sid == 1:
                ks_.append("alias1")
            return ks_

        def KW(nm):
            return [(nm, sid)]

        qs = pc[:, 0, :]
        c0 = cg * GW
        cols = slice(c0 * 64, (c0 + GW) * 64)
        qk = [("pc", 0, cg)]
        GU, DS, DC, A, E_s, Kbe, Vb = Sx["GU"], Sx["DS"], Sx["DC"], Sx["A"], Sx["E"], Sx["Kbe"], Sx["Vb"]
        al = ["alias1"] if sid == 1 else []
        P.op("pool", lambda e: e.tensor_tensor(out=GU[:], in0=u64[:, None, :].to_broadcast([64, GW, 64]), in1=tb_g[:, c0:c0 + GW, h:h + 1].to_broadcast([64, GW, 64]), op=ALU.mult),
             reads=["u64", "tb_g"] + al, writes=KW("GU"))
        bg = bank()
        P.op("pe", lambda e: e.matmul(psv(bg), lhsT=ones_f[0:64, :], rhs=ops512(GU[:]), start=True, stop=True), reads=["ones_f"] + K("GU"), writes=[pk(bg)])
        P.op("dve", lambda e: e.tensor_tensor(out=GU[:], in0=psv(bg, 64).rearrange("p (a c) -> p a c", c=64), in1=tb_gc[:, c0:c0 + GW, h:h + 1].to_broadcast([64, GW, 64]), op=ALU.subtract),
             reads=[pk(bg), "tb_gc"] + al, writes=KW("GU"))
        P.op("dve", lambda e: e.tensor_scalar(out=GU[:], in0=GU[:], scalar1=0.0, scalar2=None, op0=ALU.max), reads=K("GU"), writes=KW("GU"))
        P.op("act", lambda e: e.activation(out=GU[:], in_=GU[:], func=AF.Exp, scale=-1.0), reads=K("GU"), writes=KW("GU"))
        P.op("act", lambda e: e.activation(out=E_s[:], in_=psv(bg), func=AF.Exp), reads=[pk(bg)] + al, writes=KW("E"))
        yield
        P.op("pool", lambda e: e.tensor_tensor(out=DS[:], in0=GU[:], in1=masks8[:], op=ALU.mult), reads=K("GU") + ["masks8"], writes=KW("DS"))
        P.op("pool", lambda e: e.tensor_tensor(out=DS[:], in0=DS[:], in1=tb_nbeta[:, c0:c0 + GW, h:h + 1].to_broadcast([64, GW, 64]), op=ALU.mult), reads=K("DS") + ["tb_nbeta"], writes=KW("DS"))
        P.op("pool", lambda e: e.tensor_tensor(out=DC[:], in0=GU[:], in1=maskc8[:], op=ALU.mult), reads=K("GU") + ["maskc8"], writes=KW("DC"))
        N0, B0 = Sx["N0"], Sx["B0"]
        bk = bank()
        for a in range(GW):
            cs = slice((c0 + a) * 64, (c0 + a + 1) * 64)
            P.op("pe", lambda e, a=a, cs=cs: e.matmul(psum[0:64, bk * 512 + a * 64: bk * 512 + (a + 1) * 64], lhsT=kb[:, cs], rhs=kb[:, cs], start=True, stop=True), reads=["kb"], writes=[pk(bk)])
        P.op("dve", lambda e: e.tensor_tensor(out=ops512(N0[:]), in0=psv(bk, 64), in1=ops512(DS[:]), op=ALU.mult), reads=[pk(bk)] + K("DS"), writes=KW("N0"))
        yield
        bq = bank()
        for a in range(GW):
            cs = slice((c0 + a) * 64, (c0 + a + 1) * 64)
            P.op("pe", lambda e, a=a, cs=cs: e.matmul(psum[0:64, bq * 512 + a * 64: bq * 512 + (a + 1) * 64], lhsT=qb[:, cs], rhs=kb[:, cs], start=True, stop=True), reads=["qb", "kb"], writes=[pk(bq)])
        P.op("dve", lambda e: e.tensor_tensor(out=ops512(A[:]), in0=psv(bq, 64), in1=ops512(DC[:]), op=ALU.mult), reads=[pk(bq)] + K("DC"), writes=KW("A"))
        P.op("pool", lambda e: e.tensor_tensor(out=qsb[:, cols], in0=qs[:, cols], in1=E_s[:], op=ALU.mult), reads=qk + K("E"), writes=[("qsb", cg)])
        yield
        bt = bank()
        for a in range(GW):
            P.op("pe", lambda e, a=a: e.matmul(psum[0:64, bt * 512 + a * 64: bt * 512 + (a + 1) * 64], lhsT=N0[:, a, :], rhs=ident_b[0:64, 0:64], start=True, stop=True), reads=K("N0") + ["ident_b"], writes=[pk(bt)])
        P.op("act", lambda e: e.copy(out=ops512(B0[:]), in_=psv(bt, 64)), reads=[pk(bt)] + al, writes=KW("B0"))
        yield
        ba_ = bank()
        for a in range(GW):
            P.op("pe", lambda e, a=a: e.matmul(psum[0:64, ba_ * 512 + a * 64: ba_ * 512 + (a + 1) * 64], lhsT=A[:, a, :], rhs=ident_b[0:64, 0:64], start=True, stop=True), reads=K("A") + ["ident_b"], writes=[pk(ba_)])
        P.op("act", lambda e: e.copy(out=ops512(ATg[:, par]), in_=psv(ba_, 64)), reads=[pk(ba_)], writes=[("ATg", par)])
        R = [Sx["R0"], Sx["R1"]]
        Nn = [Sx["N0"], Sx["N1"]]
        Bn = [Sx["B0"], Sx["B1"]]
        P.op("pool", lambda e: e.tensor_tensor(out=R[0][:], in0=B0[:], in1=eye8[:], op=ALU.add), reads=K("B0") + ["eye8"], writes=KW("R0"))
        yield
        cur = 0
        for lvl in range(5):
            nxt = 1 - cur
            nk, bkk = "N%d" % cur, "B%d" % cur
            nk2, bk2 = "N%d" % nxt, "B%d" % nxt
            rk, rk2 = "R%d" % cur, "R%d" % nxt
            pn = bank()
            for a in range(GW):
                P.op("pe", lambda e, a=a, cur=cur, pn=pn: e.matmul(psum[0:64, pn * 512 + a * 64: pn * 512 + (a + 1) * 64], lhsT=Bn[cur][:, a, :], rhs=Nn[cur][:, a, :], start=True, stop=True),
                     reads=K(nk) + K(bkk), writes=[pk(pn)])
            P.op("act", lambda e, nxt=nxt, pn=pn: e.copy(out=ops512(Nn[nxt][:]), in_=psv(pn, 64)), reads=[pk(pn)] + al, writes=KW(nk2))
            yield
            if lvl < 4:
                pb = bank()
                for a in range(GW):
                    P.op("pe", lambda e, a=a, cur=cur, pb=pb: e.matmul(psum[0:64, pb * 512 + a * 64: pb * 512 + (a + 1) * 64], lhsT=Nn[cur][:, a, :], rhs=Bn[cur][:, a, :], start=True, stop=True),
                         reads=K(nk) + K(bkk), writes=[pk(pb)])
                P.op("act", lambda e, nxt=nxt, pb=pb: e.copy(out=ops512(Bn[nxt][:]), in_=psv(pb, 64)), reads=[pk(pb)] + al, writes=KW(bk2))
                yield
            pr = bank()
            for a in range(GW):
                P.op("pe", lambda e, a=a, cur=cur, nxt=nxt, pr=pr: e.matmul(psum[0:64, pr * 512 + a * 64: pr * 512 + (a + 1) * 64], lhsT=Nn[nxt][:, a, :], rhs=R[cur][:, a, :], start=True, stop=True),
                     reads=K(nk2) + K(rk), writes=[pk(pr)])
            P.op("dve", lambda e, cur=cur, nxt=nxt, pr=pr: e.tensor_tensor(out=ops512(R[nxt][:]), in0=psv(pr, 64), in1=ops512(R[cur][:]), op=ALU.add), reads=[pk(pr)] + K(rk), writes=KW(rk2))
            cur = nxt
            yield
        Rf = R[cur]
        rfk = "R%d" % cur
        bkt = bank(2)
        for a in range(GW):
            cs = slice((c0 + a) * 64, (c0 + a + 1) * 64)
            P.op("pe", lambda e, a=a, cs=cs: e.matmul(psum[0:64, bkt * 512 + a * 128: bkt * 512 + (a + 1) * 128], lhsT=kb[:, cs], rhs=ident_b[:], start=True, stop=True), reads=["kb", "ident_b"], writes=[pk(bkt), pk(bkt + 1)])
        kt3 = psum[0:64, bkt * 512:(bkt + 2) * 512].rearrange("p (a c) -> p a c", c=128)
        P.op("dve", lambda e: e.tensor_tensor(out=Kbe[:], in0=kt3, in1=tb_kbe[:, c0:c0 + GW, h:h + 1].to_broadcast([64, GW, 128]), op=ALU.mult), reads=[pk(bkt), pk(bkt + 1), "tb_kbe"] + al, writes=KW("Kbe"))
        P.op("dve", lambda e: e.tensor_tensor(out=Kdg[:, par], in0=kt3, in1=tb_kdec[:, c0:c0 + GW, h:h + 1].to_broadcast([64, GW, 128]), op=ALU.mult), reads=[pk(bkt), pk(bkt + 1), "tb_kdec"], writes=[("Kdg", par)])
        yield
        bvt = bank(2)
        for a in range(GW):
            cs = slice((c0 + a) * 64, (c0 + a + 1) * 64)
            P.op("pe", lambda e, a=a, cs=cs: e.matmul(psum[0:64, bvt * 512 + a * 128: bvt * 512 + (a + 1) * 128], lhsT=vb[:, cs], rhs=ident_b[:], start=True, stop=True), reads=["vb", "ident_b"], writes=[pk(bvt), pk(bvt + 1)])
        vt3 = psum[0:64, bvt * 512:(bvt + 2) * 512].rearrange("p (a c) -> p a c", c=128)
        P.op("dve", lambda e: e.tensor_tensor(out=Vb[:], in0=vt3, in1=tb_beta[:, c0:c0 + GW, h:h + 1].to_broadcast([64, GW, 128]), op=ALU.mult), reads=[pk(bvt), pk(bvt + 1), "tb_beta"] + al, writes=KW("Vb"))
        yield
        bu = bank(2)
        for a in range(GW):
            P.op("pe", lambda e, a=a: e.matmul(psum[0:64, bu * 512 + a * 128: bu * 512 + (a + 1) * 128], lhsT=Rf[:, a, :], rhs=Vb[:, a, :], start=True, stop=True), reads=K(rfk) + K("Vb"), writes=[pk(bu), pk(bu + 1)])
        P.op("act", lambda e: e.copy(out=Ug[:, par].rearrange("p a c -> p (a c)"), in_=psum[0:64, bu * 512:(bu + 2) * 512]), reads=[pk(bu), pk(bu + 1)], writes=[("Ug", par)])
        yield
        bw = bank()
        for a in range(GW):
            P.op("pe", lambda e, a=a: e.matmul(psum[0:128, bw * 512 + a * 64: bw * 512 + (a + 1) * 64], lhsT=Kbe[:, a, :], rhs=Rf[:, a, :], start=True, stop=True), reads=K(rfk) + K("Kbe"), writes=[pk(bw)])
        P.op("act", lambda e: e.copy(out=WTg[:, par, :], in_=psv(bw)), reads=[pk(bw)], writes=[("WTg", par)])
        yield

    s_par = [0]

    def dn_scan(h, cg, par):
        zs = pc[:, 3, :]
        c0 = cg * GW
        for a in range(GW):
            c = c0 + a
            cs = slice(c * 64, (c + 1) * 64)
            sp_, sn_ = s_par[0], 1 - s_par[0]
            vp = a % 2
            bw = bank()
            P.op("pe", lambda e, a=a, sp_=sp_, bw=bw: e.matmul(psum[0:64, bw * 512: bw * 512 + 128], lhsT=WTg[:, par, a * 64:(a + 1) * 64], rhs=Sb[:, sp_, :], start=True, stop=True),
                 reads=[("WTg", par), ("Sb", sp_)], writes=[pk(bw)])
            P.op("dve", lambda e, a=a, vp=vp, bw=bw: e.tensor_tensor(out=vnew[:, vp, :], in0=Ug[:, par, a, :], in1=psum[0:64, bw * 512: bw * 512 + 128], op=ALU.subtract),
                 reads=[("Ug", par), pk(bw)], writes=[("vnew", vp)])
            yield
            bs = bank()
            P.op("pe", lambda e, a=a, vp=vp, bs=bs: e.matmul(psum[0:128, bs * 512: bs * 512 + 128], lhsT=Kdg[:, par, a, :], rhs=vnew[:, vp, :], start=True, stop=True),
                 reads=[("Kdg", par), ("vnew", vp)], writes=[pk(bs)])
            oq = a % 4
            bo = 6 + ((c // 4) % 2)
            P.op("pe", lambda e, cs=cs, sp_=sp_, bo=bo, oq=oq: e.matmul(psum[0:64, bo * 512 + oq * 128: bo * 512 + (oq + 1) * 128], lhsT=qsb[:, cs], rhs=Sb[:, sp_, :], start=True, stop=False),
                 reads=[("qsb", cg), ("Sb", sp_)], writes=[pk(bo)])
            P.op("pe", lambda e, a=a, vp=vp, bo=bo, oq=oq: e.matmul(psum[0:64, bo * 512 + oq * 128: bo * 512 + (oq + 1) * 128], lhsT=ATg[:, par, a, :], rhs=vnew[:, vp, :], start=False, stop=True),
                 reads=[("ATg", par), ("vnew", vp)], writes=[pk(bo)])
            P.op("dve", lambda e, c=c, sp_=sp_, sn_=sn_, bs=bs: e.scalar_tensor_tensor(out=Sb[:, sn_, :], in0=Sst[:, sp_, :], scalar=tb_egl[:, c, h:h + 1], in1=psum[0:128, bs * 512: bs * 512 + 128], op0=ALU.mult, op1=ALU.add),
                 reads=[("S", sp_), "tb_egl", pk(bs)], writes=[("Sb", sn_)])
            P.op("dve", lambda e, c=c, sp_=sp_, sn_=sn_, bs=bs: e.scalar_tensor_tensor(out=Sst[:, sn_, :], in0=Sst[:, sp_, :], scalar=tb_egl[:, c, h:h + 1], in1=psum[0:128, bs * 512: bs * 512 + 128], op0=ALU.mult, op1=ALU.add),
                 reads=[("S", sp_), "tb_egl", pk(bs)], writes=[("S", sn_)])
            s_par[0] = sn_
            if oq == 3:
                c4 = c - 3
                P.op("act", lambda e, bo=bo: e.copy(out=Og[:].rearrange("p a c -> p (a c)"), in_=psv(bo, 64)), reads=[pk(bo)], writes=["Og"])
                P.op("pool", lambda e: e.tensor_tensor(out=Osq[:], in0=Og[:], in1=Og[:], op=ALU.mult), reads=["Og"], writes=["Osq"])
                P.op("dve", lambda e: e.tensor_reduce(out=oss[:], in_=Osq[:], axis=AX.X, op=ALU.add), reads=["Osq"], writes=["oss"])
                P.op("act", lambda e: e.activation(out=oss[:], in_=oss[:], func=AF.Sqrt, scale=1.0 / 128, bias=EPS), reads=["oss"], writes=["oss"])
                P.op("dve", lambda e: e.reciprocal(out=oss[:], in_=oss[:]), reads=["oss"], writes=["oss"])
                P.op("pool", lambda e: e.tensor_tensor(out=Ogb[:], in0=Og[:], in1=oss[:, :, None].to_broadcast([64, 4, 128]), op=ALU.mult), reads=["Og", "oss"], writes=["Ogb"])
                bt = bank()
                for q4 in range(4):
                    P.op("pe", lambda e, q4=q4, bt=bt: e.matmul(psum[0:128, bt * 512 + q4 * 64: bt * 512 + (q4 + 1) * 64], lhsT=Ogb[:, q4, :], rhs=ident_b[0:64, 0:64], start=True, stop=True), reads=["Ogb", "ident_b"], writes=[pk(bt)])
                P.op("dve", lambda e, c4=c4, bt=bt: e.tensor_tensor(out=yT[:, 4 + h, c4 * 64:(c4 + 4) * 64], in0=psum[0:128, bt * 512: bt * 512 + 256], in1=zs[:, c4 * 64:(c4 + 4) * 64], op=ALU.mult),
                     reads=[pk(bt), ("pc", 3, cg)], writes=[("yT", 4 + h)])
            yield

    for h in range(4):
        bank_lim[0] = 8
        proj_chunk(12 + h, 0)
        proj_chunk(16 + h, 1)
        proj_chunk(20 + h, 2)
        proj_chunk(24 + h, 3)
        for s_, m_ in ((0, h), (1, 4 + h), (2, 8 + h)):
            conv("pool", cvt[:], ["cvt"], pc[:, s_, :], pck(s_), dcw_s, "dcw_s", m_, 4)
            if s_ == 2:
                P.op("act", lambda e: e.activation(out=vb[:], in_=cvt[:], func=AF.Silu), reads=["cvt"], writes=["vb"])
            else:
                P.op("act", lambda e, s_=s_: e.activation(out=pc[:, s_, :], in_=cvt[:], func=AF.Silu), reads=["cvt"], writes=pck(s_))
        inv_rms(pc[:, 0, :], pck(0), ones_b[:], "ones_b", 1.0)
        P.op("dve", lambda e: e.scalar_tensor_tensor(out=pc[:, 0, :], in0=pc[:, 0, :], scalar=128 ** -0.5, in1=cvt[:], op0=ALU.mult, op1=ALU.mult), reads=pck(0) + ["cvt"], writes=pck(0))
        P.op("act", lambda e: e.copy(out=qb[:], in_=pc[:, 0, :]), reads=pck(0), writes=["qb"])
        inv_rms(pc[:, 1, :], pck(1), ones_b[:], "ones_b", 1.0)
        P.op("dve", lambda e: e.tensor_tensor(out=kb[:], in0=pc[:, 1, :], in1=cvt[:], op=ALU.mult), reads=pck(1) + ["cvt"], writes=["kb"])
        P.op("act", lambda e: e.activation(out=pc[:, 3, :], in_=pc[:, 3, :], func=AF.Silu), reads=pck(3), writes=pck(3))
        P.op("dve", lambda e: e.tensor_scalar(out=pc[:, 3, :], in0=pc[:, 3, :], scalar1=gdn_s[:, 0:1], scalar2=None, op0=ALU.mult), reads=pck(3) + ["gdn_s"], writes=pck(3))
        P.op("pool", lambda e: e.memset(Sst[:, 0, :], 0.0), writes=[("S", 0)])
        P.op("pool", lambda e: e.memset(Sb[:, 0, :], 0.0), writes=[("Sb", 0)])
        s_par[0] = 0
        ngr = NCH // GW
        bank_lim[0] = 6
        bank_rr[0] = 0
        P.op("pool", lambda e: e.memset(oss[:], 0.0), reads=[], writes=pck(1) + pck(2) + ["cvt", "alias1", "oss"])
        pending = list(range(ngr))
        free_sets = list(range(NSET))
        running = []
        p1_done = set()
        scan_next = 0
        scan_running = False
        while pending or running or scan_next < ngr:
            while pending and free_sets and pending[0] - NPAR < scan_next:
                cg = pending.pop(0)
                si = free_sets.pop(0)
                running.append(["p1", cg, dn_phase1(h, cg, cg % NPAR, SC[si]), si])
            if not scan_running and scan_next < ngr and scan_next in p1_done:
                running.append(["scan", scan_next, dn_scan(h, scan_next, scan_next % NPAR), None])
                scan_running = True
            for r in list(running):
                try:
                    next(r[2])
                except StopIteration:
                    running.remove(r)
                    if r[0] == "p1":
                        free_sets.append(r[3])
                        p1_done.add(r[1])
                    else:
                        scan_next += 1
                        scan_running = False
        P.op("pool", lambda e: e.memset(oss[:], 0.0), reads=[], writes=pck(1) + pck(2) + ["cvt", "alias1", "oss"])
    dn.close()
    stA.close()
    bank_lim[0] = 8
    P.barrier()

    stB = ExitStack()
    x1 = sb(stB, "x1", [128, NT, D], F32)
    stW = ExitStack()
    wout_s = sb(stW, "wout_s", [128, 8, D], BF16)
    for kc in range(8):
        P.dma("pool", lambda e, kc=kc: e.dma_start(out=wout_s[:, kc, :], in_=w_out_r[:, kc, :]), writes=[("wout", kc)])
    for t in range(NT):
        P.dma("sp", lambda e, t=t: e.dma_start(out=x1[:, t, :], in_=x_tok[t * 128:(t + 1) * 128, :]), writes=[("x1", t)])
    for t in range(NT):
        for dh in range(2):
            b = bank()
            for kc in range(8):
                P.op("pe", lambda e, t=t, dh=dh, kc=kc, b=b: e.matmul(psv(b), lhsT=yT[:, kc, t * 128:(t + 1) * 128], rhs=wout_s[:, kc, dh * 512:(dh + 1) * 512], start=(kc == 0), stop=(kc == 7)),
                     reads=[("yT", kc), ("wout", kc)], writes=[pk(b)])
            P.op("dve", lambda e, t=t, dh=dh, b=b: e.tensor_tensor(out=x1[:, t, dh * 512:(dh + 1) * 512], in0=x1[:, t, dh * 512:(dh + 1) * 512], in1=psv(b), op=ALU.add),
                 reads=[pk(b), ("x1", t)], writes=[("x1", t)])
    stW.close()

    if stage == "A":
        for t in range(NT):
            P.dma("sp", lambda e, t=t: e.dma_start(out=out[t * 128:(t + 1) * 128, :], in_=x1[:, t, :]), reads=[("x1", t)], writes=[("out", t)])
        P.finish([("out", t) for t in range(NT)])
        P.emit()
        return nc


    stackR.close()
    P.barrier()

    BIG = 1.0e30

    g2bc = sb(stB, "g2bc", [128, D], F32)
    g3bc = sb(stB, "g3bc", [128, D], F32)
    wr_s = sb(stB, "wr_s", [128, 8, 36], F32)
    ecap = sb(stB, "ecap", [128, NE], F32)
    ecap_i = sb(stB, "ecap_i", [128, NE], I32)
    ustr_b = sb(stB, "ustr_b", [128, 128], BF16)
    d1i = sb(stB, "d1i", [128, NT], I32)
    d2i = sb(stB, "d2i", [128, NT], I32)
    gt1 = sb(stB, "gt1", [128, NT], F32)
    gt2 = sb(stB, "gt2", [128, NT], F32)
    P.dma("sp", lambda e: e.dma_start(out=g2bc[:], in_=bass.AP(g2_h, 0, [[0, 128], [1, D]])), writes=["g2bc"])
    P.dma("sp", lambda e: e.dma_start(out=g3bc[:], in_=bass.AP(g3_h, 0, [[0, 128], [1, D]])), writes=["g3bc"])
    P.dma("sp", lambda e: e.dma_start(out=wr_s[:], in_=wr_r), writes=["wr_s"])
    P.op("pool", lambda e: e.iota(ecap_i[:], [[CAP, NE]], base=0, channel_multiplier=0), writes=["ecap_i"])
    P.op("dve", lambda e: e.tensor_copy(out=ecap[:], in_=ecap_i[:]), reads=["ecap_i"], writes=["ecap"])
    P.op("pool", lambda e: e.memset(ustr_b[:], 1.0), writes=["ustr_b"])
    P.op("pool", lambda e: e.affine_select(out=ustr_b[:], in_=ustr_b[:], pattern=[[1, 128]], compare_op=ALU.is_gt, fill=0.0, base=0, channel_multiplier=-1), reads=["ustr_b"], writes=["ustr_b"])

    rt = ExitStack()
    h2b = sb(rt, "h2b", [128, NT, D], BF16)
    h2f = sb(rt, "h2f", [128, 3, D], F32)
    h2lo = sb(rt, "h2lo", [128, 3, D], BF16)
    hT2 = sb(rt, "hT2", [128, 3, 2, 8, 128], BF16)
    wr_hi = sb(rt, "wr_hi", [128, 8, 36], BF16)
    wr_lo = sb(rt, "wr_lo", [128, 8, 36], BF16)
    ssq = sb(rt, "ssq", [128, NT], F32)
    Lall = sb(rt, "Lall", [128, NT, 36], F32)
    r_gmax = sb(rt, "r_gmax", [128, NT], F32)
    r_gmask = sb(rt, "r_gmask", [128, NT, 4], F32)
    r_ge = sb(rt, "r_ge", [128, NT, 4], F32)
    r_gp = sb(rt, "r_gp", [128, NT], F32)
    r_pen = sb(rt, "r_pen", [128, NT, 4], F32)
    r_el = sb(rt, "r_el", [128, NT, 32], F32)
    r_el2 = sb(rt, "r_el2", [128, NT, 32], F32)
    r_m1 = sb(rt, "r_m1", [128, NT], F32)
    r_m2 = sb(rt, "r_m2", [128, NT], F32)
    r_mask1 = sb(rt, "r_mask1", [128, NT, 32], F32)
    r_mask2 = sb(rt, "r_mask2", [128, NT, 32], F32)
    r_m12b = sb(rt, "r_m12b", [128, NT, 32], BF16)
    r_e21 = sb(rt, "r_e21", [128, NT], F32)
    r_rank = sb(rt, "r_rank", [128, NT, 32], F32)
    r_valid = sb(rt, "r_valid", [128, NT, 32], F32)
    r_slot = sb(rt, "r_slot", [128, NT, 32], F32)
    r_tmp = sb(rt, "r_tmp", [128, NT, 32], F32)
    r_d1f = sb(rt, "r_d1f", [128, NT], F32)
    r_d2f = sb(rt, "r_d2f", [128, NT], F32)
    r_v1 = sb(rt, "r_v1", [128, NT], F32)
    r_v2 = sb(rt, "r_v2", [128, NT], F32)

    ssq_keys = [("ssq", t) for t in range(NT)]
    lall_keys = [("Lall", t) for t in range(NT)]
    P.op("pool", lambda e: e.memset(ssq[:], 0.0), writes=ssq_keys)
    P.op("act", lambda e: e.copy(out=wr_hi[:], in_=wr_s[:]), reads=["wr_s"], writes=["wr_hi"])
    P.op("dve", lambda e: e.tensor_tensor(out=wr_lo[:], in0=wr_s[:], in1=wr_hi[:], op=ALU.subtract), reads=["wr_s", "wr_hi"], writes=["wr_lo"])

    def r1_tile(t):
        sl = t % 3
        P.op("act", lambda e: e.activation(out=h2f[:, sl, :], in_=x1[:, t, :], func=AF.Square, accum_out=ssq[:, t:t + 1]), reads=[("x1", t), ("ssq", t)], writes=[("h2f", sl), ("ssq", t)])
        P.op("act", lambda e: e.activation(out=ssq[:, t:t + 1], in_=ssq[:, t:t + 1], func=AF.Sqrt, scale=1.0 / D, bias=EPS), reads=[("ssq", t)], writes=[("ssq", t)])
        P.op("dve", lambda e: e.reciprocal(out=ssq[:, t:t + 1], in_=ssq[:, t:t + 1]), reads=[("ssq", t)], writes=[("ssq", t)])
        P.op("dve", lambda e: e.scalar_tensor_tensor(out=h2f[:, sl, :], in0=x1[:, t, :], scalar=ssq[:, t:t + 1], in1=g2bc[:], op0=ALU.mult, op1=ALU.mult),
             reads=[("x1", t), ("ssq", t), "g2bc"], writes=[("h2f", sl)])
        P.op("act", lambda e: e.copy(out=h2b[:, t, :], in_=h2f[:, sl, :]), reads=[("h2f", sl)], writes=[("h2b", t)])
        P.op("pool", lambda e: e.tensor_tensor(out=h2lo[:, sl, :], in0=h2f[:, sl, :], in1=h2b[:, t, :], op=ALU.subtract), reads=[("h2f", sl), ("h2b", t)], writes=[("h2lo", sl)])
        yield
        b0 = bank(2)
        for kc in range(8):
            P.op("pe", lambda e, kc=kc: e.matmul(psum[0:128, b0 * 512 + kc * 128: b0 * 512 + (kc + 1) * 128], lhsT=h2b[:, t, kc * 128:(kc + 1) * 128], rhs=ident_b[:], start=True, stop=True),
                 reads=[("h2b", t), "ident_b"], writes=[pk(b0), pk(b0 + 1)])
        P.op("dve", lambda e: e.tensor_copy(out=hT2[:, sl, 0].rearrange("p k c -> p (k c)"), in_=psum[0:128, b0 * 512:(b0 + 2) * 512]), reads=[pk(b0), pk(b0 + 1)], writes=[("hT2", sl, 0)])
        b2 = bank(2)
        for kc in range(8):
            P.op("pe", lambda e, kc=kc: e.matmul(psum[0:128, b2 * 512 + kc * 128: b2 * 512 + (kc + 1) * 128], lhsT=h2lo[:, sl, kc * 128:(kc + 1) * 128], rhs=ident_b[:], start=True, stop=True),
                 reads=[("h2lo", sl), "ident_b"], writes=[pk(b2), pk(b2 + 1)])
        P.op("dve", lambda e: e.tensor_copy(out=hT2[:, sl, 1].rearrange("p k c -> p (k c)"), in_=psum[0:128, b2 * 512:(b2 + 2) * 512]), reads=[pk(b2), pk(b2 + 1)], writes=[("hT2", sl, 1)])
        yield
        bl = bank()
        n_ = 0
        for kc in range(8):
            for hl, wt, wk in ((0, wr_hi, "wr_hi"), (1, wr_hi, "wr_hi"), (0, wr_lo, "wr_lo")):
                P.op("pe", lambda e, kc=kc, hl=hl, wt=wt, n_=n_: e.matmul(psum[0:128, bl * 512: bl * 512 + 36], lhsT=hT2[:, sl, hl, kc, :], rhs=wt[:, kc, :], start=(n_ == 0), stop=(n_ == 23)),
                     reads=[("hT2", sl, hl), wk], writes=[pk(bl)])
                n_ += 1
        P.op("act", lambda e: e.copy(out=Lall[:, t, :], in_=psum[0:128, bl * 512: bl * 512 + 36]), reads=[pk(bl)], writes=[("Lall", t)])
        yield

    gens_ = [r1_tile(t) for t in range(NT)]
    active_ = []
    nxt_ = 0
    while active_ or nxt_ < NT:
        if nxt_ < NT and len(active_) < 3:
            active_.append(gens_[nxt_])
            nxt_ += 1
        for g_ in list(active_):
            try:
                next(g_)
            except StopIteration:
                active_.remove(g_)

    GLv = Lall[:, :, 0:4]
    ELv = Lall[:, :, 4:36]

    def bc(ap, shape):
        return ap.to_broadcast(shape)

    P.op("dve", lambda e: e.tensor_reduce(out=r_gmax[:], in_=GLv, axis=AX.X, op=ALU.max), reads=lall_keys, writes=["r_gmax"])
    P.op("dve", lambda e: e.tensor_tensor(out=r_gmask[:], in0=GLv, in1=bc(r_gmax[:, :, None], [128, NT, 4]), op=ALU.is_equal), reads=lall_keys + ["r_gmax"], writes=["r_gmask"])
    P.op("dve", lambda e: e.tensor_tensor(out=r_ge[:], in0=GLv, in1=bc(r_gmax[:, :, None], [128, NT, 4]), op=ALU.subtract), reads=lall_keys + ["r_gmax"], writes=["r_ge"])
    P.op("act", lambda e: e.activation(out=r_ge[:], in_=r_ge[:], func=AF.Exp), reads=["r_ge"], writes=["r_ge"])
    P.op("dve", lambda e: e.tensor_reduce(out=r_gp[:], in_=r_ge[:], axis=AX.X, op=ALU.add), reads=["r_ge"], writes=["r_gp"])
    P.op("dve", lambda e: e.reciprocal(out=r_gp[:], in_=r_gp[:]), reads=["r_gp"], writes=["r_gp"])
    P.op("dve", lambda e: e.tensor_scalar(out=r_pen[:], in0=r_gmask[:], scalar1=BIG, scalar2=-BIG, op0=ALU.mult, op1=ALU.add), reads=["r_gmask"], writes=["r_pen"])
    P.op("dve", lambda e: e.tensor_tensor(out=r_el[:].rearrange("p t (g k) -> p t g k", k=8), in0=ELv.rearrange("p t (g k) -> p t g k", k=8), in1=bc(r_pen[:, :, :, None], [128, NT, 4, 8]), op=ALU.add),
         reads=lall_keys + ["r_pen"], writes=["r_el"])
    P.op("dve", lambda e: e.tensor_reduce(out=r_m1[:], in_=r_el[:], axis=AX.X, op=ALU.max), reads=["r_el"], writes=["r_m1"])
    P.op("dve", lambda e: e.tensor_tensor(out=r_mask1[:], in0=r_el[:], in1=bc(r_m1[:, :, None], [128, NT, 32]), op=ALU.is_equal), reads=["r_el", "r_m1"], writes=["r_mask1"])
    P.op("dve", lambda e: e.scalar_tensor_tensor(out=r_el2[:], in0=r_mask1[:], scalar=-BIG, in1=r_el[:], op0=ALU.mult, op1=ALU.add), reads=["r_mask1", "r_el"], writes=["r_el2"])
    P.op("dve", lambda e: e.tensor_reduce(out=r_m2[:], in_=r_el2[:], axis=AX.X, op=ALU.max), reads=["r_el2"], writes=["r_m2"])
    P.op("dve", lambda e: e.tensor_tensor(out=r_mask2[:], in0=r_el2[:], in1=bc(r_m2[:, :, None], [128, NT, 32]), op=ALU.is_equal), reads=["r_el2", "r_m2"], writes=["r_mask2"])
    P.op("dve", lambda e: e.tensor_tensor(out=r_m12b[:], in0=r_mask1[:], in1=r_mask2[:], op=ALU.add), reads=["r_mask1", "r_mask2"], writes=["r_m12b"])
    P.op("dve", lambda e: e.tensor_tensor(out=r_e21[:], in0=r_m2[:], in1=r_m1[:], op=ALU.subtract), reads=["r_m1", "r_m2"], writes=["r_e21"])
    P.op("act", lambda e: e.activation(out=r_e21[:], in_=r_e21[:], func=AF.Exp), reads=["r_e21"], writes=["r_e21"])
    P.op("dve", lambda e: e.tensor_scalar(out=gt1[:], in0=r_e21[:], scalar1=1.0, scalar2=None, op0=ALU.add), reads=["r_e21"], writes=["gt1"])
    P.op("dve", lambda e: e.reciprocal(out=gt1[:], in_=gt1[:]), reads=["gt1"], writes=["gt1"])
    P.op("dve", lambda e: e.tensor_tensor(out=gt1[:], in0=gt1[:], in1=r_gp[:], op=ALU.mult), reads=["gt1", "r_gp"], writes=["gt1"])
    P.op("dve", lambda e: e.tensor_tensor(out=gt2[:], in0=gt1[:], in1=r_e21[:], op=ALU.mult), reads=["gt1", "r_e21"], writes=["gt2"])

    br = bank()
    for t in range(NT):
        P.op("pe", lambda e, t=t: e.matmul(psum[0:128, br * 512 + t * 32: br * 512 + (t + 1) * 32], lhsT=ustr_b[:], rhs=r_m12b[:, t, :], start=True, stop=(t == 0)),
             reads=["ustr_b", "r_m12b"], writes=[pk(br)])
        for t2 in range(t):
            P.op("pe", lambda e, t=t, t2=t2: e.matmul(psum[0:128, br * 512 + t * 32: br * 512 + (t + 1) * 32], lhsT=ones_b[:], rhs=r_m12b[:, t2, :], start=False, stop=(t2 == t - 1)),
                 reads=["ones_b", "r_m12b"], writes=[pk(br)])
    P.op("act", lambda e: e.copy(out=r_rank[:].rearrange("p t k -> p (t k)"), in_=psv(br)), reads=[pk(br)], writes=["r_rank"])
    P.op("dve", lambda e: e.tensor_scalar(out=r_valid[:], in0=r_rank[:], scalar1=float(CAP), scalar2=None, op0=ALU.is_lt), reads=["r_rank"], writes=["r_valid"])
    P.op("dve", lambda e: e.tensor_tensor(out=r_slot[:], in0=r_rank[:], in1=bc(ecap[:, None, :], [128, NT, 32]), op=ALU.add), reads=["r_rank", "ecap"], writes=["r_slot"])
    P.op("dve", lambda e: e.tensor_scalar(out=r_slot[:], in0=r_slot[:], scalar1=-float(DUMP), scalar2=None, op0=ALU.add), reads=["r_slot"], writes=["r_slot"])
    P.op("dve", lambda e: e.tensor_tensor(out=r_slot[:], in0=r_slot[:], in1=r_valid[:], op=ALU.mult), reads=["r_slot", "r_valid"], writes=["r_slot"])
    P.op("dve", lambda e: e.tensor_scalar(out=r_slot[:], in0=r_slot[:], scalar1=float(DUMP), scalar2=None, op0=ALU.add), reads=["r_slot"], writes=["r_slot"])
    for msk, mk, df, dk, vv, vk, di, dik, gt, gk in ((r_mask1, "r_mask1", r_d1f, "r_d1f", r_v1, "r_v1", d1i, "d1i", gt1, "gt1"),
                                                     (r_mask2, "r_mask2", r_d2f, "r_d2f", r_v2, "r_v2", d2i, "d2i", gt2, "gt2")):
        P.op("dve", lambda e, msk=msk: e.tensor_tensor(out=r_tmp[:], in0=msk[:], in1=r_slot[:], op=ALU.mult), reads=[mk, "r_slot"], writes=["r_tmp"])
        P.op("dve", lambda e, df=df: e.tensor_reduce(out=df[:], in_=r_tmp[:], axis=AX.X, op=ALU.add), reads=["r_tmp"], writes=[dk])
        P.op("dve", lambda e, df=df, di=di: e.tensor_copy(out=di[:], in_=df[:]), reads=[dk], writes=[dik])
        P.op("dve", lambda e, msk=msk: e.tensor_tensor(out=r_tmp[:], in0=msk[:], in1=r_valid[:], op=ALU.mult), reads=[mk, "r_valid"], writes=["r_tmp"])
        P.op("dve", lambda e, vv=vv: e.tensor_reduce(out=vv[:], in_=r_tmp[:], axis=AX.X, op=ALU.add), reads=["r_tmp"], writes=[vk])
        P.op("dve", lambda e, vv=vv, gt=gt: e.tensor_tensor(out=gt[:], in0=gt[:], in1=vv[:], op=ALU.mult), reads=[gk, vk], writes=[gk])
    xs_keys = []
    for t in range(NT):
        for di, dik, w in ((d1i, "d1i", 0), (d2i, "d2i", 1)):
            key = ("Xs", t, w)
            xs_keys.append(key)
            P.dma("pool", lambda e, t=t, di=di: e.indirect_dma_start(out=Xs, out_offset=bass.IndirectOffsetOnAxis(ap=di[:, t:t + 1], axis=0), in_=h2b[:, t, :], in_offset=None, bounds_check=NE * CAP + 127, oob_is_err=False),
                  reads=[("h2b", t), dik] + xz_keys, writes=[key])
    if stage == "dbg":
        dbg_i = nc.dram_tensor("dbg_i", [128, 2 * NT], I32, kind="ExternalOutput").ap()
        dbg_g = nc.dram_tensor("dbg_g", [128, 2 * NT], F32, kind="ExternalOutput").ap()
        dbg_r = nc.dram_tensor("dbg_r", [128, NT * 32], F32, kind="ExternalOutput").ap()
        dbg_l = nc.dram_tensor("dbg_l", [128, NT * 36], F32, kind="ExternalOutput").ap()
        P.dma("sp", lambda e: e.dma_start(out=dbg_i[:, 0:NT], in_=d1i[:]), reads=["d1i"], writes=["dbg1"])
        P.dma("sp", lambda e: e.dma_start(out=dbg_i[:, NT:2 * NT], in_=d2i[:]), reads=["d2i"], writes=["dbg2"])
        P.dma("sp", lambda e: e.dma_start(out=dbg_g[:, 0:NT], in_=gt1[:]), reads=["gt1"], writes=["dbg3"])
        P.dma("sp", lambda e: e.dma_start(out=dbg_g[:, NT:2 * NT], in_=gt2[:]), reads=["gt2"], writes=["dbg4"])
        P.dma("sp", lambda e: e.dma_start(out=dbg_r, in_=r_rank[:].rearrange("p t k -> p (t k)")), reads=["r_rank"], writes=["dbg5"])
        P.dma("sp", lambda e: e.dma_start(out=dbg_l, in_=Lall[:].rearrange("p t k -> p (t k)")), reads=lall_keys, writes=["dbg6"])
        P.finish(["dbg1", "dbg2", "dbg3", "dbg4", "dbg5", "dbg6"])
    rt.close()
    P.barrier()

    ex = ExitStack()
    wg_s = sb(ex, "wg_s", [128, 3, 8, FF], BF16)
    wu_s = sb(ex, "wu_s", [128, 3, 8, FF], BF16)
    wd_s = sb(ex, "wd_s", [128, 3, 4, D], BF16)
    xb = sb(ex, "xb", [128, 2, 2, D], BF16)
    xbT = sb(ex, "xbT", [128, 2, 8, CAP], BF16)
    sil = sb(ex, "sil", [128, 4, CAP], F32)
    hmid = sb(ex, "hmid", [128, 2, 4, CAP], BF16)
    Yo = sb(ex, "Yo", [128, 2, D], F32)
    fss = sb(ex, "fss", [128, NT], F32)

    def load_w(e_):
        par = e_ % 3
        P.dma("pool", lambda e: e.dma_start(out=wg_s[:, par], in_=w_gate[e_].rearrange("(kc p) f -> p kc f", p=128)), writes=[("wg", par)])
        P.dma("pool", lambda e: e.dma_start(out=wu_s[:, par], in_=w_up[e_].rearrange("(kc p) f -> p kc f", p=128)), writes=[("wu", par)])
        P.dma("pool", lambda e: e.dma_start(out=wd_s[:, par], in_=w_down[e_].rearrange("(kc p) f -> p kc f", p=128)), writes=[("wd", par)])

    def load_xb(e_):
        par = e_ % 2
        P.dma("sp", lambda e: e.dma_start(out=xb[:, par], in_=Xs[e_ * CAP:(e_ + 1) * CAP, :].rearrange("(b p) d -> p b d", p=128)), reads=xs_keys, writes=[("xb", par)])

    ys_keys = []
    load_xb(0)
    load_w(0)
    load_w(1)
    for e_ in range(NE):
        par = e_ % 2
        wp = e_ % 3
        if e_ + 2 < NE:
            load_w(e_ + 2)
        if e_ + 1 < NE:
            load_xb(e_ + 1)
        for blk in range(2):
            for half in range(2):
                bt = bank()
                for q in range(4):
                    kc = half * 4 + q
                    P.op("pe", lambda e, blk=blk, kc=kc, q=q, bt=bt, par=par: e.matmul(psum[0:128, bt * 512 + q * 128: bt * 512 + (q + 1) * 128], lhsT=xb[:, par, blk, kc * 128:(kc + 1) * 128], rhs=ident_b[:], start=True, stop=True),
                         reads=[("xb", par), "ident_b"], writes=[pk(bt)])
                eng = "act" if half == 0 else "dve"
                if eng == "act":
                    P.op("act", lambda e, blk=blk, half=half, bt=bt, par=par: e.copy(out=xbT[:, par, half * 4:half * 4 + 4, blk * 128:(blk + 1) * 128], in_=psv(bt).rearrange("p (q c) -> p q c", c=128)),
                         reads=[pk(bt)], writes=[("xbT", par)])
                else:
                    P.op("dve", lambda e, blk=blk, half=half, bt=bt, par=par: e.tensor_copy(out=xbT[:, par, half * 4:half * 4 + 4, blk * 128:(blk + 1) * 128], in_=psv(bt).rearrange("p (q c) -> p q c", c=128)),
                         reads=[pk(bt)], writes=[("xbT", par)])
        ba_ = bank(2)
        for fc in range(4):
            for kc in range(8):
                P.op("pe", lambda e, fc=fc, kc=kc, par=par, wp=wp, ba_=ba_: e.matmul(psum[0:128, ba_ * 512 + fc * CAP: ba_ * 512 + (fc + 1) * CAP], lhsT=wg_s[:, wp, kc, fc * 128:(fc + 1) * 128], rhs=xbT[:, par, kc, :], start=(kc == 0), stop=(kc == 7)),
                     reads=[("wg", wp), ("xbT", par)], writes=[pk(ba_), pk(ba_ + 1)])
        bb_ = bank(2)
        for fc in range(4):
            for kc in range(8):
                P.op("pe", lambda e, fc=fc, kc=kc, par=par, wp=wp, bb_=bb_: e.matmul(psum[0:128, bb_ * 512 + fc * CAP: bb_ * 512 + (fc + 1) * CAP], lhsT=wu_s[:, wp, kc, fc * 128:(fc + 1) * 128], rhs=xbT[:, par, kc, :], start=(kc == 0), stop=(kc == 7)),
                     reads=[("wu", wp), ("xbT", par)], writes=[pk(bb_), pk(bb_ + 1)])
        P.op("act", lambda e, ba_=ba_: e.activation(out=sil[:].rearrange("p f c -> p (f c)"), in_=psum[0:128, ba_ * 512:(ba_ + 2) * 512], func=AF.Silu), reads=[pk(ba_), pk(ba_ + 1)], writes=["sil"])
        P.op("dve", lambda e, bb_=bb_, par=par: e.tensor_tensor(out=hmid[:, par].rearrange("p f c -> p (f c)"), in0=sil[:].rearrange("p f c -> p (f c)"), in1=psum[0:128, bb_ * 512:(bb_ + 2) * 512], op=ALU.mult),
             reads=["sil", pk(bb_), pk(bb_ + 1)], writes=[("hmid", par)])
        for blk in range(2):
            for dh in range(2):
                bd = bank()
                for fc in range(4):
                    P.op("pe", lambda e, blk=blk, dh=dh, fc=fc, bd=bd, par=par, wp=wp: e.matmul(psv(bd), lhsT=hmid[:, par, fc, blk * 128:(blk + 1) * 128], rhs=wd_s[:, wp, fc, dh * 512:(dh + 1) * 512], start=(fc == 0), stop=(fc == 3)),
                         reads=[("hmid", par), ("wd", wp)], writes=[pk(bd)])
                if dh == 0:
                    P.op("act", lambda e, blk=blk, dh=dh, bd=bd: e.copy(out=Yo[:, blk, dh * 512:(dh + 1) * 512], in_=psv(bd)), reads=[pk(bd)], writes=[("Yo", blk, dh)])
                else:
                    P.op("dve", lambda e, blk=blk, dh=dh, bd=bd: e.tensor_copy(out=Yo[:, blk, dh * 512:(dh + 1) * 512], in_=psv(bd)), reads=[pk(bd)], writes=[("Yo", blk, dh)])
        key = ("Ys", e_)
        ys_keys.append(key)
        P.dma("sp", lambda e, e_=e_: e.dma_start(out=Ys[e_ * CAP:(e_ + 1) * CAP, :].rearrange("(b p) d -> p b d", p=128), in_=Yo[:]), reads=[("Yo", b_, d_) for b_ in range(2) for d_ in range(2)], writes=[key])

    G1 = sb(ex, "G1", [128, 2, D], F32)
    G2 = sb(ex, "G2", [128, 2, D], F32)
    fjunk = sb(ex, "fjunk", [128, D], F32)
    P.op("pool", lambda e: e.memset(fss[:], 0.0), writes=[("fss", t) for t in range(NT)])
    def comb_tile(t):
        sl = t % 2
        P.dma("pool", lambda e: e.indirect_dma_start(out=G1[:, sl, :], out_offset=None, in_=Ys, in_offset=bass.IndirectOffsetOnAxis(ap=d1i[:, t:t + 1], axis=0)), reads=ys_keys + ["d1i", ("Yz", 0)], writes=[("G1", sl)])
        P.dma("pool", lambda e: e.indirect_dma_start(out=G2[:, sl, :], out_offset=None, in_=Ys, in_offset=bass.IndirectOffsetOnAxis(ap=d2i[:, t:t + 1], axis=0)), reads=ys_keys + ["d2i", ("Yz", 0)], writes=[("G2", sl)])
        yield
        P.op("dve", lambda e: e.scalar_tensor_tensor(out=x1[:, t, :], in0=G1[:, sl, :], scalar=gt1[:, t:t + 1], in1=x1[:, t, :], op0=ALU.mult, op1=ALU.add), reads=[("G1", sl), "gt1", ("x1", t)], writes=[("x1", t)])
        P.op("dve", lambda e: e.scalar_tensor_tensor(out=x1[:, t, :], in0=G2[:, sl, :], scalar=gt2[:, t:t + 1], in1=x1[:, t, :], op0=ALU.mult, op1=ALU.add), reads=[("G2", sl), "gt2", ("x1", t)], writes=[("x1", t)])
        P.op("act", lambda e: e.activation(out=fjunk[:], in_=x1[:, t, :], func=AF.Square, accum_out=fss[:, t:t + 1]), reads=[("x1", t), ("fss", t)], writes=["fjunk", ("fss", t)])
        P.op("act", lambda e: e.activation(out=fss[:, t:t + 1], in_=fss[:, t:t + 1], func=AF.Sqrt, scale=1.0 / D, bias=EPS), reads=[("fss", t)], writes=[("fss", t)])
        yield
        P.op("dve", lambda e: e.reciprocal(out=fss[:, t:t + 1], in_=fss[:, t:t + 1]), reads=[("fss", t)], writes=[("fss", t)])
        P.op("dve", lambda e: e.scalar_tensor_tensor(out=x1[:, t, :], in0=x1[:, t, :], scalar=fss[:, t:t + 1], in1=g3bc[:], op0=ALU.mult, op1=ALU.mult), reads=[("x1", t), ("fss", t), "g3bc"], writes=[("x1", t)])
        P.dma("sp", lambda e: e.dma_start(out=out[t * 128:(t + 1) * 128, :], in_=x1[:, t, :]), reads=[("x1", t)], writes=[("out", t)])
        yield

    gens_ = [comb_tile(t) for t in range(NT)]
    active_ = []
    nxt_ = 0
    while active_ or nxt_ < NT:
        if nxt_ < NT and len(active_) < 2:
            active_.append(gens_[nxt_])
            nxt_ += 1
        for g_ in list(active_):
            try:
                next(g_)
            except StopIteration:
                active_.remove(g_)
    P.finish([("out", t) for t in range(NT)])
    P.emit()
    return nc


def make_inputs(inp, b):
    x = np.ascontiguousarray(inp["x"][b])
    w_in = inp["w_in"][0]
    m = {
        "x_tok": x,
        "xT": np.ascontiguousarray(x.T),
        "w_in_r": np.ascontiguousarray(w_in[:, :3584].reshape(8, 128, 28, 128).transpose(2, 1, 0, 3)),
        "w_ba": np.ascontiguousarray(w_in[:, 3584:3592].reshape(8, 128, 8).transpose(1, 0, 2)),
        "g1": np.ascontiguousarray(inp["mix_norm_g"][0].reshape(8, 128).T),
        "caw": np.ascontiguousarray(inp["conv_a_w"][0].reshape(3, 4, 128).transpose(2, 1, 0)),
        "gA": np.ascontiguousarray(inp["conv_a_norm_g"][0].reshape(4, 128).T),
        "dcw": np.ascontiguousarray(inp["dn_conv_w"][0].reshape(4, 12, 128).transpose(2, 1, 0)),
        "alog": np.ascontiguousarray(inp["dn_a_log"][0]),
        "dtb": np.ascontiguousarray(inp["dn_dt_bias"][0]),
        "gdn": np.ascontiguousarray(inp["dn_norm_g"][0].reshape(128, 1)),
        "w_out_r": np.ascontiguousarray(inp["w_out"][0].reshape(8, 128, D).transpose(1, 0, 2)),
        "g2": np.ascontiguousarray(inp["ffn_norm_g"][0]),
        "wr_r": np.ascontiguousarray(np.concatenate([inp["router_group_w"][0], inp["router_expert_w"][0]], axis=1).reshape(8, 128, 36).transpose(1, 0, 2)),
        "w_gate": np.ascontiguousarray(inp["w_gate"][0]),
        "w_up": np.ascontiguousarray(inp["w_up"][0]),
        "w_down": np.ascontiguousarray(inp["w_down"][0]),
        "g3": np.ascontiguousarray(inp["final_norm_g"]),
    }
    return m


def kernel(**inputs):
    inp = {k: np.asarray(v) for k, v in inputs.items()}
    nc = build("full")
    shared = make_inputs(inp, 0)
    in_maps = []
    for b in range(8):
        m = dict(shared)
        xb = np.ascontiguousarray(inp["x"][b])
        m["x_tok"] = xb
        m["xT"] = np.ascontiguousarray(xb.T)
        in_maps.append(m)
    res = run_bass_kernel_spmd(nc, in_maps, core_ids=list(range(8)))
    return np.stack([r["out"] for r in res.results], axis=0).astype(np.float32)
```

```python
from contextlib import ExitStack
import numpy as np
import concourse.bass as bass
import concourse.mybir as mybir
from concourse.bass_utils import run_bass_kernel_spmd

F32 = mybir.dt.float32
BF16 = mybir.dt.bfloat16
I32 = mybir.dt.int32
ALU = mybir.AluOpType
AF = mybir.ActivationFunctionType
AX = mybir.AxisListType

S = 2048
D = 1024
NT = 16
NCH = 32
NE = 32
FF = 512
CAP = 256
EPS = 1e-6
DN_DT = F32


class _Eng:
    def __init__(self, eng, sem, is_pe=False):
        self.eng = eng
        self.sem = sem
        self.count = 0
        self.waited = {}
        self.is_pe = is_pe
        self.rec = []


class Prog:
    def __init__(self, nc, n_dma_sems=10):
        self.nc = nc
        self.E = {
            "pe": _Eng(nc.tensor, nc.alloc_semaphore("s_pe"), True),
            "act": _Eng(nc.scalar, nc.alloc_semaphore("s_act")),
            "dve": _Eng(nc.vector, nc.alloc_semaphore("s_dve")),
            "pool": _Eng(nc.gpsimd, nc.alloc_semaphore("s_pool")),
            "sp": _Eng(nc.sync, nc.alloc_semaphore("s_sp")),
        }
        self.dma_sems = {}
        for q in ("sp", "pool", "act"):
            self.dma_sems[q] = [[nc.alloc_semaphore(f"d_{q}{i}"), 0] for i in range(n_dma_sems)]
        self.dma_rr = {"sp": 0, "pool": 0, "act": 0}
        self.rw = {}

    def _deps(self, reads, writes):
        deps = {}

        def add(tok):
            if tok is None:
                return
            s, v = tok
            if deps.get(s.num, (None, -1))[1] < v:
                deps[s.num] = (s, v)

        for k in reads:
            st = self.rw.get(k)
            if st is not None:
                add(st["w"])
        for k in writes:
            st = self.rw.get(k)
            if st is not None:
                add(st["w"])
                for t in st["r"].values():
                    add(t)
        return deps

    def _commit(self, tok, reads, writes):
        for k in writes:
            self.rw[k] = {"w": tok, "r": {}}
        for k in reads:
            st = self.rw.setdefault(k, {"w": None, "r": {}})
            s, v = tok
            if st["r"].get(s.num, (None, -1))[1] < v:
                st["r"][s.num] = tok

    def _wait(self, e, deps, skip_own=False):
        for num, (s, v) in deps.items():
            if skip_own and num == e.sem.num:
                continue
            if e.waited.get(num, 0) < v:
                e.rec.append(("w", s, v))
                e.waited[num] = v

    def op(self, en, fn, reads=(), writes=()):
        e = self.E[en]
        deps = self._deps(reads, writes)
        self._wait(e, deps, skip_own=e.is_pe)
        e.count += 1
        e.rec.append(("i", fn, e.sem, 1))
        tok = (e.sem, e.count)
        self._commit(tok, reads, writes)
        return tok

    def dma(self, q, fn, reads=(), writes=()):
        e = self.E[q]
        deps = self._deps(reads, writes)
        self._wait(e, deps)
        pool = self.dma_sems[q]
        i = self.dma_rr[q]
        self.dma_rr[q] = (i + 1) % len(pool)
        slot = pool[i]
        s, v = slot
        if v > 0 and e.waited.get(s.num, 0) < v:
            e.rec.append(("w", s, v))
            e.waited[s.num] = v
        e.rec.append(("i", fn, s, 16))
        slot[1] = v + 16
        tok = (s, v + 16)
        self._commit(tok, reads, writes)
        return tok

    def barrier(self):
        toks = []
        for e in self.E.values():
            if e.count > 0:
                toks.append((e.sem, e.count))
        for q in self.dma_sems.values():
            for s, v in q:
                if v > 0:
                    toks.append((s, v))
        for e in self.E.values():
            for s, v in toks:
                if s.num == e.sem.num:
                    continue
                if e.waited.get(s.num, 0) < v:
                    e.rec.append(("w", s, v))
                    e.waited[s.num] = v

    def finish(self, keys):
        e = self.E["sp"]
        self._wait(e, self._deps(keys, ()))

    def emit(self):
        nc = self.nc

        def replay(e):
            def f(eng):
                for r in e.rec:
                    if r[0] == "w":
                        eng.wait_ge(r[1], r[2])
                    else:
                        r[1](eng).then_inc(r[2], r[3])
            return f

        with nc.Block() as block:
            block.sync(replay(self.E["sp"]))
            block.scalar(replay(self.E["act"]))
            block.vector(replay(self.E["dve"]))
            block.gpsimd(replay(self.E["pool"]))
            block.tensor(replay(self.E["pe"]))


def interleave(gens):
    gens = list(gens)
    while gens:
        for g in list(gens):
            try:
                next(g)
            except StopIteration:
                gens.remove(g)


def build(stage="full"):
    nc = bass.Bass("TRN2", target_bir_lowering=False)
    P = Prog(nc)

    def din(name, shape, dt=F32):
        return nc.dram_tensor(name, list(shape), dt, kind="ExternalInput")

    x_tok = din("x_tok", [S, D]).ap()
    xT = din("xT", [D, S]).ap()
    w_in_r = din("w_in_r", [28, 128, 8, 128]).ap()
    w_ba = din("w_ba", [128, 8, 8]).ap()
    g1 = din("g1", [128, 8]).ap()
    caw = din("caw", [128, 4, 3]).ap()
    gA = din("gA", [128, 4]).ap()
    dcw = din("dcw", [128, 12, 4]).ap()
    alog_h = din("alog", [4])
    dtb_h = din("dtb", [4])
    gdn = din("gdn", [128, 1]).ap()
    w_out_r = din("w_out_r", [128, 8, D]).ap()
    g2_h = din("g2", [D])
    wr_r = din("wr_r", [128, 8, 36]).ap()
    w_gate = din("w_gate", [NE, D, FF]).ap()
    w_up = din("w_up", [NE, D, FF]).ap()
    w_down = din("w_down", [NE, FF, D]).ap()
    g3_h = din("g3", [D])
    out = nc.dram_tensor("out", [S, D], F32, kind="ExternalOutput").ap()

    DUMP = NE * CAP
    Xs = nc.dram_tensor("Xs_scr", [NE * CAP + 128, D], BF16, kind="Internal").ap()
    Ys = nc.dram_tensor("Ys_scr", [NE * CAP + 128, D], BF16, kind="Internal").ap()
    Wg_b = nc.dram_tensor("Wg_b", [NE, D, FF], BF16, kind="Internal").ap()
    Wu_b = nc.dram_tensor("Wu_b", [NE, D, FF], BF16, kind="Internal").ap()
    Wd_b = nc.dram_tensor("Wd_b", [NE, FF, D], BF16, kind="Internal").ap()
    stack0 = ExitStack()

    def sb(st, name, shape, dt, side=None):
        return st.enter_context(nc.sbuf_tensor(name, list(shape), dt, side=side))

    psum = nc.alloc_psum_tensor("psum", [128, 8 * 512], F32)
    bank_rr = [0]
    bank_lim = [8]

    def bank(n=1):
        b = bank_rr[0]
        if b + n > bank_lim[0]:
            b = 0
        bank_rr[0] = (b + n) % bank_lim[0]
        return b

    def psv(b, parts=128, n=512, nb=1):
        return psum[0:parts, b * 512:b * 512 + n] if nb == 1 else psum[0:parts, b * 512:(b + nb) * 512]

    def pk(b):
        return ("ps", b)

    ident_f = sb(stack0, "ident_f", [128, 128], F32)
    ident_b = sb(stack0, "ident_b", [128, 128], BF16)
    ones_f = sb(stack0, "ones_f", [128, 128], F32)
    ones_b = sb(stack0, "ones_b", [128, 128], BF16)
    bd64_b = sb(stack0, "bd64_b", [128, 128], BF16)
    bd64_f = sb(stack0, "bd64_f", [128, 128], F32)
    u64 = sb(stack0, "u64", [64, 64], F32)
    maskc8 = sb(stack0, "maskc8", [64, 8, 64], F32)
    masks8 = sb(stack0, "masks8", [64, 8, 64], F32)
    eye8 = sb(stack0, "eye8", [64, 8, 64], F32)

    P.op("pool", lambda e: e.memset(ident_f[:], 1.0), writes=["ident_f"])
    P.op("pool", lambda e: e.affine_select(out=ident_f[:], in_=ident_f[:], pattern=[[-1, 128]], compare_op=ALU.is_equal, fill=0.0, base=0, channel_multiplier=1), reads=["ident_f"], writes=["ident_f"])
    P.op("dve", lambda e: e.tensor_copy(out=ident_b[:], in_=ident_f[:]), reads=["ident_f"], writes=["ident_b"])
    P.op("pool", lambda e: e.memset(ones_f[:], 1.0), writes=["ones_f"])
    P.op("pool", lambda e: e.memset(ones_b[:], 1.0), writes=["ones_b"])
    P.op("pool", lambda e: e.memset(bd64_f[:], 1.0), writes=["bd64_f"])
    P.op("pool", lambda e: e.affine_select(out=bd64_f[:, 0:64], in_=bd64_f[:, 0:64], pattern=[[0, 64]], compare_op=ALU.is_ge, fill=0.0, base=63, channel_multiplier=-1), reads=["bd64_f"], writes=["bd64_f"])
    P.op("pool", lambda e: e.affine_select(out=bd64_f[:, 64:128], in_=bd64_f[:, 64:128], pattern=[[0, 64]], compare_op=ALU.is_ge, fill=0.0, base=-64, channel_multiplier=1), reads=["bd64_f"], writes=["bd64_f"])
    P.op("dve", lambda e: e.tensor_copy(out=bd64_b[:], in_=bd64_f[:]), reads=["bd64_f"], writes=["bd64_b"])
    P.op("pool", lambda e: e.memset(u64[:], 1.0), writes=["u64"])
    P.op("pool", lambda e: e.affine_select(out=u64[:], in_=u64[:], pattern=[[1, 64]], compare_op=ALU.is_ge, fill=0.0, base=0, channel_multiplier=-1), reads=["u64"], writes=["u64"])
    for t_, op_, nm in ((maskc8, ALU.is_ge, "maskc8"), (masks8, ALU.is_gt, "masks8"), (eye8, ALU.is_equal, "eye8")):
        P.op("pool", lambda e, t_=t_: e.memset(t_[:], 1.0), writes=[nm])
        P.op("pool", lambda e, t_=t_, op_=op_: e.affine_select(out=t_[:], in_=t_[:], pattern=[[0, 8], [-1, 64]], compare_op=op_, fill=0.0, base=0, channel_multiplier=1), reads=[nm], writes=[nm])

    g1_s = sb(stack0, "g1_s", [128, 8], F32)
    caw_s = sb(stack0, "caw_s", [128, 4, 3], F32)
    gA_s = sb(stack0, "gA_s", [128, 4], F32)
    dcw_s = sb(stack0, "dcw_s", [128, 12, 4], F32)
    gdn_s = sb(stack0, "gdn_s", [128, 1], F32)
    P.dma("sp", lambda e: e.dma_start(out=g1_s[:], in_=g1), writes=["g1_s"])
    P.dma("sp", lambda e: e.dma_start(out=caw_s[:], in_=caw), writes=["caw_s"])
    P.dma("sp", lambda e: e.dma_start(out=gA_s[:], in_=gA), writes=["gA_s"])
    P.dma("sp", lambda e: e.dma_start(out=dcw_s[:], in_=dcw), writes=["dcw_s"])
    P.dma("sp", lambda e: e.dma_start(out=gdn_s[:], in_=gdn), writes=["gdn_s"])

    stackR = ExitStack()
    yT = sb(stackR, "yT", [128, 8, S], BF16, side="right")

    stA = ExitStack()
    hT = sb(stA, "hT", [128, 8, S], BF16)
    wring = sb(stA, "wring", [128, 2, 8, 128], BF16)
    wba_s = sb(stA, "wba_s", [128, 8, 8], BF16)
    pc = sb(stA, "pc", [128, 4, S], F32)
    cvt = sb(stA, "cvt", [128, S], F32)
    sqb = sb(stA, "sqb", [128, 2, S], BF16)
    tb_ba = sb(stA, "tb_ba", [64, NCH, 8], F32)
    tb_alog = sb(stA, "tb_alog", [64, NCH, 4], F32)
    tb_dtb = sb(stA, "tb_dtb", [64, NCH, 4], F32)
    tb_beta = sb(stA, "tb_beta", [64, NCH, 4], F32)
    tb_nbeta = sb(stA, "tb_nbeta", [64, NCH, 4], F32)
    tb_t0 = sb(stA, "tb_t0", [64, NCH, 4], F32)
    tb_t1 = sb(stA, "tb_t1", [64, NCH, 4], F32)
    tb_g = sb(stA, "tb_g", [64, NCH, 4], F32)
    tb_gc = sb(stA, "tb_gc", [64, NCH, 4], F32)
    tb_gl = sb(stA, "tb_gl", [128, NCH, 4], F32)
    tb_egl = sb(stA, "tb_egl", [128, NCH, 4], F32)
    tb_kbe = sb(stA, "tb_kbe", [64, NCH, 4], F32)
    tb_kdec = sb(stA, "tb_kdec", [64, NCH, 4], F32)

    zt = sb(stA, "zt", [128, 2048], BF16)
    P.op("pool", lambda e: e.memset(zt[:], 0.0), writes=["zt"])
    xz_keys = []
    for i in range(NE):
        P.dma("sp", lambda e, i=i: e.dma_start(out=Xs[i * 256:(i + 1) * 256, :].rearrange("(p b) d -> p (b d)", b=2), in_=zt[:]), reads=["zt"], writes=[("Xz", i)])
        xz_keys.append(("Xz", i))
    P.dma("sp", lambda e: e.dma_start(out=Xs[DUMP:DUMP + 128, :], in_=zt[:, 0:D]), reads=["zt"], writes=[("Xz", NE)])
    xz_keys.append(("Xz", NE))
    P.dma("sp", lambda e: e.dma_start(out=Ys[DUMP:DUMP + 128, :], in_=zt[:, 0:D]), reads=["zt"], writes=[("Yz", 0)])
    P.dma("pool", lambda e: e.dma_start(out=wba_s[:], in_=w_ba), writes=["wba_s"])
    ab_s = sb(stA, "ab_s", [64, 2, 4], F32)
    P.dma("sp", lambda e: e.dma_start(out=ab_s[:, 0, :], in_=bass.AP(alog_h, 0, [[0, 64], [1, 4]])), writes=["ab_s0"])
    P.dma("sp", lambda e: e.dma_start(out=ab_s[:, 1, :], in_=bass.AP(dtb_h, 0, [[0, 64], [1, 4]])), writes=["ab_s1"])
    P.op("dve", lambda e: e.tensor_copy(out=tb_alog[:], in_=ab_s[:, 0:1, :].to_broadcast([64, NCH, 4])), reads=["ab_s0"], writes=["tb_alog"])
    P.op("dve", lambda e: e.tensor_copy(out=tb_dtb[:], in_=ab_s[:, 1:2, :].to_broadcast([64, NCH, 4])), reads=["ab_s1"], writes=["tb_dtb"])

    stA2 = ExitStack()
    xs = sb(stA2, "xs", [128, 2, S], F32)
    rbc = sb(stA2, "rbc", [128, S], F32)
    for kc in range(8):
        sl = kc % 2
        P.dma("sp", lambda e, kc=kc, sl=sl: e.dma_start(out=xs[:, sl, :], in_=xT[kc * 128:(kc + 1) * 128, :]), writes=[("xs", sl)])
        P.op("act", lambda e, sl=sl: e.activation(out=sqb[:, sl, :], in_=xs[:, sl, :], func=AF.Square), reads=[("xs", sl)], writes=[("sqb", sl)])
        for tb in range(4):
            P.op("pe", lambda e, kc=kc, sl=sl, tb=tb: e.matmul(psv(tb), lhsT=ones_b[:], rhs=sqb[:, sl, tb * 512:(tb + 1) * 512], start=(kc == 0), stop=(kc == 7)),
                 reads=[("sqb", sl), "ones_b"], writes=[pk(tb)])
    for tb in range(4):
        P.op("act", lambda e, tb=tb: e.activation(out=rbc[:, tb * 512:(tb + 1) * 512], in_=psv(tb), func=AF.Sqrt, scale=1.0 / D, bias=EPS), reads=[pk(tb)], writes=[("rbc", tb)])
        P.op("dve", lambda e, tb=tb: e.reciprocal(out=rbc[:, tb * 512:(tb + 1) * 512], in_=rbc[:, tb * 512:(tb + 1) * 512]), reads=[("rbc", tb)], writes=[("rbc", tb)])
    for kc in range(8):
        sl = kc % 2
        P.dma("sp", lambda e, kc=kc, sl=sl: e.dma_start(out=xs[:, sl, :], in_=xT[kc * 128:(kc + 1) * 128, :]), writes=[("xs", sl)])
        P.op("dve", lambda e, kc=kc, sl=sl: e.scalar_tensor_tensor(out=hT[:, kc, :], in0=xs[:, sl, :], scalar=g1_s[:, kc:kc + 1], in1=rbc[:], op0=ALU.mult, op1=ALU.mult),
             reads=[("xs", sl), "g1_s"] + [("rbc", tb) for tb in range(4)], writes=[("hT", kc)])
    stA2.close()
    hT_keys = [("hT", kc) for kc in range(8)]

    pre_list = []
    for e_ in range(NE):
        pre_list.append((Wg_b, w_gate, "g", e_))
        pre_list.append((Wu_b, w_up, "u", e_))
        pre_list.append((Wd_b, w_down, "d", e_))
    pre_i = [0]

    def prestage(n):
        for _ in range(n):
            if pre_i[0] >= len(pre_list):
                return
            dst, src, kd, e_ = pre_list[pre_i[0]]
            pre_i[0] += 1
            P.dma("pool", lambda e, dst=dst, src=src, e_=e_: e.dma_start(out=dst[e_], in_=src[e_]), writes=[("Wb", kd, e_)])

    wr_i = [0]

    def proj_chunk(c, slot):
        ws = wr_i[0] % 2
        wr_i[0] += 1
        P.dma("pool", lambda e: e.dma_start(out=wring[:, ws, :, :], in_=w_in_r[c]), writes=[("wring", ws)])
        if c < 12:
            prestage(1)
        for tb in range(4):
            b = bank()
            for kc in range(8):
                P.op("pe", lambda e, kc=kc, tb=tb, b=b: e.matmul(psv(b), lhsT=wring[:, ws, kc, :], rhs=hT[:, kc, tb * 512:(tb + 1) * 512], start=(kc == 0), stop=(kc == 7)),
                     reads=[("wring", ws), ("hT", kc)], writes=[pk(b)])
            P.op("act", lambda e, tb=tb, b=b: e.copy(out=pc[:, slot, tb * 512:(tb + 1) * 512], in_=psv(b)), reads=[pk(b)], writes=[("pc", slot, tb)])

    def pck(slot):
        return [("pc", slot, tb) for tb in range(4)]

    bb = bank()
    for c in range(NCH):
        for kc in range(8):
            P.op("pe", lambda e, c=c, kc=kc: e.matmul(psum[0:64, bb * 512 + c * 8: bb * 512 + c * 8 + 8], lhsT=hT[:, kc, c * 64:(c + 1) * 64], rhs=wba_s[:, kc, :], start=(kc == 0), stop=(kc == 7)),
                 reads=[("hT", kc), "wba_s"], writes=[pk(bb)])
    P.op("act", lambda e: e.copy(out=tb_ba[:].rearrange("p c k -> p (c k)"), in_=psum[0:64, bb * 512: bb * 512 + 256]), reads=[pk(bb)], writes=["tb_ba"])
    P.op("act", lambda e: e.activation(out=tb_beta[:], in_=tb_ba[:, :, 0:4], func=AF.Sigmoid), reads=["tb_ba"], writes=["tb_beta"])
    P.op("dve", lambda e: e.tensor_scalar(out=tb_nbeta[:], in0=tb_beta[:], scalar1=-1.0, scalar2=None, op0=ALU.mult), reads=["tb_beta"], writes=["tb_nbeta"])
    P.op("dve", lambda e: e.tensor_tensor(out=tb_t0[:], in0=tb_ba[:, :, 4:8], in1=tb_dtb[:], op=ALU.add), reads=["tb_ba", "tb_dtb"], writes=["tb_t0"])
    P.op("act", lambda e: e.activation(out=tb_t1[:], in_=tb_t0[:], func=AF.Abs), reads=["tb_t0"], writes=["tb_t1"])
    P.op("act", lambda e: e.activation(out=tb_t1[:], in_=tb_t1[:], func=AF.Exp, scale=-1.0), reads=["tb_t1"], writes=["tb_t1"])
    P.op("act", lambda e: e.activation(out=tb_t1[:], in_=tb_t1[:], func=AF.Ln, bias=1.0), reads=["tb_t1"], writes=["tb_t1"])
    P.op("dve", lambda e: e.tensor_scalar(out=tb_t0[:], in0=tb_t0[:], scalar1=0.0, scalar2=None, op0=ALU.max), reads=["tb_t0"], writes=["tb_t0"])
    P.op("dve", lambda e: e.tensor_tensor(out=tb_t0[:], in0=tb_t0[:], in1=tb_t1[:], op=ALU.add), reads=["tb_t0", "tb_t1"], writes=["tb_t0"])
    P.op("act", lambda e: e.activation(out=tb_alog[:], in_=tb_alog[:], func=AF.Exp), reads=["tb_alog"], writes=["tb_alog"])
    P.op("dve", lambda e: e.scalar_tensor_tensor(out=tb_g[:], in0=tb_t0[:], scalar=-1.0, in1=tb_alog[:], op0=ALU.mult, op1=ALU.mult), reads=["tb_t0", "tb_alog"], writes=["tb_g"])
    gflat = tb_g[:].rearrange("p c k -> p (c k)")
    b1 = bank()
    P.op("pe", lambda e: e.matmul(psum[0:64, b1 * 512:b1 * 512 + 128], lhsT=u64[:], rhs=gflat, start=True, stop=True), reads=["u64", "tb_g"], writes=[pk(b1)])
    P.op("act", lambda e: e.copy(out=tb_gc[:].rearrange("p c k -> p (c k)"), in_=psum[0:64, b1 * 512:b1 * 512 + 128]), reads=[pk(b1)], writes=["tb_gc"])
    b2 = bank()
    P.op("pe", lambda e: e.matmul(psum[0:128, b2 * 512:b2 * 512 + 128], lhsT=ones_f[0:64, :], rhs=gflat, start=True, stop=True), reads=["ones_f", "tb_g"], writes=[pk(b2)])
    P.op("act", lambda e: e.copy(out=tb_gl[:].rearrange("p c k -> p (c k)"), in_=psum[0:128, b2 * 512:b2 * 512 + 128]), reads=[pk(b2)], writes=["tb_gl"])
    P.op("act", lambda e: e.activation(out=tb_egl[:], in_=tb_gl[:], func=AF.Exp), reads=["tb_gl"], writes=["tb_egl"])
    P.op("act", lambda e: e.activation(out=tb_t1[:], in_=tb_gc[:], func=AF.Exp), reads=["tb_gc"], writes=["tb_t1"])
    P.op("dve", lambda e: e.tensor_tensor(out=tb_kbe[:], in0=tb_beta[:], in1=tb_t1[:], op=ALU.mult), reads=["tb_beta", "tb_t1"], writes=["tb_kbe"])
    P.op("dve", lambda e: e.tensor_tensor(out=tb_kdec[:], in0=tb_gl[0:64], in1=tb_gc[:], op=ALU.subtract), reads=["tb_gl", "tb_gc"], writes=["tb_kdec"])
    P.op("act", lambda e: e.activation(out=tb_kdec[:], in_=tb_kdec[:], func=AF.Exp), reads=["tb_kdec"], writes=["tb_kdec"])

    def conv(eng, dst, dkeys, src, skeys, wtile, wkey, widx, K):
        P.op("act", lambda e: e.activation(out=dst, in_=src, func=AF.Copy, scale=wtile[:, widx, K - 1:K]), reads=skeys + [wkey], writes=dkeys)
        for j in range(K - 1):
            sh = K - 1 - j
            P.op("dve", lambda e, j=j, sh=sh: e.scalar_tensor_tensor(out=dst[:, sh:], in0=src[:, 0:S - sh], scalar=wtile[:, widx, j:j + 1], in1=dst[:, sh:], op0=ALU.mult, op1=ALU.add),
                 reads=skeys + [wkey] + dkeys, writes=dkeys)

    sq_i = [0]

    def inv_rms(src, skeys, lhs, lkey, scale):
        sl = sq_i[0] % 2
        sq_i[0] += 1
        P.op("act", lambda e: e.activation(out=sqb[:, sl, :], in_=src, func=AF.Square), reads=skeys, writes=[("sqb", sl)])
        for tb in range(4):
            b = bank()
            P.op("pe", lambda e, tb=tb, b=b: e.matmul(psv(b), lhsT=lhs, rhs=sqb[:, sl, tb * 512:(tb + 1) * 512], start=True, stop=True), reads=[("sqb", sl), lkey], writes=[pk(b)])
            P.op("act", lambda e, tb=tb, b=b: e.activation(out=cvt[:, tb * 512:(tb + 1) * 512], in_=psv(b), func=AF.Ln, scale=scale, bias=EPS), reads=[pk(b)], writes=["cvt"])
        P.op("act", lambda e: e.activation(out=cvt[:], in_=cvt[:], func=AF.Exp, scale=-0.5), reads=["cvt"], writes=["cvt"])

    for j in range(4):
        proj_chunk(j, 0)
        proj_chunk(4 + j, 1)
        proj_chunk(8 + j, 2)
        P.op("dve", lambda e: e.tensor_tensor(out=pc[:, 0, :], in0=pc[:, 0, :], in1=pc[:, 2, :], op=ALU.mult), reads=pck(0) + pck(2), writes=pck(0))
        conv("pool", pc[:, 2, :], pck(2), pc[:, 0, :], pck(0), caw_s, "caw_s", j, 3)
        P.op("dve", lambda e: e.tensor_tensor(out=pc[:, 2, :], in0=pc[:, 2, :], in1=pc[:, 1, :], op=ALU.mult), reads=pck(1) + pck(2), writes=pck(2))
        inv_rms(pc[:, 2, :], pck(2), bd64_b[:], "bd64_b", 1.0 / 64)
        P.op("dve", lambda e, j=j: e.scalar_tensor_tensor(out=yT[:, j, :], in0=pc[:, 2, :], scalar=gA_s[:, j:j + 1], in1=cvt[:], op0=ALU.mult, op1=ALU.mult),
             reads=pck(2) + ["gA_s", "cvt"], writes=[("yT", j)])

    dn = ExitStack()
    GW = 8
    NSET = 2
    qb = sb(dn, "qb", [128, S], BF16)
    kb = sb(dn, "kb", [128, S], BF16)
    vb = sb(dn, "vb", [128, S], BF16)
    NPAR = 3
    ATg = sb(dn, "ATg", [64, NPAR, GW, 64], BF16)
    Kdg = sb(dn, "Kdg", [64, NPAR, GW, 128], BF16)
    Ug = sb(dn, "Ug", [64, NPAR, GW, 128], F32)
    WTg = sb(dn, "WTg", [128, NPAR, GW * 64], BF16)
    qsb = sb(dn, "qsb", [128, S], BF16)
    Sst = sb(dn, "Sst", [128, 2, 128], F32)
    Sb = sb(dn, "Sb", [128, 2, 128], BF16)
    vnew = sb(dn, "vnew", [64, 2, 128], BF16)
    Og = sb(dn, "Og", [64, 4, 128], F32)
    Ogb = sb(dn, "Ogb", [64, 4, 128], BF16)
    Osq = sb(dn, "Osq", [64, 4, 128], F32)
    oss = sb(dn, "oss", [64, 4], F32)

    SC = []
    s0 = {"id": 0}
    s0["GU"] = sb(dn, "s0_GU", [64, GW, 64], F32)[:]
    s0["E"] = sb(dn, "s0_E", [128, GW * 64], F32)[:]
    for nm in ("DS", "DC", "A", "N0", "B0", "N1", "B1", "R0", "R1"):
        s0[nm] = sb(dn, "s0_" + nm, [64, GW, 64], BF16)[:]
    s0["Kbe"] = sb(dn, "s0_Kbe", [64, GW, 128], BF16)[:]
    s0["Vb"] = sb(dn, "s0_Vb", [64, GW, 128], BF16)[:]
    SC.append(s0)

    def bfv(ap, c):
        return ap.bitcast(BF16).rearrange("p (a c) -> p a c", c=c)

    s1 = {"id": 1}
    s1["GU"] = pc[0:64, 1, 0:512].rearrange("p (a c) -> p a c", c=64)
    s1["E"] = pc[:, 1, 512:1024]
    for i_, nm in enumerate(("DS", "DC", "A", "N0")):
        s1[nm] = bfv(pc[0:64, 1, 1024 + 256 * i_:1280 + 256 * i_], 64)
    for i_, nm in enumerate(("B0", "N1", "B1", "R0", "R1")):
        s1[nm] = bfv(pc[0:64, 2, 256 * i_:256 * (i_ + 1)], 64)
    s1["Kbe"] = bfv(pc[0:64, 2, 1280:1792], 128)
    s1["Vb"] = bfv(cvt[0:64, 0:512], 128)
    SC.append(s1)

    def ops512(v):
        return v.rearrange("p a c -> p (a c)")

    def dn_phase1(h, cg, par, Sx):
        sid = Sx["id"]

        def K(nm):
            ks_ = [(nm, sid)]
            if sid == 1:
                ks_.append("alias1")
            return ks_

        def KW(nm):
            return [(nm, sid)]

        qs = pc[:, 0, :]
        c0 = cg * GW
        cols = slice(c0 * 64, (c0 + GW) * 64)
        qk = [("pc", 0, cg)]
        GU, DS, DC, A, E_s, Kbe, Vb = Sx["GU"], Sx["DS"], Sx["DC"], Sx["A"], Sx["E"], Sx["Kbe"], Sx["Vb"]
        al = ["alias1"] if sid == 1 else []
        P.op("pool", lambda e: e.tensor_tensor(out=GU[:], in0=u64[:, None, :].to_broadcast([64, GW, 64]), in1=tb_g[:, c0:c0 + GW, h:h + 1].to_broadcast([64, GW, 64]), op=ALU.mult),
             reads=["u64", "tb_g"] + al, writes=KW("GU"))
        bg = bank()
        P.op("pe", lambda e: e.matmul(psv(bg), lhsT=ones_f[0:64, :], rhs=ops512(GU[:]), start=True, stop=True), reads=["ones_f"] + K("GU"), writes=[pk(bg)])
        P.op("dve", lambda e: e.tensor_tensor(out=GU[:], in0=psv(bg, 64).rearrange("p (a c) -> p a c", c=64), in1=tb_gc[:, c0:c0 + GW, h:h + 1].to_broadcast([64, GW, 64]), op=ALU.subtract),
             reads=[pk(bg), "tb_gc"] + al, writes=KW("GU"))
        P.op("dve", lambda e: e.tensor_scalar(out=GU[:], in0=GU[:], scalar1=0.0, scalar2=None, op0=ALU.max), reads=K("GU"), writes=KW("GU"))
        P.op("act", lambda e: e.activation(out=GU[:], in_=GU[:], func=AF.Exp, scale=-1.0), reads=K("GU"), writes=KW("GU"))
        P.op("act", lambda e: e.activation(out=E_s[:], in_=psv(bg), func=AF.Exp), reads=[pk(bg)] + al, writes=KW("E"))
        yield
        P.op("pool", lambda e: e.tensor_tensor(out=DS[:], in0=GU[:], in1=masks8[:], op=ALU.mult), reads=K("GU") + ["masks8"], writes=KW("DS"))
        P.op("pool", lambda e: e.tensor_tensor(out=DS[:], in0=DS[:], in1=tb_nbeta[:, c0:c0 + GW, h:h + 1].to_broadcast([64, GW, 64]), op=ALU.mult), reads=K("DS") + ["tb_nbeta"], writes=KW("DS"))
        P.op("pool", lambda e: e.tensor_tensor(out=DC[:], in0=GU[:], in1=maskc8[:], op=ALU.mult), reads=K("GU") + ["maskc8"], writes=KW("DC"))
        N0, B0 = Sx["N0"], Sx["B0"]
        bk = bank()
        for a in range(GW):
            cs = slice((c0 + a) * 64, (c0 + a + 1) * 64)
            P.op("pe", lambda e, a=a, cs=cs: e.matmul(psum[0:64, bk * 512 + a * 64: bk * 512 + (a + 1) * 64], lhsT=kb[:, cs], rhs=kb[:, cs], start=True, stop=True), reads=["kb"], writes=[pk(bk)])
        P.op("dve", lambda e: e.tensor_tensor(out=ops512(N0[:]), in0=psv(bk, 64), in1=ops512(DS[:]), op=ALU.mult), reads=[pk(bk)] + K("DS"), writes=KW("N0"))
        yield
        bq = bank()
        for a in range(GW):
            cs = slice((c0 + a) * 64, (c0 + a + 1) * 64)
            P.op("pe", lambda e, a=a, cs=cs: e.matmul(psum[0:64, bq * 512 + a * 64: bq * 512 + (a + 1) * 64], lhsT=qb[:, cs], rhs=kb[:, cs], start=True, stop=True), reads=["qb", "kb"], writes=[pk(bq)])
        P.op("dve", lambda e: e.tensor_tensor(out=ops512(A[:]), in0=psv(bq, 64), in1=ops512(DC[:]), op=ALU.mult), reads=[pk(bq)] + K("DC"), writes=KW("A"))
        P.op("pool", lambda e: e.tensor_tensor(out=qsb[:, cols], in0=qs[:, cols], in1=E_s[:], op=ALU.mult), reads=qk + K("E"), writes=[("qsb", cg)])
        yield
        bt = bank()
        for a in range(GW):
            P.op("pe", lambda e, a=a: e.matmul(psum[0:64, bt * 512 + a * 64: bt * 512 + (a + 1) * 64], lhsT=N0[:, a, :], rhs=ident_b[0:64, 0:64], start=True, stop=True), reads=K("N0") + ["ident_b"], writes=[pk(bt)])
        P.op("act", lambda e: e.copy(out=ops512(B0[:]), in_=psv(bt, 64)), reads=[pk(bt)] + al, writes=KW("B0"))
        yield
        ba_ = bank()
        for a in range(GW):
            P.op("pe", lambda e, a=a: e.matmul(psum[0:64, ba_ * 512 + a * 64: ba_ * 512 + (a + 1) * 64], lhsT=A[:, a, :], rhs=ident_b[0:64, 0:64], start=True, stop=True), reads=K("A") + ["ident_b"], writes=[pk(ba_)])
        P.op("act", lambda e: e.copy(out=ops512(ATg[:, par]), in_=psv(ba_, 64)), reads=[pk(ba_)], writes=[("ATg", par)])
        R = [Sx["R0"], Sx["R1"]]
        Nn = [Sx["N0"], Sx["N1"]]
        Bn = [Sx["B0"], Sx["B1"]]
        P.op("pool", lambda e: e.tensor_tensor(out=R[0][:], in0=B0[:], in1=eye8[:], op=ALU.add), reads=K("B0") + ["eye8"], writes=KW("R0"))
        yield
        cur = 0
        for lvl in range(5):
            nxt = 1 - cur
            nk, bkk = "N%d" % cur, "B%d" % cur
            nk2, bk2 = "N%d" % nxt, "B%d" % nxt
            rk, rk2 = "R%d" % cur, "R%d" % nxt
            pn = bank()
            for a in range(GW):
                P.op("pe", lambda e, a=a, cur=cur, pn=pn: e.matmul(psum[0:64, pn * 512 + a * 64: pn * 512 + (a + 1) * 64], lhsT=Bn[cur][:, a, :], rhs=Nn[cur][:, a, :], start=True, stop=True),
                     reads=K(nk) + K(bkk), writes=[pk(pn)])
            P.op("act", lambda e, nxt=nxt, pn=pn: e.copy(out=ops512(Nn[nxt][:]), in_=psv(pn, 64)), reads=[pk(pn)] + al, writes=KW(nk2))
            yield
            if lvl < 4:
                pb = bank()
                for a in range(GW):
                    P.op("pe", lambda e, a=a, cur=cur, pb=pb: e.matmul(psum[0:64, pb * 512 + a * 64: pb * 512 + (a + 1) * 64], lhsT=Nn[cur][:, a, :], rhs=Bn[cur][:, a, :], start=True, stop=True),
                         reads=K(nk) + K(bkk), writes=[pk(pb)])
                P.op("act", lambda e, nxt=nxt, pb=pb: e.copy(out=ops512(Bn[nxt][:]), in_=psv(pb, 64)), reads=[pk(pb)] + al, writes=KW(bk2))
                yield
            pr = bank()
            for a in range(GW):
                P.op("pe", lambda e, a=a, cur=cur, nxt=nxt, pr=pr: e.matmul(psum[0:64, pr * 512 + a * 64: pr * 512 + (a + 1) * 64], lhsT=Nn[nxt][:, a, :], rhs=R[cur][:, a, :], start=True, stop=True),
                     reads=K(nk2) + K(rk), writes=[pk(pr)])
            P.op("dve", lambda e, cur=cur, nxt=nxt, pr=pr: e.tensor_tensor(out=ops512(R[nxt][:]), in0=psv(pr, 64), in1=ops512(R[cur][:]), op=ALU.add), reads=[pk(pr)] + K(rk), writes=KW(rk2))
            cur = nxt
            yield
        Rf = R[cur]
        rfk = "R%d" % cur
        bkt = bank(2)
        for a in range(GW):
            cs = slice((c0 + a) * 64, (c0 + a + 1) * 64)
            P.op("pe", lambda e, a=a, cs=cs: e.matmul(psum[0:64, bkt * 512 + a * 128: bkt * 512 + (a + 1) * 128], lhsT=kb[:, cs], rhs=ident_b[:], start=True, stop=True), reads=["kb", "ident_b"], writes=[pk(bkt), pk(bkt + 1)])
        kt3 = psum[0:64, bkt * 512:(bkt + 2) * 512].rearrange("p (a c) -> p a c", c=128)
        P.op("dve", lambda e: e.tensor_tensor(out=Kbe[:], in0=kt3, in1=tb_kbe[:, c0:c0 + GW, h:h + 1].to_broadcast([64, GW, 128]), op=ALU.mult), reads=[pk(bkt), pk(bkt + 1), "tb_kbe"] + al, writes=KW("Kbe"))
        P.op("dve", lambda e: e.tensor_tensor(out=Kdg[:, par], in0=kt3, in1=tb_kdec[:, c0:c0 + GW, h:h + 1].to_broadcast([64, GW, 128]), op=ALU.mult), reads=[pk(bkt), pk(bkt + 1), "tb_kdec"], writes=[("Kdg", par)])
        yield
        bvt = bank(2)
        for a in range(GW):
            cs = slice((c0 + a) * 64, (c0 + a + 1) * 64)
            P.op("pe", lambda e, a=a, cs=cs: e.matmul(psum[0:64, bvt * 512 + a * 128: bvt * 512 + (a + 1) * 128], lhsT=vb[:, cs], rhs=ident_b[:], start=True, stop=True), reads=["vb", "ident_b"], writes=[pk(bvt), pk(bvt + 1)])
        vt3 = psum[0:64, bvt * 512:(bvt + 2) * 512].rearrange("p (a c) -> p a c", c=128)
        P.op("dve", lambda e: e.tensor_tensor(out=Vb[:], in0=vt3, in1=tb_beta[:, c0:c0 + GW, h:h + 1].to_broadcast([64, GW, 128]), op=ALU.mult), reads=[pk(bvt), pk(bvt + 1), "tb_beta"] + al, writes=KW("Vb"))
        yield
        bu = bank(2)
        for a in range(GW):
            P.op("pe", lambda e, a=a: e.matmul(psum[0:64, bu * 512 + a * 128: bu * 512 + (a + 1) * 128], lhsT=Rf[:, a, :], rhs=Vb[:, a, :], start=True, stop=True), reads=K(rfk) + K("Vb"), writes=[pk(bu), pk(bu + 1)])
        P.op("act", lambda e: e.copy(out=Ug[:, par].rearrange("p a c -> p (a c)"), in_=psum[0:64, bu * 512:(bu + 2) * 512]), reads=[pk(bu), pk(bu + 1)], writes=[("Ug", par)])
        yield
        bw = bank()
        for a in range(GW):
            P.op("pe", lambda e, a=a: e.matmul(psum[0:128, bw * 512 + a * 64: bw * 512 + (a + 1) * 64], lhsT=Kbe[:, a, :], rhs=Rf[:, a, :], start=True, stop=True), reads=K(rfk) + K("Kbe"), writes=[pk(bw)])
        P.op("act", lambda e: e.copy(out=WTg[:, par, :], in_=psv(bw)), reads=[pk(bw)], writes=[("WTg", par)])
        yield

    s_par = [0]

    def dn_scan(h, cg, par):
        zs = pc[:, 3, :]
        c0 = cg * GW
        for a in range(GW):
            c = c0 + a
            cs = slice(c * 64, (c + 1) * 64)
            sp_, sn_ = s_par[0], 1 - s_par[0]
            vp = a % 2
            bw = bank()
            P.op("pe", lambda e, a=a, sp_=sp_, bw=bw: e.matmul(psum[0:64, bw * 512: bw * 512 + 128], lhsT=WTg[:, par, a * 64:(a + 1) * 64], rhs=Sb[:, sp_, :], start=True, stop=True),
                 reads=[("WTg", par), ("Sb", sp_)], writes=[pk(bw)])
            P.op("dve", lambda e, a=a, vp=vp, bw=bw: e.tensor_tensor(out=vnew[:, vp, :], in0=Ug[:, par, a, :], in1=psum[0:64, bw * 512: bw * 512 + 128], op=ALU.subtract),
                 reads=[("Ug", par), pk(bw)], writes=[("vnew", vp)])
            yield
            bs = bank()
            P.op("pe", lambda e, a=a, vp=vp, bs=bs: e.matmul(psum[0:128, bs * 512: bs * 512 + 128], lhsT=Kdg[:, par, a, :], rhs=vnew[:, vp, :], start=True, stop=True),
                 reads=[("Kdg", par), ("vnew", vp)], writes=[pk(bs)])
            oq = a % 4
            bo = 6 + ((c // 4) % 2)
            P.op("pe", lambda e, cs=cs, sp_=sp_, bo=bo, oq=oq: e.matmul(psum[0:64, bo * 512 + oq * 128: bo * 512 + (oq + 1) * 128], lhsT=qsb[:, cs], rhs=Sb[:, sp_, :], start=True, stop=False),
                 reads=[("qsb", cg), ("Sb", sp_)], writes=[pk(bo)])
            P.op("pe", lambda e, a=a, vp=vp, bo=bo, oq=oq: e.matmul(psum[0:64, bo * 512 + oq * 128: bo * 512 + (oq + 1) * 128], lhsT=ATg[:, par, a, :], rhs=vnew[:, vp, :], start=False, stop=True),
                 reads=[("ATg", par), ("vnew", vp)], writes=[pk(bo)])
            P.op("dve", lambda e, c=c, sp_=sp_, sn_=sn_, bs=bs: e.scalar_tensor_tensor(out=Sb[:, sn_, :], in0=Sst[:, sp_, :], scalar=tb_egl[:, c, h:h + 1], in1=psum[0:128, bs * 512: bs * 512 + 128], op0=ALU.mult, op1=ALU.add),
                 reads=[("S", sp_), "tb_egl", pk(bs)], writes=[("Sb", sn_)])
            P.op("dve", lambda e, c=c, sp_=sp_, sn_=sn_, bs=bs: e.scalar_tensor_tensor(out=Sst[:, sn_, :], in0=Sst[:, sp_, :], scalar=tb_egl[:, c, h:h + 1], in1=psum[0:128, bs * 512: bs * 512 + 128], op0=ALU.mult, op1=ALU.add),
                 reads=[("S", sp_), "tb_egl", pk(bs)], writes=[("S", sn_)])
            s_par[0] = sn_
            if oq == 3:
                c4 = c - 3
                P.op("act", lambda e, bo=bo: e.copy(out=Og[:].rearrange("p a c -> p (a c)"), in_=psv(bo, 64)), reads=[pk(bo)], writes=["Og"])
                P.op("pool", lambda e: e.tensor_tensor(out=Osq[:], in0=Og[:], in1=Og[:], op=ALU.mult), reads=["Og"], writes=["Osq"])
                P.op("dve", lambda e: e.tensor_reduce(out=oss[:], in_=Osq[:], axis=AX.X, op=ALU.add), reads=["Osq"], writes=["oss"])
                P.op("act", lambda e: e.activation(out=oss[:], in_=oss[:], func=AF.Sqrt, scale=1.0 / 128, bias=EPS), reads=["oss"], writes=["oss"])
                P.op("dve", lambda e: e.reciprocal(out=oss[:], in_=oss[:]), reads=["oss"], writes=["oss"])
                P.op("pool", lambda e: e.tensor_tensor(out=Ogb[:], in0=Og[:], in1=oss[:, :, None].to_broadcast([64, 4, 128]), op=ALU.mult), reads=["Og", "oss"], writes=["Ogb"])
                bt = bank()
                for q4 in range(4):
                    P.op("pe", lambda e, q4=q4, bt=bt: e.matmul(psum[0:128, bt * 512 + q4 * 64: bt * 512 + (q4 + 1) * 64], lhsT=Ogb[:, q4, :], rhs=ident_b[0:64, 0:64], start=True, stop=True), reads=["Ogb", "ident_b"], writes=[pk(bt)])
                P.op("dve", lambda e, c4=c4, bt=bt: e.tensor_tensor(out=yT[:, 4 + h, c4 * 64:(c4 + 4) * 64], in0=psum[0:128, bt * 512: bt * 512 + 256], in1=zs[:, c4 * 64:(c4 + 4) * 64], op=ALU.mult),
                     reads=[pk(bt), ("pc", 3, cg)], writes=[("yT", 4 + h)])
            yield

    for h in range(4):
        bank_lim[0] = 8
        proj_chunk(12 + h, 0)
        proj_chunk(16 + h, 1)
        proj_chunk(20 + h, 2)
        proj_chunk(24 + h, 3)
        for s_, m_ in ((0, h), (1, 4 + h), (2, 8 + h)):
            conv("pool", cvt[:], ["cvt"], pc[:, s_, :], pck(s_), dcw_s, "dcw_s", m_, 4)
            if s_ == 2:
                P.op("act", lambda e: e.activation(out=vb[:], in_=cvt[:], func=AF.Silu), reads=["cvt"], writes=["vb"])
            else:
                P.op("act", lambda e, s_=s_: e.activation(out=pc[:, s_, :], in_=cvt[:], func=AF.Silu), reads=["cvt"], writes=pck(s_))
        inv_rms(pc[:, 0, :], pck(0), ones_b[:], "ones_b", 1.0)
        P.op("dve", lambda e: e.scalar_tensor_tensor(out=pc[:, 0, :], in0=pc[:, 0, :], scalar=128 ** -0.5, in1=cvt[:], op0=ALU.mult, op1=ALU.mult), reads=pck(0) + ["cvt"], writes=pck(0))
        P.op("act", lambda e: e.copy(out=qb[:], in_=pc[:, 0, :]), reads=pck(0), writes=["qb"])
        inv_rms(pc[:, 1, :], pck(1), ones_b[:], "ones_b", 1.0)
        P.op("dve", lambda e: e.tensor_tensor(out=kb[:], in0=pc[:, 1, :], in1=cvt[:], op=ALU.mult), reads=pck(1) + ["cvt"], writes=["kb"])
        P.op("act", lambda e: e.activation(out=pc[:, 3, :], in_=pc[:, 3, :], func=AF.Silu), reads=pck(3), writes=pck(3))
        P.op("dve", lambda e: e.tensor_scalar(out=pc[:, 3, :], in0=pc[:, 3, :], scalar1=gdn_s[:, 0:1], scalar2=None, op0=ALU.mult), reads=pck(3) + ["gdn_s"], writes=pck(3))
        P.op("pool", lambda e: e.memset(Sst[:, 0, :], 0.0), writes=[("S", 0)])
        P.op("pool", lambda e: e.memset(Sb[:, 0, :], 0.0), writes=[("Sb", 0)])
        s_par[0] = 0
        ngr = NCH // GW
        bank_lim[0] = 6
        bank_rr[0] = 0
        P.op("pool", lambda e: e.memset(oss[:], 0.0), reads=[], writes=pck(1) + pck(2) + ["cvt", "alias1", "oss"])
        pending = list(range(ngr))
        free_sets = list(range(NSET))
        running = []
        p1_done = set()
        scan_next = 0
        scan_running = False
        rnd = 0
        while pending or running or scan_next < ngr:
            rnd += 1
            if rnd % 4 == 0:
                prestage(1)
            while pending and free_sets and pending[0] - NPAR < scan_next:
                cg = pending.pop(0)
                si = free_sets.pop(0)
                running.append(["p1", cg, dn_phase1(h, cg, cg % NPAR, SC[si]), si])
            if not scan_running and scan_next < ngr and scan_next in p1_done:
                running.append(["scan", scan_next, dn_scan(h, scan_next, scan_next % NPAR), None])
                scan_running = True
            for r in list(running):
                try:
                    next(r[2])
                except StopIteration:
                    running.remove(r)
                    if r[0] == "p1":
                        free_sets.append(r[3])
                        p1_done.add(r[1])
                    else:
                        scan_next += 1
                        scan_running = False
        P.op("pool", lambda e: e.memset(oss[:], 0.0), reads=[], writes=pck(1) + pck(2) + ["cvt", "alias1", "oss"])
    prestage(len(pre_list))
    print("prestage DMAs issued before flush point; total", pre_i[0])
    dn.close()
    stA.close()
    bank_lim[0] = 8
    P.barrier()

    stB = ExitStack()
    x1 = sb(stB, "x1", [128, NT, D], F32)
    stW = ExitStack()
    wout_s = sb(stW, "wout_s", [128, 8, D], BF16)
    for kc in range(8):
        P.dma("pool", lambda e, kc=kc: e.dma_start(out=wout_s[:, kc, :], in_=w_out_r[:, kc, :]), writes=[("wout", kc)])
    for t in range(NT):
        P.dma("sp", lambda e, t=t: e.dma_start(out=x1[:, t, :], in_=x_tok[t * 128:(t + 1) * 128, :]), writes=[("x1", t)])
    for t in range(NT):
        for dh in range(2):
            b = bank()
            for kc in range(8):
                P.op("pe", lambda e, t=t, dh=dh, kc=kc, b=b: e.matmul(psv(b), lhsT=yT[:, kc, t * 128:(t + 1) * 128], rhs=wout_s[:, kc, dh * 512:(dh + 1) * 512], start=(kc == 0), stop=(kc == 7)),
                     reads=[("yT", kc), ("wout", kc)], writes=[pk(b)])
            P.op("dve", lambda e, t=t, dh=dh, b=b: e.tensor_tensor(out=x1[:, t, dh * 512:(dh + 1) * 512], in0=x1[:, t, dh * 512:(dh + 1) * 512], in1=psv(b), op=ALU.add),
                 reads=[pk(b), ("x1", t)], writes=[("x1", t)])
    stW.close()

    if stage == "A":
        for t in range(NT):
            P.dma("sp", lambda e, t=t: e.dma_start(out=out[t * 128:(t + 1) * 128, :], in_=x1[:, t, :]), reads=[("x1", t)], writes=[("out", t)])
        P.finish([("out", t) for t in range(NT)])
        P.emit()
        return nc


    stackR.close()
    P.barrier()

    BIG = 1.0e30

    g2bc = sb(stB, "g2bc", [128, D], F32)
    g3bc = sb(stB, "g3bc", [128, D], F32)
    wr_s = sb(stB, "wr_s", [128, 8, 36], F32)
    ecap = sb(stB, "ecap", [128, NE], F32)
    ecap_i = sb(stB, "ecap_i", [128, NE], I32)
    ustr_b = sb(stB, "ustr_b", [128, 128], BF16)
    d1i = sb(stB, "d1i", [128, NT], I32)
    d2i = sb(stB, "d2i", [128, NT], I32)
    gt1 = sb(stB, "gt1", [128, NT], F32)
    gt2 = sb(stB, "gt2", [128, NT], F32)
    P.dma("sp", lambda e: e.dma_start(out=g2bc[:], in_=bass.AP(g2_h, 0, [[0, 128], [1, D]])), writes=["g2bc"])
    P.dma("sp", lambda e: e.dma_start(out=g3bc[:], in_=bass.AP(g3_h, 0, [[0, 128], [1, D]])), writes=["g3bc"])
    P.dma("sp", lambda e: e.dma_start(out=wr_s[:], in_=wr_r), writes=["wr_s"])
    P.op("pool", lambda e: e.iota(ecap_i[:], [[CAP, NE]], base=0, channel_multiplier=0), writes=["ecap_i"])
    P.op("dve", lambda e: e.tensor_copy(out=ecap[:], in_=ecap_i[:]), reads=["ecap_i"], writes=["ecap"])
    P.op("pool", lambda e: e.memset(ustr_b[:], 1.0), writes=["ustr_b"])
    P.op("pool", lambda e: e.affine_select(out=ustr_b[:], in_=ustr_b[:], pattern=[[1, 128]], compare_op=ALU.is_gt, fill=0.0, base=0, channel_multiplier=-1), reads=["ustr_b"], writes=["ustr_b"])

    rt = ExitStack()
    h2b = sb(rt, "h2b", [128, NT, D], BF16)
    h2f = sb(rt, "h2f", [128, 3, D], F32)
    h2lo = sb(rt, "h2lo", [128, 3, D], BF16)
    hT2 = sb(rt, "hT2", [128, 3, 2, 8, 128], BF16)
    wr_hi = sb(rt, "wr_hi", [128, 8, 36], BF16)
    wr_lo = sb(rt, "wr_lo", [128, 8, 36], BF16)
    ssq = sb(rt, "ssq", [128, NT], F32)
    Lall = sb(rt, "Lall", [128, NT, 36], F32)
    r_gmax = sb(rt, "r_gmax", [128, NT], F32)
    r_gmask = sb(rt, "r_gmask", [128, NT, 4], F32)
    r_ge = sb(rt, "r_ge", [128, NT, 4], F32)
    r_gp = sb(rt, "r_gp", [128, NT], F32)
    r_pen = sb(rt, "r_pen", [128, NT, 4], F32)
    r_el = sb(rt, "r_el", [128, NT, 32], F32)
    r_el2 = sb(rt, "r_el2", [128, NT, 32], F32)
    r_m1 = sb(rt, "r_m1", [128, NT], F32)
    r_m2 = sb(rt, "r_m2", [128, NT], F32)
    r_mask1 = sb(rt, "r_mask1", [128, NT, 32], F32)
    r_mask2 = sb(rt, "r_mask2", [128, NT, 32], F32)
    r_m12b = sb(rt, "r_m12b", [128, NT, 32], BF16)
    r_e21 = sb(rt, "r_e21", [128, NT], F32)
    r_rank = sb(rt, "r_rank", [128, NT, 32], F32)
    r_valid = sb(rt, "r_valid", [128, NT, 32], F32)
    r_slot = sb(rt, "r_slot", [128, NT, 32], F32)
    r_tmp = sb(rt, "r_tmp", [128, NT, 32], F32)
    r_d1f = sb(rt, "r_d1f", [128, NT], F32)
    r_d2f = sb(rt, "r_d2f", [128, NT], F32)
    r_v1 = sb(rt, "r_v1", [128, NT], F32)
    r_v2 = sb(rt, "r_v2", [128, NT], F32)

    ssq_keys = [("ssq", t) for t in range(NT)]
    lall_keys = [("Lall", t) for t in range(NT)]
    P.op("pool", lambda e: e.memset(ssq[:], 0.0), writes=ssq_keys)
    P.op("act", lambda e: e.copy(out=wr_hi[:], in_=wr_s[:]), reads=["wr_s"], writes=["wr_hi"])
    P.op("dve", lambda e: e.tensor_tensor(out=wr_lo[:], in0=wr_s[:], in1=wr_hi[:], op=ALU.subtract), reads=["wr_s", "wr_hi"], writes=["wr_lo"])

    def r1_tile(t):
        sl = t % 3
        P.op("act", lambda e: e.activation(out=h2f[:, sl, :], in_=x1[:, t, :], func=AF.Square, accum_out=ssq[:, t:t + 1]), reads=[("x1", t), ("ssq", t)], writes=[("h2f", sl), ("ssq", t)])
        P.op("act", lambda e: e.activation(out=ssq[:, t:t + 1], in_=ssq[:, t:t + 1], func=AF.Sqrt, scale=1.0 / D, bias=EPS), reads=[("ssq", t)], writes=[("ssq", t)])
        P.op("dve", lambda e: e.reciprocal(out=ssq[:, t:t + 1], in_=ssq[:, t:t + 1]), reads=[("ssq", t)], writes=[("ssq", t)])
        P.op("dve", lambda e: e.scalar_tensor_tensor(out=h2f[:, sl, :], in0=x1[:, t, :], scalar=ssq[:, t:t + 1], in1=g2bc[:], op0=ALU.mult, op1=ALU.mult),
             reads=[("x1", t), ("ssq", t), "g2bc"], writes=[("h2f", sl)])
        P.op("act", lambda e: e.copy(out=h2b[:, t, :], in_=h2f[:, sl, :]), reads=[("h2f", sl)], writes=[("h2b", t)])
        P.op("pool", lambda e: e.tensor_tensor(out=h2lo[:, sl, :], in0=h2f[:, sl, :], in1=h2b[:, t, :], op=ALU.subtract), reads=[("h2f", sl), ("h2b", t)], writes=[("h2lo", sl)])
        yield
        b0 = bank(2)
        for kc in range(8):
            P.op("pe", lambda e, kc=kc: e.matmul(psum[0:128, b0 * 512 + kc * 128: b0 * 512 + (kc + 1) * 128], lhsT=h2b[:, t, kc * 128:(kc + 1) * 128], rhs=ident_b[:], start=True, stop=True),
                 reads=[("h2b", t), "ident_b"], writes=[pk(b0), pk(b0 + 1)])
        P.op("dve", lambda e: e.tensor_copy(out=hT2[:, sl, 0].rearrange("p k c -> p (k c)"), in_=psum[0:128, b0 * 512:(b0 + 2) * 512]), reads=[pk(b0), pk(b0 + 1)], writes=[("hT2", sl, 0)])
        b2 = bank(2)
        for kc in range(8):
            P.op("pe", lambda e, kc=kc: e.matmul(psum[0:128, b2 * 512 + kc * 128: b2 * 512 + (kc + 1) * 128], lhsT=h2lo[:, sl, kc * 128:(kc + 1) * 128], rhs=ident_b[:], start=True, stop=True),
                 reads=[("h2lo", sl), "ident_b"], writes=[pk(b2), pk(b2 + 1)])
        P.op("dve", lambda e: e.tensor_copy(out=hT2[:, sl, 1].rearrange("p k c -> p (k c)"), in_=psum[0:128, b2 * 512:(b2 + 2) * 512]), reads=[pk(b2), pk(b2 + 1)], writes=[("hT2", sl, 1)])
        yield
        bl = bank()
        n_ = 0
        for kc in range(8):
            for hl, wt, wk in ((0, wr_hi, "wr_hi"), (1, wr_hi, "wr_hi"), (0, wr_lo, "wr_lo")):
                P.op("pe", lambda e, kc=kc, hl=hl, wt=wt, n_=n_: e.matmul(psum[0:128, bl * 512: bl * 512 + 36], lhsT=hT2[:, sl, hl, kc, :], rhs=wt[:, kc, :], start=(n_ == 0), stop=(n_ == 23)),
                     reads=[("hT2", sl, hl), wk], writes=[pk(bl)])
                n_ += 1
        P.op("act", lambda e: e.copy(out=Lall[:, t, :], in_=psum[0:128, bl * 512: bl * 512 + 36]), reads=[pk(bl)], writes=[("Lall", t)])
        yield

    gens_ = [r1_tile(t) for t in range(NT)]
    active_ = []
    nxt_ = 0
    while active_ or nxt_ < NT:
        if nxt_ < NT and len(active_) < 3:
            active_.append(gens_[nxt_])
            nxt_ += 1
        for g_ in list(active_):
            try:
                next(g_)
            except StopIteration:
                active_.remove(g_)

    GLv = Lall[:, :, 0:4]
    ELv = Lall[:, :, 4:36]

    def bc(ap, shape):
        return ap.to_broadcast(shape)

    P.op("dve", lambda e: e.tensor_reduce(out=r_gmax[:], in_=GLv, axis=AX.X, op=ALU.max), reads=lall_keys, writes=["r_gmax"])
    P.op("dve", lambda e: e.tensor_tensor(out=r_gmask[:], in0=GLv, in1=bc(r_gmax[:, :, None], [128, NT, 4]), op=ALU.is_equal), reads=lall_keys + ["r_gmax"], writes=["r_gmask"])
    P.op("dve", lambda e: e.tensor_tensor(out=r_ge[:], in0=GLv, in1=bc(r_gmax[:, :, None], [128, NT, 4]), op=ALU.subtract), reads=lall_keys + ["r_gmax"], writes=["r_ge"])
    P.op("act", lambda e: e.activation(out=r_ge[:], in_=r_ge[:], func=AF.Exp), reads=["r_ge"], writes=["r_ge"])
    P.op("dve", lambda e: e.tensor_reduce(out=r_gp[:], in_=r_ge[:], axis=AX.X, op=ALU.add), reads=["r_ge"], writes=["r_gp"])
    P.op("dve", lambda e: e.reciprocal(out=r_gp[:], in_=r_gp[:]), reads=["r_gp"], writes=["r_gp"])
    P.op("dve", lambda e: e.tensor_scalar(out=r_pen[:], in0=r_gmask[:], scalar1=BIG, scalar2=-BIG, op0=ALU.mult, op1=ALU.add), reads=["r_gmask"], writes=["r_pen"])
    P.op("dve", lambda e: e.tensor_tensor(out=r_el[:].rearrange("p t (g k) -> p t g k", k=8), in0=ELv.rearrange("p t (g k) -> p t g k", k=8), in1=bc(r_pen[:, :, :, None], [128, NT, 4, 8]), op=ALU.add),
         reads=lall_keys + ["r_pen"], writes=["r_el"])
    P.op("dve", lambda e: e.tensor_reduce(out=r_m1[:], in_=r_el[:], axis=AX.X, op=ALU.max), reads=["r_el"], writes=["r_m1"])
    P.op("dve", lambda e: e.tensor_tensor(out=r_mask1[:], in0=r_el[:], in1=bc(r_m1[:, :, None], [128, NT, 32]), op=ALU.is_equal), reads=["r_el", "r_m1"], writes=["r_mask1"])
    P.op("dve", lambda e: e.scalar_tensor_tensor(out=r_el2[:], in0=r_mask1[:], scalar=-BIG, in1=r_el[:], op0=ALU.mult, op1=ALU.add), reads=["r_mask1", "r_el"], writes=["r_el2"])
    P.op("dve", lambda e: e.tensor_reduce(out=r_m2[:], in_=r_el2[:], axis=AX.X, op=ALU.max), reads=["r_el2"], writes=["r_m2"])
    P.op("dve", lambda e: e.tensor_tensor(out=r_mask2[:], in0=r_el2[:], in1=bc(r_m2[:, :, None], [128, NT, 32]), op=ALU.is_equal), reads=["r_el2", "r_m2"], writes=["r_mask2"])
    P.op("dve", lambda e: e.tensor_tensor(out=r_m12b[:], in0=r_mask1[:], in1=r_mask2[:], op=ALU.add), reads=["r_mask1", "r_mask2"], writes=["r_m12b"])
    P.op("dve", lambda e: e.tensor_tensor(out=r_e21[:], in0=r_m2[:], in1=r_m1[:], op=ALU.subtract), reads=["r_m1", "r_m2"], writes=["r_e21"])
    P.op("act", lambda e: e.activation(out=r_e21[:], in_=r_e21[:], func=AF.Exp), reads=["r_e21"], writes=["r_e21"])
    P.op("dve", lambda e: e.tensor_scalar(out=gt1[:], in0=r_e21[:], scalar1=1.0, scalar2=None, op0=ALU.add), reads=["r_e21"], writes=["gt1"])
    P.op("dve", lambda e: e.reciprocal(out=gt1[:], in_=gt1[:]), reads=["gt1"], writes=["gt1"])
    P.op("dve", lambda e: e.tensor_tensor(out=gt1[:], in0=gt1[:], in1=r_gp[:], op=ALU.mult), reads=["gt1", "r_gp"], writes=["gt1"])
    P.op("dve", lambda e: e.tensor_tensor(out=gt2[:], in0=gt1[:], in1=r_e21[:], op=ALU.mult), reads=["gt1", "r_e21"], writes=["gt2"])

    br = bank()
    for t in range(NT):
        P.op("pe", lambda e, t=t: e.matmul(psum[0:128, br * 512 + t * 32: br * 512 + (t + 1) * 32], lhsT=ustr_b[:], rhs=r_m12b[:, t, :], start=True, stop=(t == 0)),
             reads=["ustr_b", "r_m12b"], writes=[pk(br)])
        for t2 in range(t):
            P.op("pe", lambda e, t=t, t2=t2: e.matmul(psum[0:128, br * 512 + t * 32: br * 512 + (t + 1) * 32], lhsT=ones_b[:], rhs=r_m12b[:, t2, :], start=False, stop=(t2 == t - 1)),
                 reads=["ones_b", "r_m12b"], writes=[pk(br)])
    P.op("act", lambda e: e.copy(out=r_rank[:].rearrange("p t k -> p (t k)"), in_=psv(br)), reads=[pk(br)], writes=["r_rank"])
    P.op("dve", lambda e: e.tensor_scalar(out=r_valid[:], in0=r_rank[:], scalar1=float(CAP), scalar2=None, op0=ALU.is_lt), reads=["r_rank"], writes=["r_valid"])
    P.op("dve", lambda e: e.tensor_tensor(out=r_slot[:], in0=r_rank[:], in1=bc(ecap[:, None, :], [128, NT, 32]), op=ALU.add), reads=["r_rank", "ecap"], writes=["r_slot"])
    P.op("dve", lambda e: e.tensor_scalar(out=r_slot[:], in0=r_slot[:], scalar1=-float(DUMP), scalar2=None, op0=ALU.add), reads=["r_slot"], writes=["r_slot"])
    P.op("dve", lambda e: e.tensor_tensor(out=r_slot[:], in0=r_slot[:], in1=r_valid[:], op=ALU.mult), reads=["r_slot", "r_valid"], writes=["r_slot"])
    P.op("dve", lambda e: e.tensor_scalar(out=r_slot[:], in0=r_slot[:], scalar1=float(DUMP), scalar2=None, op0=ALU.add), reads=["r_slot"], writes=["r_slot"])
    for msk, mk, df, dk, vv, vk, di, dik, gt, gk in ((r_mask1, "r_mask1", r_d1f, "r_d1f", r_v1, "r_v1", d1i, "d1i", gt1, "gt1"),
                                                     (r_mask2, "r_mask2", r_d2f, "r_d2f", r_v2, "r_v2", d2i, "d2i", gt2, "gt2")):
        P.op("dve", lambda e, msk=msk: e.tensor_tensor(out=r_tmp[:], in0=msk[:], in1=r_slot[:], op=ALU.mult), reads=[mk, "r_slot"], writes=["r_tmp"])
        P.op("dve", lambda e, df=df: e.tensor_reduce(out=df[:], in_=r_tmp[:], axis=AX.X, op=ALU.add), reads=["r_tmp"], writes=[dk])
        P.op("dve", lambda e, df=df, di=di: e.tensor_copy(out=di[:], in_=df[:]), reads=[dk], writes=[dik])
        P.op("dve", lambda e, msk=msk: e.tensor_tensor(out=r_tmp[:], in0=msk[:], in1=r_valid[:], op=ALU.mult), reads=[mk, "r_valid"], writes=["r_tmp"])
        P.op("dve", lambda e, vv=vv: e.tensor_reduce(out=vv[:], in_=r_tmp[:], axis=AX.X, op=ALU.add), reads=["r_tmp"], writes=[vk])
        P.op("dve", lambda e, vv=vv, gt=gt: e.tensor_tensor(out=gt[:], in0=gt[:], in1=vv[:], op=ALU.mult), reads=[gk, vk], writes=[gk])
    xs_keys = []
    for t in range(NT):
        for di, dik, w in ((d1i, "d1i", 0), (d2i, "d2i", 1)):
            key = ("Xs", t, w)
            xs_keys.append(key)
            P.dma("pool", lambda e, t=t, di=di: e.indirect_dma_start(out=Xs, out_offset=bass.IndirectOffsetOnAxis(ap=di[:, t:t + 1], axis=0), in_=h2b[:, t, :], in_offset=None, bounds_check=NE * CAP + 127, oob_is_err=False),
                  reads=[("h2b", t), dik] + xz_keys, writes=[key])
    if stage == "dbg":
        dbg_i = nc.dram_tensor("dbg_i", [128, 2 * NT], I32, kind="ExternalOutput").ap()
        dbg_g = nc.dram_tensor("dbg_g", [128, 2 * NT], F32, kind="ExternalOutput").ap()
        dbg_r = nc.dram_tensor("dbg_r", [128, NT * 32], F32, kind="ExternalOutput").ap()
        dbg_l = nc.dram_tensor("dbg_l", [128, NT * 36], F32, kind="ExternalOutput").ap()
        P.dma("sp", lambda e: e.dma_start(out=dbg_i[:, 0:NT], in_=d1i[:]), reads=["d1i"], writes=["dbg1"])
        P.dma("sp", lambda e: e.dma_start(out=dbg_i[:, NT:2 * NT], in_=d2i[:]), reads=["d2i"], writes=["dbg2"])
        P.dma("sp", lambda e: e.dma_start(out=dbg_g[:, 0:NT], in_=gt1[:]), reads=["gt1"], writes=["dbg3"])
        P.dma("sp", lambda e: e.dma_start(out=dbg_g[:, NT:2 * NT], in_=gt2[:]), reads=["gt2"], writes=["dbg4"])
        P.dma("sp", lambda e: e.dma_start(out=dbg_r, in_=r_rank[:].rearrange("p t k -> p (t k)")), reads=["r_rank"], writes=["dbg5"])
        P.dma("sp", lambda e: e.dma_start(out=dbg_l, in_=Lall[:].rearrange("p t k -> p (t k)")), reads=lall_keys, writes=["dbg6"])
        P.finish(["dbg1", "dbg2", "dbg3", "dbg4", "dbg5", "dbg6"])
    rt.close()
    P.barrier()

    ex = ExitStack()
    wg_s = sb(ex, "wg_s", [128, 3, 8, FF], BF16)
    wu_s = sb(ex, "wu_s", [128, 3, 8, FF], BF16)
    wd_s = sb(ex, "wd_s", [128, 3, 4, D], BF16)
    xb = sb(ex, "xb", [128, 2, 2, D], BF16)
    xbT = sb(ex, "xbT", [128, 2, 8, CAP], BF16)
    sil = sb(ex, "sil", [128, 4, CAP], F32)
    hmid = sb(ex, "hmid", [128, 2, 4, CAP], BF16)
    Yo = sb(ex, "Yo", [128, 2, D], BF16)
    fss = sb(ex, "fss", [128, NT], F32)

    def load_w(e_):
        par = e_ % 3
        P.dma("sp", lambda e: e.dma_start(out=wg_s[:, par], in_=Wg_b[e_].rearrange("(kc p) f -> p kc f", p=128)), reads=[("Wb", "g", e_)], writes=[("wg", par)])
        P.dma("sp", lambda e: e.dma_start(out=wu_s[:, par], in_=Wu_b[e_].rearrange("(kc p) f -> p kc f", p=128)), reads=[("Wb", "u", e_)], writes=[("wu", par)])
        P.dma("sp", lambda e: e.dma_start(out=wd_s[:, par], in_=Wd_b[e_].rearrange("(kc p) f -> p kc f", p=128)), reads=[("Wb", "d", e_)], writes=[("wd", par)])

    def load_xb(e_):
        par = e_ % 2
        P.dma("sp", lambda e: e.dma_start(out=xb[:, par], in_=Xs[e_ * CAP:(e_ + 1) * CAP, :].rearrange("(b p) d -> p b d", p=128)), reads=xs_keys, writes=[("xb", par)])

    ys_keys = []
    load_xb(0)
    load_w(0)
    load_w(1)

    def expert(e_):
        par = e_ % 2
        wp = e_ % 3
        if e_ + 1 < NE:
            load_xb(e_ + 1)
        for blk in range(2):
            for half in range(2):
                bt = bank()
                for q in range(4):
                    kc = half * 4 + q
                    P.op("pe", lambda e, blk=blk, kc=kc, q=q, bt=bt: e.matmul(psum[0:128, bt * 512 + q * 128: bt * 512 + (q + 1) * 128], lhsT=xb[:, par, blk, kc * 128:(kc + 1) * 128], rhs=ident_b[:], start=True, stop=True),
                         reads=[("xb", par), "ident_b"], writes=[pk(bt)])
                if half == 0:
                    P.op("act", lambda e, blk=blk, half=half, bt=bt: e.copy(out=xbT[:, par, half * 4:half * 4 + 4, blk * 128:(blk + 1) * 128], in_=psv(bt).rearrange("p (q c) -> p q c", c=128)),
                         reads=[pk(bt)], writes=[("xbT", par)])
                else:
                    P.op("dve", lambda e, blk=blk, half=half, bt=bt: e.tensor_copy(out=xbT[:, par, half * 4:half * 4 + 4, blk * 128:(blk + 1) * 128], in_=psv(bt).rearrange("p (q c) -> p q c", c=128)),
                         reads=[pk(bt)], writes=[("xbT", par)])
        yield
        ba_ = bank(2)
        for fc in range(4):
            for kc in range(8):
                P.op("pe", lambda e, fc=fc, kc=kc: e.matmul(psum[0:128, ba_ * 512 + fc * CAP: ba_ * 512 + (fc + 1) * CAP], lhsT=wg_s[:, wp, kc, fc * 128:(fc + 1) * 128], rhs=xbT[:, par, kc, :], start=(kc == 0), stop=(kc == 7)),
                     reads=[("wg", wp), ("xbT", par)], writes=[pk(ba_), pk(ba_ + 1)])
        P.op("act", lambda e: e.activation(out=sil[:].rearrange("p f c -> p (f c)"), in_=psum[0:128, ba_ * 512:(ba_ + 2) * 512], func=AF.Silu), reads=[pk(ba_), pk(ba_ + 1)], writes=["sil"])
        bb_ = bank(2)
        for fc in range(4):
            for kc in range(8):
                P.op("pe", lambda e, fc=fc, kc=kc: e.matmul(psum[0:128, bb_ * 512 + fc * CAP: bb_ * 512 + (fc + 1) * CAP], lhsT=wu_s[:, wp, kc, fc * 128:(fc + 1) * 128], rhs=xbT[:, par, kc, :], start=(kc == 0), stop=(kc == 7)),
                     reads=[("wu", wp), ("xbT", par)], writes=[pk(bb_), pk(bb_ + 1)])
        P.op("dve", lambda e: e.tensor_tensor(out=hmid[:, par].rearrange("p f c -> p (f c)"), in0=sil[:].rearrange("p f c -> p (f c)"), in1=psum[0:128, bb_ * 512:(bb_ + 2) * 512], op=ALU.mult),
             reads=["sil", pk(bb_), pk(bb_ + 1)], writes=[("hmid", par)])
        yield
        if e_ + 2 < NE:
            load_w(e_ + 2)
        for blk in range(2):
            for dh in range(2):
                bd = bank()
                for fc in range(4):
                    P.op("pe", lambda e, blk=blk, dh=dh, fc=fc, bd=bd: e.matmul(psv(bd), lhsT=hmid[:, par, fc, blk * 128:(blk + 1) * 128], rhs=wd_s[:, wp, fc, dh * 512:(dh + 1) * 512], start=(fc == 0), stop=(fc == 3)),
                         reads=[("hmid", par), ("wd", wp)], writes=[pk(bd)])
                if dh == 0:
                    P.op("act", lambda e, blk=blk, dh=dh, bd=bd: e.copy(out=Yo[:, blk, dh * 512:(dh + 1) * 512], in_=psv(bd)), reads=[pk(bd)], writes=[("Yo", blk, dh)])
                else:
                    P.op("dve", lambda e, blk=blk, dh=dh, bd=bd: e.tensor_copy(out=Yo[:, blk, dh * 512:(dh + 1) * 512], in_=psv(bd)), reads=[pk(bd)], writes=[("Yo", blk, dh)])
        key = ("Ys", e_)
        ys_keys.append(key)
        P.dma("sp", lambda e: e.dma_start(out=Ys[e_ * CAP:(e_ + 1) * CAP, :].rearrange("(b p) d -> p b d", p=128), in_=Yo[:]), reads=[("Yo", b_, d_) for b_ in range(2) for d_ in range(2)], writes=[key])

    gens_ = [expert(e_) for e_ in range(NE)]
    active_ = []
    nxt_ = 0
    while active_ or nxt_ < NE:
        if nxt_ < NE and len(active_) < 2:
            active_.append(gens_[nxt_])
            nxt_ += 1
        for g_ in list(active_):
            try:
                next(g_)
            except StopIteration:
                active_.remove(g_)

    G1 = sb(ex, "G1", [128, 2, D], BF16)
    G2 = sb(ex, "G2", [128, 2, D], BF16)
    fjunk = sb(ex, "fjunk", [128, D], F32)
    P.op("pool", lambda e: e.memset(fss[:], 0.0), writes=[("fss", t) for t in range(NT)])
    def comb_tile(t):
        sl = t % 2
        P.dma("pool", lambda e: e.indirect_dma_start(out=G1[:, sl, :], out_offset=None, in_=Ys, in_offset=bass.IndirectOffsetOnAxis(ap=d1i[:, t:t + 1], axis=0)), reads=ys_keys + ["d1i", ("Yz", 0)], writes=[("G1", sl)])
        P.dma("pool", lambda e: e.indirect_dma_start(out=G2[:, sl, :], out_offset=None, in_=Ys, in_offset=bass.IndirectOffsetOnAxis(ap=d2i[:, t:t + 1], axis=0)), reads=ys_keys + ["d2i", ("Yz", 0)], writes=[("G2", sl)])
        yield
        P.op("dve", lambda e: e.scalar_tensor_tensor(out=x1[:, t, :], in0=G1[:, sl, :], scalar=gt1[:, t:t + 1], in1=x1[:, t, :], op0=ALU.mult, op1=ALU.add), reads=[("G1", sl), "gt1", ("x1", t)], writes=[("x1", t)])
        P.op("dve", lambda e: e.scalar_tensor_tensor(out=x1[:, t, :], in0=G2[:, sl, :], scalar=gt2[:, t:t + 1], in1=x1[:, t, :], op0=ALU.mult, op1=ALU.add), reads=[("G2", sl), "gt2", ("x1", t)], writes=[("x1", t)])
        P.op("act", lambda e: e.activation(out=fjunk[:], in_=x1[:, t, :], func=AF.Square, accum_out=fss[:, t:t + 1]), reads=[("x1", t), ("fss", t)], writes=["fjunk", ("fss", t)])
        P.op("act", lambda e: e.activation(out=fss[:, t:t + 1], in_=fss[:, t:t + 1], func=AF.Sqrt, scale=1.0 / D, bias=EPS), reads=[("fss", t)], writes=[("fss", t)])
        yield
        P.op("dve", lambda e: e.reciprocal(out=fss[:, t:t + 1], in_=fss[:, t:t + 1]), reads=[("fss", t)], writes=[("fss", t)])
        P.op("dve", lambda e: e.scalar_tensor_tensor(out=x1[:, t, :], in0=x1[:, t, :], scalar=fss[:, t:t + 1], in1=g3bc[:], op0=ALU.mult, op1=ALU.mult), reads=[("x1", t), ("fss", t), "g3bc"], writes=[("x1", t)])
        P.dma("sp", lambda e: e.dma_start(out=out[t * 128:(t + 1) * 128, :], in_=x1[:, t, :]), reads=[("x1", t)], writes=[("out", t)])
        yield

    gens_ = [comb_tile(t) for t in range(NT)]
    active_ = []
    nxt_ = 0
    while active_ or nxt_ < NT:
        if nxt_ < NT and len(active_) < 2:
            active_.append(gens_[nxt_])
            nxt_ += 1
        for g_ in list(active_):
            try:
                next(g_)
            except StopIteration:
                active_.remove(g_)
    P.finish([("out", t) for t in range(NT)])
    P.emit()
    return nc


def make_inputs(inp, b):
    x = np.ascontiguousarray(inp["x"][b])
    w_in = inp["w_in"][0]
    m = {
        "x_tok": x,
        "xT": np.ascontiguousarray(x.T),
        "w_in_r": np.ascontiguousarray(w_in[:, :3584].reshape(8, 128, 28, 128).transpose(2, 1, 0, 3)),
        "w_ba": np.ascontiguousarray(w_in[:, 3584:3592].reshape(8, 128, 8).transpose(1, 0, 2)),
        "g1": np.ascontiguousarray(inp["mix_norm_g"][0].reshape(8, 128).T),
        "caw": np.ascontiguousarray(inp["conv_a_w"][0].reshape(3, 4, 128).transpose(2, 1, 0)),
        "gA": np.ascontiguousarray(inp["conv_a_norm_g"][0].reshape(4, 128).T),
        "dcw": np.ascontiguousarray(inp["dn_conv_w"][0].reshape(4, 12, 128).transpose(2, 1, 0)),
        "alog": np.ascontiguousarray(inp["dn_a_log"][0]),
        "dtb": np.ascontiguousarray(inp["dn_dt_bias"][0]),
        "gdn": np.ascontiguousarray(inp["dn_norm_g"][0].reshape(128, 1)),
        "w_out_r": np.ascontiguousarray(inp["w_out"][0].reshape(8, 128, D).transpose(1, 0, 2)),
        "g2": np.ascontiguousarray(inp["ffn_norm_g"][0]),
        "wr_r": np.ascontiguousarray(np.concatenate([inp["router_group_w"][0], inp["router_expert_w"][0]], axis=1).reshape(8, 128, 36).transpose(1, 0, 2)),
        "w_gate": np.ascontiguousarray(inp["w_gate"][0]),
        "w_up": np.ascontiguousarray(inp["w_up"][0]),
        "w_down": np.ascontiguousarray(inp["w_down"][0]),
        "g3": np.ascontiguousarray(inp["final_norm_g"]),
    }
    return m


def kernel(**inputs):
    inp = {k: np.asarray(v) for k, v in inputs.items()}
    nc = build("full")
    shared = make_inputs(inp, 0)
    in_maps = []
    for b in range(8):
        m = dict(shared)
        xb = np.ascontiguousarray(inp["x"][b])
        m["x_tok"] = xb
        m["xT"] = np.ascontiguousarray(xb.T)
        in_maps.append(m)
    res = run_bass_kernel_spmd(nc, in_maps, core_ids=list(range(8)))
    return np.stack([r["out"] for r in res.results], axis=0).astype(np.float32)
```

```python
from contextlib import ExitStack
import numpy as np
import concourse.bass as bass
import concourse.mybir as mybir
from concourse.bass_utils import run_bass_kernel_spmd

F32 = mybir.dt.float32
BF16 = mybir.dt.bfloat16
I32 = mybir.dt.int32
ALU = mybir.AluOpType
AF = mybir.ActivationFunctionType
AX = mybir.AxisListType

S = 2048
D = 1024
NT = 16
NCH = 32
NE = 32
FF = 512
CAP = 256
EPS = 1e-6
DN_DT = F32


class _Eng:
    def __init__(self, eng, sem, is_pe=False):
        self.eng = eng
        self.sem = sem
        self.count = 0
        self.waited = {}
        self.is_pe = is_pe
        self.rec = []


class Prog:
    def __init__(self, nc, n_dma_sems=10):
        self.nc = nc
        self.E = {
            "pe": _Eng(nc.tensor, nc.alloc_semaphore("s_pe"), True),
            "act": _Eng(nc.scalar, nc.alloc_semaphore("s_act")),
            "dve": _Eng(nc.vector, nc.alloc_semaphore("s_dve")),
            "pool": _Eng(nc.gpsimd, nc.alloc_semaphore("s_pool")),
            "sp": _Eng(nc.sync, nc.alloc_semaphore("s_sp")),
        }
        self.dma_sems = {}
        for q in ("sp", "pool", "act"):
            self.dma_sems[q] = [[nc.alloc_semaphore(f"d_{q}{i}"), 0] for i in range(n_dma_sems)]
        self.dma_rr = {"sp": 0, "pool": 0, "act": 0}
        self.rw = {}

    def _deps(self, reads, writes):
        deps = {}

        def add(tok):
            if tok is None:
                return
            s, v = tok
            if deps.get(s.num, (None, -1))[1] < v:
                deps[s.num] = (s, v)

        for k in reads:
            st = self.rw.get(k)
            if st is not None:
                add(st["w"])
        for k in writes:
            st = self.rw.get(k)
            if st is not None:
                add(st["w"])
                for t in st["r"].values():
                    add(t)
        return deps

    def _commit(self, tok, reads, writes):
        for k in writes:
            self.rw[k] = {"w": tok, "r": {}}
        for k in reads:
            st = self.rw.setdefault(k, {"w": None, "r": {}})
            s, v = tok
            if st["r"].get(s.num, (None, -1))[1] < v:
                st["r"][s.num] = tok

    def _wait(self, e, deps, skip_own=False):
        for num, (s, v) in deps.items():
            if skip_own and num == e.sem.num:
                continue
            if e.waited.get(num, 0) < v:
                e.rec.append(("w", s, v))
                e.waited[num] = v

    def op(self, en, fn, reads=(), writes=()):
        e = self.E[en]
        deps = self._deps(reads, writes)
        self._wait(e, deps, skip_own=e.is_pe)
        e.count += 1
        e.rec.append(("i", fn, e.sem, 1))
        tok = (e.sem, e.count)
        self._commit(tok, reads, writes)
        return tok

    def dma(self, q, fn, reads=(), writes=()):
        e = self.E[q]
        deps = self._deps(reads, writes)
        self._wait(e, deps)
        pool = self.dma_sems[q]
        i = self.dma_rr[q]
        self.dma_rr[q] = (i + 1) % len(pool)
        slot = pool[i]
        s, v = slot
        if v > 0 and e.waited.get(s.num, 0) < v:
            e.rec.append(("w", s, v))
            e.waited[s.num] = v
        e.rec.append(("i", fn, s, 16))
        slot[1] = v + 16
        tok = (s, v + 16)
        self._commit(tok, reads, writes)
        return tok

    def barrier(self):
        toks = []
        for e in self.E.values():
            if e.count > 0:
                toks.append((e.sem, e.count))
        for q in self.dma_sems.values():
            for s, v in q:
                if v > 0:
                    toks.append((s, v))
        for e in self.E.values():
            for s, v in toks:
                if s.num == e.sem.num:
                    continue
                if e.waited.get(s.num, 0) < v:
                    e.rec.append(("w", s, v))
                    e.waited[s.num] = v

    def finish(self, keys):
        e = self.E["sp"]
        self._wait(e, self._deps(keys, ()))

    def emit(self):
        nc = self.nc

        def replay(e):
            def f(eng):
                for r in e.rec:
                    if r[0] == "w":
                        eng.wait_ge(r[1], r[2])
                    else:
                        r[1](eng).then_inc(r[2], r[3])
            return f

        with nc.Block() as block:
            block.sync(replay(self.E["sp"]))
            block.scalar(replay(self.E["act"]))
            block.vector(replay(self.E["dve"]))
            block.gpsimd(replay(self.E["pool"]))
            block.tensor(replay(self.E["pe"]))


def interleave(gens):
    gens = list(gens)
    while gens:
        for g in list(gens):
            try:
                next(g)
            except StopIteration:
                gens.remove(g)


def build(stage="full"):
    nc = bass.Bass("TRN2", target_bir_lowering=False)
    P = Prog(nc)

    def din(name, shape, dt=F32):
        return nc.dram_tensor(name, list(shape), dt, kind="ExternalInput")

    x_tok = din("x_tok", [S, D]).ap()
    xT = din("xT", [D, S]).ap()
    w_in_r = din("w_in_r", [28, 128, 8, 128]).ap()
    w_ba = din("w_ba", [128, 8, 8]).ap()
    g1 = din("g1", [128, 8]).ap()
    caw = din("caw", [128, 4, 3]).ap()
    gA = din("gA", [128, 4]).ap()
    dcw = din("dcw", [128, 12, 4]).ap()
    alog_h = din("alog", [4])
    dtb_h = din("dtb", [4])
    gdn = din("gdn", [128, 1]).ap()
    w_out_r = din("w_out_r", [128, 8, D]).ap()
    g2_h = din("g2", [D])
    wr_r = din("wr_r", [128, 8, 36]).ap()
    w_gate = din("w_gate", [NE, D, FF]).ap()
    w_up = din("w_up", [NE, D, FF]).ap()
    w_down = din("w_down", [NE, FF, D]).ap()
    g3_h = din("g3", [D])
    out = nc.dram_tensor("out", [S, D], F32, kind="ExternalOutput").ap()

    DUMP = NE * CAP
    Xs = nc.dram_tensor("Xs_scr", [NE * CAP + 128, D], BF16, kind="Internal").ap()
    Ys = nc.dram_tensor("Ys_scr", [NE * CAP + 128, D], BF16, kind="Internal").ap()
    Wg_b = nc.dram_tensor("Wg_b", [NE, D, FF], BF16, kind="Internal").ap()
    Wu_b = nc.dram_tensor("Wu_b", [NE, D, FF], BF16, kind="Internal").ap()
    Wd_b = nc.dram_tensor("Wd_b", [NE, FF, D], BF16, kind="Internal").ap()
    stack0 = ExitStack()

    def sb(st, name, shape, dt, side=None):
        return st.enter_context(nc.sbuf_tensor(name, list(shape), dt, side=side))

    psum = nc.alloc_psum_tensor("psum", [128, 8 * 512], F32)
    bank_rr = [0]
    bank_lim = [8]

    def bank(n=1):
        b = bank_rr[0]
        if b + n > bank_lim[0]:
            b = 0
        bank_rr[0] = (b + n) % bank_lim[0]
        return b

    def psv(b, parts=128, n=512, nb=1):
        return psum[0:parts, b * 512:b * 512 + n] if nb == 1 else psum[0:parts, b * 512:(b + nb) * 512]

    def pk(b):
        return ("ps", b)

    ident_f = sb(stack0, "ident_f", [128, 128], F32)
    ident_b = sb(stack0, "ident_b", [128, 128], BF16)
    ones_f = sb(stack0, "ones_f", [128, 128], F32)
    ones_b = sb(stack0, "ones_b", [128, 128], BF16)
    bd64_b = sb(stack0, "bd64_b", [128, 128], BF16)
    bd64_f = sb(stack0, "bd64_f", [128, 128], F32)
    u64 = sb(stack0, "u64", [64, 64], F32)
    maskc8 = sb(stack0, "maskc8", [64, 8, 64], F32)
    masks8 = sb(stack0, "masks8", [64, 8, 64], F32)
    eye8 = sb(stack0, "eye8", [64, 8, 64], F32)

    P.op("pool", lambda e: e.memset(ident_f[:], 1.0), writes=["ident_f"])
    P.op("pool", lambda e: e.affine_select(out=ident_f[:], in_=ident_f[:], pattern=[[-1, 128]], compare_op=ALU.is_equal, fill=0.0, base=0, channel_multiplier=1), reads=["ident_f"], writes=["ident_f"])
    P.op("dve", lambda e: e.tensor_copy(out=ident_b[:], in_=ident_f[:]), reads=["ident_f"], writes=["ident_b"])
    P.op("pool", lambda e: e.memset(ones_f[:], 1.0), writes=["ones_f"])
    P.op("pool", lambda e: e.memset(ones_b[:], 1.0), writes=["ones_b"])
    P.op("pool", lambda e: e.memset(bd64_f[:], 1.0), writes=["bd64_f"])
    P.op("pool", lambda e: e.affine_select(out=bd64_f[:, 0:64], in_=bd64_f[:, 0:64], pattern=[[0, 64]], compare_op=ALU.is_ge, fill=0.0, base=63, channel_multiplier=-1), reads=["bd64_f"], writes=["bd64_f"])
    P.op("pool", lambda e: e.affine_select(out=bd64_f[:, 64:128], in_=bd64_f[:, 64:128], pattern=[[0, 64]], compare_op=ALU.is_ge, fill=0.0, base=-64, channel_multiplier=1), reads=["bd64_f"], writes=["bd64_f"])
    P.op("dve", lambda e: e.tensor_copy(out=bd64_b[:], in_=bd64_f[:]), reads=["bd64_f"], writes=["bd64_b"])
    P.op("pool", lambda e: e.memset(u64[:], 1.0), writes=["u64"])
    P.op("pool", lambda e: e.affine_select(out=u64[:], in_=u64[:], pattern=[[1, 64]], compare_op=ALU.is_ge, fill=0.0, base=0, channel_multiplier=-1), reads=["u64"], writes=["u64"])
    for t_, op_, nm in ((maskc8, ALU.is_ge, "maskc8"), (masks8, ALU.is_gt, "masks8"), (eye8, ALU.is_equal, "eye8")):
        P.op("pool", lambda e, t_=t_: e.memset(t_[:], 1.0), writes=[nm])
        P.op("pool", lambda e, t_=t_, op_=op_: e.affine_select(out=t_[:], in_=t_[:], pattern=[[0, 8], [-1, 64]], compare_op=op_, fill=0.0, base=0, channel_multiplier=1), reads=[nm], writes=[nm])

    g1_s = sb(stack0, "g1_s", [128, 8], F32)
    caw_s = sb(stack0, "caw_s", [128, 4, 3], F32)
    gA_s = sb(stack0, "gA_s", [128, 4], F32)
    dcw_s = sb(stack0, "dcw_s", [128, 12, 4], F32)
    gdn_s = sb(stack0, "gdn_s", [128, 1], F32)
    P.dma("sp", lambda e: e.dma_start(out=g1_s[:], in_=g1), writes=["g1_s"])
    P.dma("sp", lambda e: e.dma_start(out=caw_s[:], in_=caw), writes=["caw_s"])
    P.dma("sp", lambda e: e.dma_start(out=gA_s[:], in_=gA), writes=["gA_s"])
    P.dma("sp", lambda e: e.dma_start(out=dcw_s[:], in_=dcw), writes=["dcw_s"])
    P.dma("sp", lambda e: e.dma_start(out=gdn_s[:], in_=gdn), writes=["gdn_s"])

    stackR = ExitStack()
    yT = sb(stackR, "yT", [128, 8, S], BF16, side="right")

    stA = ExitStack()
    hT = sb(stA, "hT", [128, 8, S], BF16)
    wring = sb(stA, "wring", [128, 2, 8, 128], BF16)
    wba_s = sb(stA, "wba_s", [128, 8, 8], BF16)
    pc = sb(stA, "pc", [128, 4, S], F32)
    cvt = sb(stA, "cvt", [128, S], F32)
    sqb = sb(stA, "sqb", [128, 2, S], BF16)
    tb_ba = sb(stA, "tb_ba", [64, NCH, 8], F32)
    tb_alog = sb(stA, "tb_alog", [64, NCH, 4], F32)
    tb_dtb = sb(stA, "tb_dtb", [64, NCH, 4], F32)
    tb_beta = sb(stA, "tb_beta", [64, NCH, 4], F32)
    tb_nbeta = sb(stA, "tb_nbeta", [64, NCH, 4], F32)
    tb_t0 = sb(stA, "tb_t0", [64, NCH, 4], F32)
    tb_t1 = sb(stA, "tb_t1", [64, NCH, 4], F32)
    tb_g = sb(stA, "tb_g", [64, NCH, 4], F32)
    tb_gc = sb(stA, "tb_gc", [64, NCH, 4], F32)
    tb_gl = sb(stA, "tb_gl", [128, NCH, 4], F32)
    tb_egl = sb(stA, "tb_egl", [128, NCH, 4], F32)
    tb_kbe = sb(stA, "tb_kbe", [64, NCH, 4], F32)
    tb_kdec = sb(stA, "tb_kdec", [64, NCH, 4], F32)

    zt = sb(stA, "zt", [128, 2048], BF16)
    P.op("pool", lambda e: e.memset(zt[:], 0.0), writes=["zt"])
    xz_keys = []
    for i in range(NE):
        P.dma("sp", lambda e, i=i: e.dma_start(out=Xs[i * 256:(i + 1) * 256, :].rearrange("(p b) d -> p (b d)", b=2), in_=zt[:]), reads=["zt"], writes=[("Xz", i)])
        xz_keys.append(("Xz", i))
    P.dma("sp", lambda e: e.dma_start(out=Xs[DUMP:DUMP + 128, :], in_=zt[:, 0:D]), reads=["zt"], writes=[("Xz", NE)])
    xz_keys.append(("Xz", NE))
    P.dma("sp", lambda e: e.dma_start(out=Ys[DUMP:DUMP + 128, :], in_=zt[:, 0:D]), reads=["zt"], writes=[("Yz", 0)])
    P.dma("pool", lambda e: e.dma_start(out=wba_s[:], in_=w_ba), writes=["wba_s"])
    ab_s = sb(stA, "ab_s", [64, 2, 4], F32)
    P.dma("sp", lambda e: e.dma_start(out=ab_s[:, 0, :], in_=bass.AP(alog_h, 0, [[0, 64], [1, 4]])), writes=["ab_s0"])
    P.dma("sp", lambda e: e.dma_start(out=ab_s[:, 1, :], in_=bass.AP(dtb_h, 0, [[0, 64], [1, 4]])), writes=["ab_s1"])
    P.op("dve", lambda e: e.tensor_copy(out=tb_alog[:], in_=ab_s[:, 0:1, :].to_broadcast([64, NCH, 4])), reads=["ab_s0"], writes=["tb_alog"])
    P.op("dve", lambda e: e.tensor_copy(out=tb_dtb[:], in_=ab_s[:, 1:2, :].to_broadcast([64, NCH, 4])), reads=["ab_s1"], writes=["tb_dtb"])

    stA2 = ExitStack()
    xs = sb(stA2, "xs", [128, 2, S], F32)
    rbc = sb(stA2, "rbc", [128, S], F32)
    for kc in range(8):
        sl = kc % 2
        P.dma("sp", lambda e, kc=kc, sl=sl: e.dma_start(out=xs[:, sl, :], in_=xT[kc * 128:(kc + 1) * 128, :]), writes=[("xs", sl)])
        P.op("act", lambda e, sl=sl: e.activation(out=sqb[:, sl, :], in_=xs[:, sl, :], func=AF.Square), reads=[("xs", sl)], writes=[("sqb", sl)])
        for tb in range(4):
            P.op("pe", lambda e, kc=kc, sl=sl, tb=tb: e.matmul(psv(tb), lhsT=ones_b[:], rhs=sqb[:, sl, tb * 512:(tb + 1) * 512], start=(kc == 0), stop=(kc == 7)),
                 reads=[("sqb", sl), "ones_b"], writes=[pk(tb)])
    for tb in range(4):
        P.op("act", lambda e, tb=tb: e.activation(out=rbc[:, tb * 512:(tb + 1) * 512], in_=psv(tb), func=AF.Sqrt, scale=1.0 / D, bias=EPS), reads=[pk(tb)], writes=[("rbc", tb)])
        P.op("dve", lambda e, tb=tb: e.reciprocal(out=rbc[:, tb * 512:(tb + 1) * 512], in_=rbc[:, tb * 512:(tb + 1) * 512]), reads=[("rbc", tb)], writes=[("rbc", tb)])
    for kc in range(8):
        sl = kc % 2
        P.dma("sp", lambda e, kc=kc, sl=sl: e.dma_start(out=xs[:, sl, :], in_=xT[kc * 128:(kc + 1) * 128, :]), writes=[("xs", sl)])
        P.op("dve", lambda e, kc=kc, sl=sl: e.scalar_tensor_tensor(out=hT[:, kc, :], in0=xs[:, sl, :], scalar=g1_s[:, kc:kc + 1], in1=rbc[:], op0=ALU.mult, op1=ALU.mult),
             reads=[("xs", sl), "g1_s"] + [("rbc", tb) for tb in range(4)], writes=[("hT", kc)])
    stA2.close()
    hT_keys = [("hT", kc) for kc in range(8)]

    pre_list = []
    for e_ in range(NE):
        pre_list.append((Wg_b, w_gate, "g", e_))
        pre_list.append((Wu_b, w_up, "u", e_))
        pre_list.append((Wd_b, w_down, "d", e_))
    pre_i = [0]

    def prestage(n):
        for _ in range(n):
            if pre_i[0] >= len(pre_list):
                return
            dst, src, kd, e_ = pre_list[pre_i[0]]
            pre_i[0] += 1
            P.dma("pool", lambda e, dst=dst, src=src, e_=e_: e.dma_start(out=dst[e_], in_=src[e_]), writes=[("Wb", kd, e_)])

    wr_i = [0]

    def proj_chunk(c, slot, dst=None, dname="pc"):
        if dst is None:
            dst = pc
        ws = wr_i[0] % 2
        wr_i[0] += 1
        P.dma("pool", lambda e: e.dma_start(out=wring[:, ws, :, :], in_=w_in_r[c]), writes=[("wring", ws)])
        if c < 12:
            prestage(1)
        for tb in range(4):
            b = bank()
            for kc in range(8):
                P.op("pe", lambda e, kc=kc, tb=tb, b=b: e.matmul(psv(b), lhsT=wring[:, ws, kc, :], rhs=hT[:, kc, tb * 512:(tb + 1) * 512], start=(kc == 0), stop=(kc == 7)),
                     reads=[("wring", ws), ("hT", kc)], writes=[pk(b)])
            P.op("act", lambda e, tb=tb, b=b: e.copy(out=dst[:, slot, tb * 512:(tb + 1) * 512], in_=psv(b)), reads=[pk(b)], writes=[(dname, slot, tb)])

    def pck(slot):
        return [("pc", slot, tb) for tb in range(4)]

    bb = bank()
    for c in range(NCH):
        for kc in range(8):
            P.op("pe", lambda e, c=c, kc=kc: e.matmul(psum[0:64, bb * 512 + c * 8: bb * 512 + c * 8 + 8], lhsT=hT[:, kc, c * 64:(c + 1) * 64], rhs=wba_s[:, kc, :], start=(kc == 0), stop=(kc == 7)),
                 reads=[("hT", kc), "wba_s"], writes=[pk(bb)])
    P.op("act", lambda e: e.copy(out=tb_ba[:].rearrange("p c k -> p (c k)"), in_=psum[0:64, bb * 512: bb * 512 + 256]), reads=[pk(bb)], writes=["tb_ba"])
    P.op("act", lambda e: e.activation(out=tb_beta[:], in_=tb_ba[:, :, 0:4], func=AF.Sigmoid), reads=["tb_ba"], writes=["tb_beta"])
    P.op("dve", lambda e: e.tensor_scalar(out=tb_nbeta[:], in0=tb_beta[:], scalar1=-1.0, scalar2=None, op0=ALU.mult), reads=["tb_beta"], writes=["tb_nbeta"])
    P.op("dve", lambda e: e.tensor_tensor(out=tb_t0[:], in0=tb_ba[:, :, 4:8], in1=tb_dtb[:], op=ALU.add), reads=["tb_ba", "tb_dtb"], writes=["tb_t0"])
    P.op("act", lambda e: e.activation(out=tb_t1[:], in_=tb_t0[:], func=AF.Abs), reads=["tb_t0"], writes=["tb_t1"])
    P.op("act", lambda e: e.activation(out=tb_t1[:], in_=tb_t1[:], func=AF.Exp, scale=-1.0), reads=["tb_t1"], writes=["tb_t1"])
    P.op("act", lambda e: e.activation(out=tb_t1[:], in_=tb_t1[:], func=AF.Ln, bias=1.0), reads=["tb_t1"], writes=["tb_t1"])
    P.op("dve", lambda e: e.tensor_scalar(out=tb_t0[:], in0=tb_t0[:], scalar1=0.0, scalar2=None, op0=ALU.max), reads=["tb_t0"], writes=["tb_t0"])
    P.op("dve", lambda e: e.tensor_tensor(out=tb_t0[:], in0=tb_t0[:], in1=tb_t1[:], op=ALU.add), reads=["tb_t0", "tb_t1"], writes=["tb_t0"])
    P.op("act", lambda e: e.activation(out=tb_alog[:], in_=tb_alog[:], func=AF.Exp), reads=["tb_alog"], writes=["tb_alog"])
    P.op("dve", lambda e: e.scalar_tensor_tensor(out=tb_g[:], in0=tb_t0[:], scalar=-1.0, in1=tb_alog[:], op0=ALU.mult, op1=ALU.mult), reads=["tb_t0", "tb_alog"], writes=["tb_g"])
    gflat = tb_g[:].rearrange("p c k -> p (c k)")
    b1 = bank()
    P.op("pe", lambda e: e.matmul(psum[0:64, b1 * 512:b1 * 512 + 128], lhsT=u64[:], rhs=gflat, start=True, stop=True), reads=["u64", "tb_g"], writes=[pk(b1)])
    P.op("act", lambda e: e.copy(out=tb_gc[:].rearrange("p c k -> p (c k)"), in_=psum[0:64, b1 * 512:b1 * 512 + 128]), reads=[pk(b1)], writes=["tb_gc"])
    b2 = bank()
    P.op("pe", lambda e: e.matmul(psum[0:128, b2 * 512:b2 * 512 + 128], lhsT=ones_f[0:64, :], rhs=gflat, start=True, stop=True), reads=["ones_f", "tb_g"], writes=[pk(b2)])
    P.op("act", lambda e: e.copy(out=tb_gl[:].rearrange("p c k -> p (c k)"), in_=psum[0:128, b2 * 512:b2 * 512 + 128]), reads=[pk(b2)], writes=["tb_gl"])
    P.op("act", lambda e: e.activation(out=tb_egl[:], in_=tb_gl[:], func=AF.Exp), reads=["tb_gl"], writes=["tb_egl"])
    P.op("act", lambda e: e.activation(out=tb_t1[:], in_=tb_gc[:], func=AF.Exp), reads=["tb_gc"], writes=["tb_t1"])
    P.op("dve", lambda e: e.tensor_tensor(out=tb_kbe[:], in0=tb_beta[:], in1=tb_t1[:], op=ALU.mult), reads=["tb_beta", "tb_t1"], writes=["tb_kbe"])
    P.op("dve", lambda e: e.tensor_tensor(out=tb_kdec[:], in0=tb_gl[0:64], in1=tb_gc[:], op=ALU.subtract), reads=["tb_gl", "tb_gc"], writes=["tb_kdec"])
    P.op("act", lambda e: e.activation(out=tb_kdec[:], in_=tb_kdec[:], func=AF.Exp), reads=["tb_kdec"], writes=["tb_kdec"])

    def conv(eng, dst, dkeys, src, skeys, wtile, wkey, widx, K):
        P.op("act", lambda e: e.activation(out=dst, in_=src, func=AF.Copy, scale=wtile[:, widx, K - 1:K]), reads=skeys + [wkey], writes=dkeys)
        for j in range(K - 1):
            sh = K - 1 - j
            P.op("dve", lambda e, j=j, sh=sh: e.scalar_tensor_tensor(out=dst[:, sh:], in0=src[:, 0:S - sh], scalar=wtile[:, widx, j:j + 1], in1=dst[:, sh:], op0=ALU.mult, op1=ALU.add),
                 reads=skeys + [wkey] + dkeys, writes=dkeys)

    sq_i = [0]

    def inv_rms(src, skeys, lhs, lkey, scale):
        sl = sq_i[0] % 2
        sq_i[0] += 1
        P.op("act", lambda e: e.activation(out=sqb[:, sl, :], in_=src, func=AF.Square), reads=skeys, writes=[("sqb", sl)])
        for tb in range(4):
            b = bank()
            P.op("pe", lambda e, tb=tb, b=b: e.matmul(psv(b), lhsT=lhs, rhs=sqb[:, sl, tb * 512:(tb + 1) * 512], start=True, stop=True), reads=[("sqb", sl), lkey], writes=[pk(b)])
            P.op("act", lambda e, tb=tb, b=b: e.activation(out=cvt[:, tb * 512:(tb + 1) * 512], in_=psv(b), func=AF.Ln, scale=scale, bias=EPS), reads=[pk(b)], writes=["cvt"])
        P.op("act", lambda e: e.activation(out=cvt[:], in_=cvt[:], func=AF.Exp, scale=-0.5), reads=["cvt"], writes=["cvt"])

    stMA = ExitStack()
    pcx = sb(stMA, "pcx", [128, 3, S], F32)

    def mixer_unit(j):
        buf, nm = (pc, "pc") if j % 2 == 0 else (pcx, "pcx")

        def kk(i):
            return [(nm, i, tb) for tb in range(4)]

        proj_chunk(j, 0, buf, nm)
        yield
        proj_chunk(8 + j, 2, buf, nm)
        yield
        proj_chunk(4 + j, 1, buf, nm)
        yield
        P.op("dve", lambda e: e.tensor_tensor(out=buf[:, 0, :], in0=buf[:, 0, :], in1=buf[:, 2, :], op=ALU.mult), reads=kk(0) + kk(2), writes=kk(0))
        conv("pool", buf[:, 2, :], kk(2), buf[:, 0, :], kk(0), caw_s, "caw_s", j, 3)
        yield
        P.op("dve", lambda e: e.tensor_tensor(out=buf[:, 2, :], in0=buf[:, 2, :], in1=buf[:, 1, :], op=ALU.mult), reads=kk(1) + kk(2), writes=kk(2))
        inv_rms(buf[:, 2, :], kk(2), bd64_b[:], "bd64_b", 1.0 / 64)
        yield
        P.op("dve", lambda e: e.scalar_tensor_tensor(out=yT[:, j, :], in0=buf[:, 2, :], scalar=gA_s[:, j:j + 1], in1=cvt[:], op0=ALU.mult, op1=ALU.mult),
             reads=kk(2) + ["gA_s", "cvt"], writes=[("yT", j)])

    gens_ = [mixer_unit(j) for j in range(4)]
    active_ = []
    nxt_ = 0
    rnd_ = 0
    while active_ or nxt_ < 4:
        if nxt_ < 4 and len(active_) < 2 and (not active_ or rnd_ >= 3):
            active_.append(gens_[nxt_])
            nxt_ += 1
            rnd_ = 0
        rnd_ += 1
        for g_ in list(active_):
            try:
                next(g_)
            except StopIteration:
                active_.remove(g_)
    stMA.close()
    P.barrier()

    dn = ExitStack()
    GW = 8
    NSET = 2
    qb = sb(dn, "qb", [128, S], BF16)
    kb = sb(dn, "kb", [128, S], BF16)
    vb = sb(dn, "vb", [128, S], BF16)
    NPAR = 3
    ATg = sb(dn, "ATg", [64, NPAR, GW, 64], BF16)
    Kdg = sb(dn, "Kdg", [64, NPAR, GW, 128], BF16)
    Ug = sb(dn, "Ug", [64, NPAR, GW, 128], F32)
    WTg = sb(dn, "WTg", [128, NPAR, GW * 64], BF16)
    qsb = sb(dn, "qsb", [128, S], BF16)
    Sst = sb(dn, "Sst", [128, 2, 128], F32)
    Sb = sb(dn, "Sb", [128, 2, 128], BF16)
    vnew = sb(dn, "vnew", [64, 2, 128], BF16)
    Og = sb(dn, "Og", [64, 4, 128], F32)
    Ogb = sb(dn, "Ogb", [64, 4, 128], BF16)
    Osq = sb(dn, "Osq", [64, 4, 128], F32)
    oss = sb(dn, "oss", [64, 4], F32)

    SC = []
    s0 = {"id": 0}
    s0["GU"] = sb(dn, "s0_GU", [64, GW, 64], F32)[:]
    s0["E"] = sb(dn, "s0_E", [128, GW * 64], F32)[:]
    for nm in ("DS", "DC", "A", "N0", "B0", "N1", "B1", "R0", "R1"):
        s0[nm] = sb(dn, "s0_" + nm, [64, GW, 64], BF16)[:]
    s0["Kbe"] = sb(dn, "s0_Kbe", [64, GW, 128], BF16)[:]
    s0["Vb"] = sb(dn, "s0_Vb", [64, GW, 128], BF16)[:]
    SC.append(s0)

    def bfv(ap, c):
        return ap.bitcast(BF16).rearrange("p (a c) -> p a c", c=c)

    s1 = {"id": 1}
    s1["GU"] = pc[0:64, 1, 0:512].rearrange("p (a c) -> p a c", c=64)
    s1["E"] = pc[:, 1, 512:1024]
    for i_, nm in enumerate(("DS", "DC", "A", "N0")):
        s1[nm] = bfv(pc[0:64, 1, 1024 + 256 * i_:1280 + 256 * i_], 64)
    for i_, nm in enumerate(("B0", "N1", "B1", "R0", "R1")):
        s1[nm] = bfv(pc[0:64, 2, 256 * i_:256 * (i_ + 1)], 64)
    s1["Kbe"] = bfv(pc[0:64, 2, 1280:1792], 128)
    s1["Vb"] = bfv(cvt[0:64, 0:512], 128)
    SC.append(s1)

    def ops512(v):
        return v.rearrange("p a c -> p (a c)")

    def dn_phase1(h, cg, par, Sx):
        sid = Sx["id"]

        def K(nm):
            ks_ = [(nm, sid)]
            if sid == 1:
                ks_.append("alias1")
            return ks_

        def KW(nm):
            return [(nm, sid)]

        qs = pc[:, 0, :]
        c0 = cg * GW
        cols = slice(c0 * 64, (c0 + GW) * 64)
        qk = [("pc", 0, cg)]
        GU, DS, DC, A, E_s, Kbe, Vb = Sx["GU"], Sx["DS"], Sx["DC"], Sx["A"], Sx["E"], Sx["Kbe"], Sx["Vb"]
        al = ["alias1"] if sid == 1 else []
        P.op("pool", lambda e: e.tensor_tensor(out=GU[:], in0=u64[:, None, :].to_broadcast([64, GW, 64]), in1=tb_g[:, c0:c0 + GW, h:h + 1].to_broadcast([64, GW, 64]), op=ALU.mult),
             reads=["u64", "tb_g"] + al, writes=KW("GU"))
        bg = bank()
        P.op("pe", lambda e: e.matmul(psv(bg), lhsT=ones_f[0:64, :], rhs=ops512(GU[:]), start=True, stop=True), reads=["ones_f"] + K("GU"), writes=[pk(bg)])
        P.op("dve", lambda e: e.tensor_tensor(out=GU[:], in0=psv(bg, 64).rearrange("p (a c) -> p a c", c=64), in1=tb_gc[:, c0:c0 + GW, h:h + 1].to_broadcast([64, GW, 64]), op=ALU.subtract),
             reads=[pk(bg), "tb_gc"] + al, writes=KW("GU"))
        P.op("dve", lambda e: e.tensor_scalar(out=GU[:], in0=GU[:], scalar1=0.0, scalar2=None, op0=ALU.max), reads=K("GU"), writes=KW("GU"))
        P.op("act", lambda e: e.activation(out=GU[:], in_=GU[:], func=AF.Exp, scale=-1.0), reads=K("GU"), writes=KW("GU"))
        P.op("act", lambda e: e.activation(out=E_s[:], in_=psv(bg), func=AF.Exp), reads=[pk(bg)] + al, writes=KW("E"))
        yield
        P.op("pool", lambda e: e.tensor_tensor(out=DS[:], in0=GU[:], in1=masks8[:], op=ALU.mult), reads=K("GU") + ["masks8"], writes=KW("DS"))
        P.op("pool", lambda e: e.tensor_tensor(out=DS[:], in0=DS[:], in1=tb_nbeta[:, c0:c0 + GW, h:h + 1].to_broadcast([64, GW, 64]), op=ALU.mult), reads=K("DS") + ["tb_nbeta"], writes=KW("DS"))
        P.op("pool", lambda e: e.tensor_tensor(out=DC[:], in0=GU[:], in1=maskc8[:], op=ALU.mult), reads=K("GU") + ["maskc8"], writes=KW("DC"))
        N0, B0 = Sx["N0"], Sx["B0"]
        bk = bank()
        for a in range(GW):
            cs = slice((c0 + a) * 64, (c0 + a + 1) * 64)
            P.op("pe", lambda e, a=a, cs=cs: e.matmul(psum[0:64, bk * 512 + a * 64: bk * 512 + (a + 1) * 64], lhsT=kb[:, cs], rhs=kb[:, cs], start=True, stop=True), reads=["kb"], writes=[pk(bk)])
        P.op("dve", lambda e: e.tensor_tensor(out=ops512(N0[:]), in0=psv(bk, 64), in1=ops512(DS[:]), op=ALU.mult), reads=[pk(bk)] + K("DS"), writes=KW("N0"))
        yield
        bq = bank()
        for a in range(GW):
            cs = slice((c0 + a) * 64, (c0 + a + 1) * 64)
            P.op("pe", lambda e, a=a, cs=cs: e.matmul(psum[0:64, bq * 512 + a * 64: bq * 512 + (a + 1) * 64], lhsT=qb[:, cs], rhs=kb[:, cs], start=True, stop=True), reads=["qb", "kb"], writes=[pk(bq)])
        P.op("dve", lambda e: e.tensor_tensor(out=ops512(A[:]), in0=psv(bq, 64), in1=ops512(DC[:]), op=ALU.mult), reads=[pk(bq)] + K("DC"), writes=KW("A"))
        P.op("pool", lambda e: e.tensor_tensor(out=qsb[:, cols], in0=qs[:, cols], in1=E_s[:], op=ALU.mult), reads=qk + K("E"), writes=[("qsb", cg)])
        yield
        bt = bank()
        for a in range(GW):
            P.op("pe", lambda e, a=a: e.matmul(psum[0:64, bt * 512 + a * 64: bt * 512 + (a + 1) * 64], lhsT=N0[:, a, :], rhs=ident_b[0:64, 0:64], start=True, stop=True), reads=K("N0") + ["ident_b"], writes=[pk(bt)])
        P.op("act", lambda e: e.copy(out=ops512(B0[:]), in_=psv(bt, 64)), reads=[pk(bt)] + al, writes=KW("B0"))
        yield
        ba_ = bank()
        for a in range(GW):
            P.op("pe", lambda e, a=a: e.matmul(psum[0:64, ba_ * 512 + a * 64: ba_ * 512 + (a + 1) * 64], lhsT=A[:, a, :], rhs=ident_b[0:64, 0:64], start=True, stop=True), reads=K("A") + ["ident_b"], writes=[pk(ba_)])
        P.op("act", lambda e: e.copy(out=ops512(ATg[:, par]), in_=psv(ba_, 64)), reads=[pk(ba_)], writes=[("ATg", par)])
        R = [Sx["R0"], Sx["R1"]]
        Nn = [Sx["N0"], Sx["N1"]]
        Bn = [Sx["B0"], Sx["B1"]]
        P.op("pool", lambda e: e.tensor_tensor(out=R[0][:], in0=B0[:], in1=eye8[:], op=ALU.add), reads=K("B0") + ["eye8"], writes=KW("R0"))
        yield
        cur = 0
        for lvl in range(5):
            nxt = 1 - cur
            nk, bkk = "N%d" % cur, "B%d" % cur
            nk2, bk2 = "N%d" % nxt, "B%d" % nxt
            rk, rk2 = "R%d" % cur, "R%d" % nxt
            pn = bank()
            for a in range(GW):
                P.op("pe", lambda e, a=a, cur=cur, pn=pn: e.matmul(psum[0:64, pn * 512 + a * 64: pn * 512 + (a + 1) * 64], lhsT=Bn[cur][:, a, :], rhs=Nn[cur][:, a, :], start=True, stop=True),
                     reads=K(nk) + K(bkk), writes=[pk(pn)])
            P.op("act", lambda e, nxt=nxt, pn=pn: e.copy(out=ops512(Nn[nxt][:]), in_=psv(pn, 64)), reads=[pk(pn)] + al, writes=KW(nk2))
            yield
            if lvl < 4:
                pb = bank()
                for a in range(GW):
                    P.op("pe", lambda e, a=a, cur=cur, pb=pb: e.matmul(psum[0:64, pb * 512 + a * 64: pb * 512 + (a + 1) * 64], lhsT=Nn[cur][:, a, :], rhs=Bn[cur][:, a, :], start=True, stop=True),
                         reads=K(nk) + K(bkk), writes=[pk(pb)])
                P.op("act", lambda e, nxt=nxt, pb=pb: e.copy(out=ops512(Bn[nxt][:]), in_=psv(pb, 64)), reads=[pk(pb)] + al, writes=KW(bk2))
                yield
            pr = bank()
            for a in range(GW):
                P.op("pe", lambda e, a=a, cur=cur, nxt=nxt, pr=pr: e.matmul(psum[0:64, pr * 512 + a * 64: pr * 512 + (a + 1) * 64], lhsT=Nn[nxt][:, a, :], rhs=R[cur][:, a, :], start=True, stop=True),
                     reads=K(nk2) + K(rk), writes=[pk(pr)])
            P.op("dve", lambda e, cur=cur, nxt=nxt, pr=pr: e.tensor_tensor(out=ops512(R[nxt][:]), in0=psv(pr, 64), in1=ops512(R[cur][:]), op=ALU.add), reads=[pk(pr)] + K(rk), writes=KW(rk2))
            cur = nxt
            yield
        Rf = R[cur]
        rfk = "R%d" % cur
        bkt = bank(2)
        for a in range(GW):
            cs = slice((c0 + a) * 64, (c0 + a + 1) * 64)
            P.op("pe", lambda e, a=a, cs=cs: e.matmul(psum[0:64, bkt * 512 + a * 128: bkt * 512 + (a + 1) * 128], lhsT=kb[:, cs], rhs=ident_b[:], start=True, stop=True), reads=["kb", "ident_b"], writes=[pk(bkt), pk(bkt + 1)])
        kt3 = psum[0:64, bkt * 512:(bkt + 2) * 512].rearrange("p (a c) -> p a c", c=128)
        P.op("dve", lambda e: e.tensor_tensor(out=Kbe[:], in0=kt3, in1=tb_kbe[:, c0:c0 + GW, h:h + 1].to_broadcast([64, GW, 128]), op=ALU.mult), reads=[pk(bkt), pk(bkt + 1), "tb_kbe"] + al, writes=KW("Kbe"))
        P.op("dve", lambda e: e.tensor_tensor(out=Kdg[:, par], in0=kt3, in1=tb_kdec[:, c0:c0 + GW, h:h + 1].to_broadcast([64, GW, 128]), op=ALU.mult), reads=[pk(bkt), pk(bkt + 1), "tb_kdec"], writes=[("Kdg", par)])
        yield
        bvt = bank(2)
        for a in range(GW):
            cs = slice((c0 + a) * 64, (c0 + a + 1) * 64)
            P.op("pe", lambda e, a=a, cs=cs: e.matmul(psum[0:64, bvt * 512 + a * 128: bvt * 512 + (a + 1) * 128], lhsT=vb[:, cs], rhs=ident_b[:], start=True, stop=True), reads=["vb", "ident_b"], writes=[pk(bvt), pk(bvt + 1)])
        vt3 = psum[0:64, bvt * 512:(bvt + 2) * 512].rearrange("p (a c) -> p a c", c=128)
        P.op("dve", lambda e: e.tensor_tensor(out=Vb[:], in0=vt3, in1=tb_beta[:, c0:c0 + GW, h:h + 1].to_broadcast([64, GW, 128]), op=ALU.mult), reads=[pk(bvt), pk(bvt + 1), "tb_beta"] + al, writes=KW("Vb"))
        yield
        bu = bank(2)
        for a in range(GW):
            P.op("pe", lambda e, a=a: e.matmul(psum[0:64, bu * 512 + a * 128: bu * 512 + (a + 1) * 128], lhsT=Rf[:, a, :], rhs=Vb[:, a, :], start=True, stop=True), reads=K(rfk) + K("Vb"), writes=[pk(bu), pk(bu + 1)])
        P.op("act", lambda e: e.copy(out=Ug[:, par].rearrange("p a c -> p (a c)"), in_=psum[0:64, bu * 512:(bu + 2) * 512]), reads=[pk(bu), pk(bu + 1)], writes=[("Ug", par)])
        yield
        bw = bank()
        for a in range(GW):
            P.op("pe", lambda e, a=a: e.matmul(psum[0:128, bw * 512 + a * 64: bw * 512 + (a + 1) * 64], lhsT=Kbe[:, a, :], rhs=Rf[:, a, :], start=True, stop=True), reads=K(rfk) + K("Kbe"), writes=[pk(bw)])
        P.op("act", lambda e: e.copy(out=WTg[:, par, :], in_=psv(bw)), reads=[pk(bw)], writes=[("WTg", par)])
        yield

    s_par = [0]

    def dn_scan(h, cg, par):
        zs = pc[:, 3, :]
        c0 = cg * GW
        for a in range(GW):
            c = c0 + a
            cs = slice(c * 64, (c + 1) * 64)
            sp_, sn_ = s_par[0], 1 - s_par[0]
            vp = a % 2
            bw = bank()
            P.op("pe", lambda e, a=a, sp_=sp_, bw=bw: e.matmul(psum[0:64, bw * 512: bw * 512 + 128], lhsT=WTg[:, par, a * 64:(a + 1) * 64], rhs=Sb[:, sp_, :], start=True, stop=True),
                 reads=[("WTg", par), ("Sb", sp_)], writes=[pk(bw)])
            P.op("dve", lambda e, a=a, vp=vp, bw=bw: e.tensor_tensor(out=vnew[:, vp, :], in0=Ug[:, par, a, :], in1=psum[0:64, bw * 512: bw * 512 + 128], op=ALU.subtract),
                 reads=[("Ug", par), pk(bw)], writes=[("vnew", vp)])
            yield
            bs = bank()
            P.op("pe", lambda e, a=a, vp=vp, bs=bs: e.matmul(psum[0:128, bs * 512: bs * 512 + 128], lhsT=Kdg[:, par, a, :], rhs=vnew[:, vp, :], start=True, stop=True),
                 reads=[("Kdg", par), ("vnew", vp)], writes=[pk(bs)])
            oq = a % 4
            bo = 6 + ((c // 4) % 2)
            P.op("pe", lambda e, cs=cs, sp_=sp_, bo=bo, oq=oq: e.matmul(psum[0:64, bo * 512 + oq * 128: bo * 512 + (oq + 1) * 128], lhsT=qsb[:, cs], rhs=Sb[:, sp_, :], start=True, stop=False),
                 reads=[("qsb", cg), ("Sb", sp_)], writes=[pk(bo)])
            P.op("pe", lambda e, a=a, vp=vp, bo=bo, oq=oq: e.matmul(psum[0:64, bo * 512 + oq * 128: bo * 512 + (oq + 1) * 128], lhsT=ATg[:, par, a, :], rhs=vnew[:, vp, :], start=False, stop=True),
                 reads=[("ATg", par), ("vnew", vp)], writes=[pk(bo)])
            P.op("dve", lambda e, c=c, sp_=sp_, sn_=sn_, bs=bs: e.scalar_tensor_tensor(out=Sb[:, sn_, :], in0=Sst[:, sp_, :], scalar=tb_egl[:, c, h:h + 1], in1=psum[0:128, bs * 512: bs * 512 + 128], op0=ALU.mult, op1=ALU.add),
                 reads=[("S", sp_), "tb_egl", pk(bs)], writes=[("Sb", sn_)])
            P.op("dve", lambda e, c=c, sp_=sp_, sn_=sn_, bs=bs: e.scalar_tensor_tensor(out=Sst[:, sn_, :], in0=Sst[:, sp_, :], scalar=tb_egl[:, c, h:h + 1], in1=psum[0:128, bs * 512: bs * 512 + 128], op0=ALU.mult, op1=ALU.add),
                 reads=[("S", sp_), "tb_egl", pk(bs)], writes=[("S", sn_)])
            s_par[0] = sn_
            if oq == 3:
                c4 = c - 3
                P.op("act", lambda e, bo=bo: e.copy(out=Og[:].rearrange("p a c -> p (a c)"), in_=psv(bo, 64)), reads=[pk(bo)], writes=["Og"])
                P.op("pool", lambda e: e.tensor_tensor(out=Osq[:], in0=Og[:], in1=Og[:], op=ALU.mult), reads=["Og"], writes=["Osq"])
                P.op("dve", lambda e: e.tensor_reduce(out=oss[:], in_=Osq[:], axis=AX.X, op=ALU.add), reads=["Osq"], writes=["oss"])
                P.op("act", lambda e: e.activation(out=oss[:], in_=oss[:], func=AF.Sqrt, scale=1.0 / 128, bias=EPS), reads=["oss"], writes=["oss"])
                P.op("dve", lambda e: e.reciprocal(out=oss[:], in_=oss[:]), reads=["oss"], writes=["oss"])
                P.op("pool", lambda e: e.tensor_tensor(out=Ogb[:], in0=Og[:], in1=oss[:, :, None].to_broadcast([64, 4, 128]), op=ALU.mult), reads=["Og", "oss"], writes=["Ogb"])
                bt = bank()
                for q4 in range(4):
                    P.op("pe", lambda e, q4=q4, bt=bt: e.matmul(psum[0:128, bt * 512 + q4 * 64: bt * 512 + (q4 + 1) * 64], lhsT=Ogb[:, q4, :], rhs=ident_b[0:64, 0:64], start=True, stop=True), reads=["Ogb", "ident_b"], writes=[pk(bt)])
                P.op("dve", lambda e, c4=c4, bt=bt: e.tensor_tensor(out=yT[:, 4 + h, c4 * 64:(c4 + 4) * 64], in0=psum[0:128, bt * 512: bt * 512 + 256], in1=zs[:, c4 * 64:(c4 + 4) * 64], op=ALU.mult),
                     reads=[pk(bt), ("pc", 3, cg)], writes=[("yT", 4 + h)])
            yield

    for h in range(4):
        bank_lim[0] = 8
        proj_chunk(12 + h, 0)
        proj_chunk(16 + h, 1)
        proj_chunk(20 + h, 2)
        proj_chunk(24 + h, 3)
        for s_, m_ in ((0, h), (1, 4 + h), (2, 8 + h)):
            conv("pool", cvt[:], ["cvt"], pc[:, s_, :], pck(s_), dcw_s, "dcw_s", m_, 4)
            if s_ == 2:
                P.op("act", lambda e: e.activation(out=vb[:], in_=cvt[:], func=AF.Silu), reads=["cvt"], writes=["vb"])
            else:
                P.op("act", lambda e, s_=s_: e.activation(out=pc[:, s_, :], in_=cvt[:], func=AF.Silu), reads=["cvt"], writes=pck(s_))
        inv_rms(pc[:, 0, :], pck(0), ones_b[:], "ones_b", 1.0)
        P.op("dve", lambda e: e.scalar_tensor_tensor(out=pc[:, 0, :], in0=pc[:, 0, :], scalar=128 ** -0.5, in1=cvt[:], op0=ALU.mult, op1=ALU.mult), reads=pck(0) + ["cvt"], writes=pck(0))
        P.op("act", lambda e: e.copy(out=qb[:], in_=pc[:, 0, :]), reads=pck(0), writes=["qb"])
        inv_rms(pc[:, 1, :], pck(1), ones_b[:], "ones_b", 1.0)
        P.op("dve", lambda e: e.tensor_tensor(out=kb[:], in0=pc[:, 1, :], in1=cvt[:], op=ALU.mult), reads=pck(1) + ["cvt"], writes=["kb"])
        P.op("act", lambda e: e.activation(out=pc[:, 3, :], in_=pc[:, 3, :], func=AF.Silu), reads=pck(3), writes=pck(3))
        P.op("dve", lambda e: e.tensor_scalar(out=pc[:, 3, :], in0=pc[:, 3, :], scalar1=gdn_s[:, 0:1], scalar2=None, op0=ALU.mult), reads=pck(3) + ["gdn_s"], writes=pck(3))
        P.op("pool", lambda e: e.memset(Sst[:, 0, :], 0.0), writes=[("S", 0)])
        P.op("pool", lambda e: e.memset(Sb[:, 0, :], 0.0), writes=[("Sb", 0)])
        s_par[0] = 0
        ngr = NCH // GW
        bank_lim[0] = 6
        bank_rr[0] = 0
        P.op("pool", lambda e: e.memset(oss[:], 0.0), reads=[], writes=pck(1) + pck(2) + ["cvt", "alias1", "oss"])
        pending = list(range(ngr))
        free_sets = list(range(NSET))
        running = []
        p1_done = set()
        scan_next = 0
        scan_running = False
        rnd = 0
        while pending or running or scan_next < ngr:
            rnd += 1
            if rnd % 4 == 0:
                prestage(1)
            while pending and free_sets and pending[0] - NPAR < scan_next:
                cg = pending.pop(0)
                si = free_sets.pop(0)
                running.append(["p1", cg, dn_phase1(h, cg, cg % NPAR, SC[si]), si])
            if not scan_running and scan_next < ngr and scan_next in p1_done:
                running.append(["scan", scan_next, dn_scan(h, scan_next, scan_next % NPAR), None])
                scan_running = True
            for r in list(running):
                try:
                    next(r[2])
                except StopIteration:
                    running.remove(r)
                    if r[0] == "p1":
                        free_sets.append(r[3])
                        p1_done.add(r[1])
                    else:
                        scan_next += 1
                        scan_running = False
        P.op("pool", lambda e: e.memset(oss[:], 0.0), reads=[], writes=pck(1) + pck(2) + ["cvt", "alias1", "oss"])
    prestage(len(pre_list))
    print("prestage DMAs issued before flush point; total", pre_i[0])
    dn.close()
    stA.close()
    bank_lim[0] = 8
    P.barrier()

    stB = ExitStack()
    x1 = sb(stB, "x1", [128, NT, D], F32)
    stW = ExitStack()
    wout_s = sb(stW, "wout_s", [128, 8, D], BF16)
    for kc in range(8):
        P.dma("pool", lambda e, kc=kc: e.dma_start(out=wout_s[:, kc, :], in_=w_out_r[:, kc, :]), writes=[("wout", kc)])
    for t in range(NT):
        P.dma("sp", lambda e, t=t: e.dma_start(out=x1[:, t, :], in_=x_tok[t * 128:(t + 1) * 128, :]), writes=[("x1", t)])
    for t in range(NT):
        for dh in range(2):
            b = bank()
            for kc in range(8):
                P.op("pe", lambda e, t=t, dh=dh, kc=kc, b=b: e.matmul(psv(b), lhsT=yT[:, kc, t * 128:(t + 1) * 128], rhs=wout_s[:, kc, dh * 512:(dh + 1) * 512], start=(kc == 0), stop=(kc == 7)),
                     reads=[("yT", kc), ("wout", kc)], writes=[pk(b)])
            P.op("dve", lambda e, t=t, dh=dh, b=b: e.tensor_tensor(out=x1[:, t, dh * 512:(dh + 1) * 512], in0=x1[:, t, dh * 512:(dh + 1) * 512], in1=psv(b), op=ALU.add),
                 reads=[pk(b), ("x1", t)], writes=[("x1", t)])
    stW.close()

    if stage == "A":
        for t in range(NT):
            P.dma("sp", lambda e, t=t: e.dma_start(out=out[t * 128:(t + 1) * 128, :], in_=x1[:, t, :]), reads=[("x1", t)], writes=[("out", t)])
        P.finish([("out", t) for t in range(NT)])
        P.emit()
        return nc


    stackR.close()
    P.barrier()

    BIG = 1.0e30

    g2bc = sb(stB, "g2bc", [128, D], F32)
    g3bc = sb(stB, "g3bc", [128, D], F32)
    wr_s = sb(stB, "wr_s", [128, 8, 36], F32)
    ecap = sb(stB, "ecap", [128, NE], F32)
    ecap_i = sb(stB, "ecap_i", [128, NE], I32)
    ustr_b = sb(stB, "ustr_b", [128, 128], BF16)
    d1i = sb(stB, "d1i", [128, NT], I32)
    d2i = sb(stB, "d2i", [128, NT], I32)
    gt1 = sb(stB, "gt1", [128, NT], F32)
    gt2 = sb(stB, "gt2", [128, NT], F32)
    P.dma("sp", lambda e: e.dma_start(out=g2bc[:], in_=bass.AP(g2_h, 0, [[0, 128], [1, D]])), writes=["g2bc"])
    P.dma("sp", lambda e: e.dma_start(out=g3bc[:], in_=bass.AP(g3_h, 0, [[0, 128], [1, D]])), writes=["g3bc"])
    P.dma("sp", lambda e: e.dma_start(out=wr_s[:], in_=wr_r), writes=["wr_s"])
    P.op("pool", lambda e: e.iota(ecap_i[:], [[CAP, NE]], base=0, channel_multiplier=0), writes=["ecap_i"])
    P.op("dve", lambda e: e.tensor_copy(out=ecap[:], in_=ecap_i[:]), reads=["ecap_i"], writes=["ecap"])
    P.op("pool", lambda e: e.memset(ustr_b[:], 1.0), writes=["ustr_b"])
    P.op("pool", lambda e: e.affine_select(out=ustr_b[:], in_=ustr_b[:], pattern=[[1, 128]], compare_op=ALU.is_gt, fill=0.0, base=0, channel_multiplier=-1), reads=["ustr_b"], writes=["ustr_b"])

    rt = ExitStack()
    h2b = sb(rt, "h2b", [128, NT, D], BF16)
    h2f = sb(rt, "h2f", [128, 3, D], F32)
    h2lo = sb(rt, "h2lo", [128, 3, D], BF16)
    hT2 = sb(rt, "hT2", [128, 3, 2, 8, 128], BF16)
    wr_hi = sb(rt, "wr_hi", [128, 8, 36], BF16)
    wr_lo = sb(rt, "wr_lo", [128, 8, 36], BF16)
    ssq = sb(rt, "ssq", [128, NT], F32)
    Lall = sb(rt, "Lall", [128, NT, 36], F32)
    r_gmax = sb(rt, "r_gmax", [128, NT], F32)
    r_gmask = sb(rt, "r_gmask", [128, NT, 4], F32)
    r_ge = sb(rt, "r_ge", [128, NT, 4], F32)
    r_gp = sb(rt, "r_gp", [128, NT], F32)
    r_pen = sb(rt, "r_pen", [128, NT, 4], F32)
    r_el = sb(rt, "r_el", [128, NT, 32], F32)
    r_el2 = sb(rt, "r_el2", [128, NT, 32], F32)
    r_m1 = sb(rt, "r_m1", [128, NT], F32)
    r_m2 = sb(rt, "r_m2", [128, NT], F32)
    r_mask1 = sb(rt, "r_mask1", [128, NT, 32], F32)
    r_mask2 = sb(rt, "r_mask2", [128, NT, 32], F32)
    r_m12b = sb(rt, "r_m12b", [128, NT, 32], BF16)
    r_e21 = sb(rt, "r_e21", [128, NT], F32)
    r_rank = sb(rt, "r_rank", [128, NT, 32], F32)
    r_valid = sb(rt, "r_valid", [128, NT, 32], F32)
    r_slot = sb(rt, "r_slot", [128, NT, 32], F32)
    r_tmp = sb(rt, "r_tmp", [128, NT, 32], F32)
    r_d1f = sb(rt, "r_d1f", [128, NT], F32)
    r_d2f = sb(rt, "r_d2f", [128, NT], F32)
    r_v1 = sb(rt, "r_v1", [128, NT], F32)
    r_v2 = sb(rt, "r_v2", [128, NT], F32)

    ssq_keys = [("ssq", t) for t in range(NT)]
    lall_keys = [("Lall", t) for t in range(NT)]
    P.op("pool", lambda e: e.memset(ssq[:], 0.0), writes=ssq_keys)
    P.op("act", lambda e: e.copy(out=wr_hi[:], in_=wr_s[:]), reads=["wr_s"], writes=["wr_hi"])
    P.op("dve", lambda e: e.tensor_tensor(out=wr_lo[:], in0=wr_s[:], in1=wr_hi[:], op=ALU.subtract), reads=["wr_s", "wr_hi"], writes=["wr_lo"])

    def r1_tile(t):
        sl = t % 3
        P.op("act", lambda e: e.activation(out=h2f[:, sl, :], in_=x1[:, t, :], func=AF.Square, accum_out=ssq[:, t:t + 1]), reads=[("x1", t), ("ssq", t)], writes=[("h2f", sl), ("ssq", t)])
        P.op("act", lambda e: e.activation(out=ssq[:, t:t + 1], in_=ssq[:, t:t + 1], func=AF.Sqrt, scale=1.0 / D, bias=EPS), reads=[("ssq", t)], writes=[("ssq", t)])
        P.op("dve", lambda e: e.reciprocal(out=ssq[:, t:t + 1], in_=ssq[:, t:t + 1]), reads=[("ssq", t)], writes=[("ssq", t)])
        P.op("dve", lambda e: e.scalar_tensor_tensor(out=h2f[:, sl, :], in0=x1[:, t, :], scalar=ssq[:, t:t + 1], in1=g2bc[:], op0=ALU.mult, op1=ALU.mult),
             reads=[("x1", t), ("ssq", t), "g2bc"], writes=[("h2f", sl)])
        P.op("act", lambda e: e.copy(out=h2b[:, t, :], in_=h2f[:, sl, :]), reads=[("h2f", sl)], writes=[("h2b", t)])
        P.op("pool", lambda e: e.tensor_tensor(out=h2lo[:, sl, :], in0=h2f[:, sl, :], in1=h2b[:, t, :], op=ALU.subtract), reads=[("h2f", sl), ("h2b", t)], writes=[("h2lo", sl)])
        yield
        b0 = bank(2)
        for kc in range(8):
            P.op("pe", lambda e, kc=kc: e.matmul(psum[0:128, b0 * 512 + kc * 128: b0 * 512 + (kc + 1) * 128], lhsT=h2b[:, t, kc * 128:(kc + 1) * 128], rhs=ident_b[:], start=True, stop=True),
                 reads=[("h2b", t), "ident_b"], writes=[pk(b0), pk(b0 + 1)])
        P.op("dve", lambda e: e.tensor_copy(out=hT2[:, sl, 0].rearrange("p k c -> p (k c)"), in_=psum[0:128, b0 * 512:(b0 + 2) * 512]), reads=[pk(b0), pk(b0 + 1)], writes=[("hT2", sl, 0)])
        b2 = bank(2)
        for kc in range(8):
            P.op("pe", lambda e, kc=kc: e.matmul(psum[0:128, b2 * 512 + kc * 128: b2 * 512 + (kc + 1) * 128], lhsT=h2lo[:, sl, kc * 128:(kc + 1) * 128], rhs=ident_b[:], start=True, stop=True),
                 reads=[("h2lo", sl), "ident_b"], writes=[pk(b2), pk(b2 + 1)])
        P.op("dve", lambda e: e.tensor_copy(out=hT2[:, sl, 1].rearrange("p k c -> p (k c)"), in_=psum[0:128, b2 * 512:(b2 + 2) * 512]), reads=[pk(b2), pk(b2 + 1)], writes=[("hT2", sl, 1)])
        yield
        bl = bank()
        n_ = 0
        for kc in range(8):
            for hl, wt, wk in ((0, wr_hi, "wr_hi"), (1, wr_hi, "wr_hi"), (0, wr_lo, "wr_lo")):
                P.op("pe", lambda e, kc=kc, hl=hl, wt=wt, n_=n_: e.matmul(psum[0:128, bl * 512: bl * 512 + 36], lhsT=hT2[:, sl, hl, kc, :], rhs=wt[:, kc, :], start=(n_ == 0), stop=(n_ == 23)),
                     reads=[("hT2", sl, hl), wk], writes=[pk(bl)])
                n_ += 1
        P.op("act", lambda e: e.copy(out=Lall[:, t, :], in_=psum[0:128, bl * 512: bl * 512 + 36]), reads=[pk(bl)], writes=[("Lall", t)])
        yield

    gens_ = [r1_tile(t) for t in range(NT)]
    active_ = []
    nxt_ = 0
    while active_ or nxt_ < NT:
        if nxt_ < NT and len(active_) < 3:
            active_.append(gens_[nxt_])
            nxt_ += 1
        for g_ in list(active_):
            try:
                next(g_)
            except StopIteration:
                active_.remove(g_)

    GLv = Lall[:, :, 0:4]
    ELv = Lall[:, :, 4:36]

    def bc(ap, shape):
        return ap.to_broadcast(shape)

    P.op("dve", lambda e: e.tensor_reduce(out=r_gmax[:], in_=GLv, axis=AX.X, op=ALU.max), reads=lall_keys, writes=["r_gmax"])
    P.op("dve", lambda e: e.tensor_tensor(out=r_gmask[:], in0=GLv, in1=bc(r_gmax[:, :, None], [128, NT, 4]), op=ALU.is_equal), reads=lall_keys + ["r_gmax"], writes=["r_gmask"])
    P.op("dve", lambda e: e.tensor_tensor(out=r_ge[:], in0=GLv, in1=bc(r_gmax[:, :, None], [128, NT, 4]), op=ALU.subtract), reads=lall_keys + ["r_gmax"], writes=["r_ge"])
    P.op("act", lambda e: e.activation(out=r_ge[:], in_=r_ge[:], func=AF.Exp), reads=["r_ge"], writes=["r_ge"])
    P.op("dve", lambda e: e.tensor_reduce(out=r_gp[:], in_=r_ge[:], axis=AX.X, op=ALU.add), reads=["r_ge"], writes=["r_gp"])
    P.op("dve", lambda e: e.reciprocal(out=r_gp[:], in_=r_gp[:]), reads=["r_gp"], writes=["r_gp"])
    P.op("dve", lambda e: e.tensor_scalar(out=r_pen[:], in0=r_gmask[:], scalar1=BIG, scalar2=-BIG, op0=ALU.mult, op1=ALU.add), reads=["r_gmask"], writes=["r_pen"])
    P.op("dve", lambda e: e.tensor_tensor(out=r_el[:].rearrange("p t (g k) -> p t g k", k=8), in0=ELv.rearrange("p t (g k) -> p t g k", k=8), in1=bc(r_pen[:, :, :, None], [128, NT, 4, 8]), op=ALU.add),
         reads=lall_keys + ["r_pen"], writes=["r_el"])
    P.op("dve", lambda e: e.tensor_reduce(out=r_m1[:], in_=r_el[:], axis=AX.X, op=ALU.max), reads=["r_el"], writes=["r_m1"])
    P.op("dve", lambda e: e.tensor_tensor(out=r_mask1[:], in0=r_el[:], in1=bc(r_m1[:, :, None], [128, NT, 32]), op=ALU.is_equal), reads=["r_el", "r_m1"], writes=["r_mask1"])
    P.op("dve", lambda e: e.scalar_tensor_tensor(out=r_el2[:], in0=r_mask1[:], scalar=-BIG, in1=r_el[:], op0=ALU.mult, op1=ALU.add), reads=["r_mask1", "r_el"], writes=["r_el2"])
    P.op("dve", lambda e: e.tensor_reduce(out=r_m2[:], in_=r_el2[:], axis=AX.X, op=ALU.max), reads=["r_el2"], writes=["r_m2"])
    P.op("dve", lambda e: e.tensor_tensor(out=r_mask2[:], in0=r_el2[:], in1=bc(r_m2[:, :, None], [128, NT, 32]), op=ALU.is_equal), reads=["r_el2", "r_m2"], writes=["r_mask2"])
    P.op("dve", lambda e: e.tensor_tensor(out=r_m12b[:], in0=r_mask1[:], in1=r_mask2[:], op=ALU.add), reads=["r_mask1", "r_mask2"], writes=["r_m12b"])
    P.op("dve", lambda e: e.tensor_tensor(out=r_e21[:], in0=r_m2[:], in1=r_m1[:], op=ALU.subtract), reads=["r_m1", "r_m2"], writes=["r_e21"])
    P.op("act", lambda e: e.activation(out=r_e21[:], in_=r_e21[:], func=AF.Exp), reads=["r_e21"], writes=["r_e21"])
    P.op("dve", lambda e: e.tensor_scalar(out=gt1[:], in0=r_e21[:], scalar1=1.0, scalar2=None, op0=ALU.add), reads=["r_e21"], writes=["gt1"])
    P.op("dve", lambda e: e.reciprocal(out=gt1[:], in_=gt1[:]), reads=["gt1"], writes=["gt1"])
    P.op("dve", lambda e: e.tensor_tensor(out=gt1[:], in0=gt1[:], in1=r_gp[:], op=ALU.mult), reads=["gt1", "r_gp"], writes=["gt1"])
    P.op("dve", lambda e: e.tensor_tensor(out=gt2[:], in0=gt1[:], in1=r_e21[:], op=ALU.mult), reads=["gt1", "r_e21"], writes=["gt2"])

    br = bank()
    for t in range(NT):
        P.op("pe", lambda e, t=t: e.matmul(psum[0:128, br * 512 + t * 32: br * 512 + (t + 1) * 32], lhsT=ustr_b[:], rhs=r_m12b[:, t, :], start=True, stop=(t == 0)),
             reads=["ustr_b", "r_m12b"], writes=[pk(br)])
        for t2 in range(t):
            P.op("pe", lambda e, t=t, t2=t2: e.matmul(psum[0:128, br * 512 + t * 32: br * 512 + (t + 1) * 32], lhsT=ones_b[:], rhs=r_m12b[:, t2, :], start=False, stop=(t2 == t - 1)),
                 reads=["ones_b", "r_m12b"], writes=[pk(br)])
    P.op("act", lambda e: e.copy(out=r_rank[:].rearrange("p t k -> p (t k)"), in_=psv(br)), reads=[pk(br)], writes=["r_rank"])
    P.op("dve", lambda e: e.tensor_scalar(out=r_valid[:], in0=r_rank[:], scalar1=float(CAP), scalar2=None, op0=ALU.is_lt), reads=["r_rank"], writes=["r_valid"])
    P.op("dve", lambda e: e.tensor_tensor(out=r_slot[:], in0=r_rank[:], in1=bc(ecap[:, None, :], [128, NT, 32]), op=ALU.add), reads=["r_rank", "ecap"], writes=["r_slot"])
    P.op("dve", lambda e: e.tensor_scalar(out=r_slot[:], in0=r_slot[:], scalar1=-float(DUMP), scalar2=None, op0=ALU.add), reads=["r_slot"], writes=["r_slot"])
    P.op("dve", lambda e: e.tensor_tensor(out=r_slot[:], in0=r_slot[:], in1=r_valid[:], op=ALU.mult), reads=["r_slot", "r_valid"], writes=["r_slot"])
    P.op("dve", lambda e: e.tensor_scalar(out=r_slot[:], in0=r_slot[:], scalar1=float(DUMP), scalar2=None, op0=ALU.add), reads=["r_slot"], writes=["r_slot"])
    for msk, mk, df, dk, vv, vk, di, dik, gt, gk in ((r_mask1, "r_mask1", r_d1f, "r_d1f", r_v1, "r_v1", d1i, "d1i", gt1, "gt1"),
                                                     (r_mask2, "r_mask2", r_d2f, "r_d2f", r_v2, "r_v2", d2i, "d2i", gt2, "gt2")):
        P.op("dve", lambda e, msk=msk: e.tensor_tensor(out=r_tmp[:], in0=msk[:], in1=r_slot[:], op=ALU.mult), reads=[mk, "r_slot"], writes=["r_tmp"])
        P.op("dve", lambda e, df=df: e.tensor_reduce(out=df[:], in_=r_tmp[:], axis=AX.X, op=ALU.add), reads=["r_tmp"], writes=[dk])
        P.op("dve", lambda e, df=df, di=di: e.tensor_copy(out=di[:], in_=df[:]), reads=[dk], writes=[dik])
        P.op("dve", lambda e, msk=msk: e.tensor_tensor(out=r_tmp[:], in0=msk[:], in1=r_valid[:], op=ALU.mult), reads=[mk, "r_valid"], writes=["r_tmp"])
        P.op("dve", lambda e, vv=vv: e.tensor_reduce(out=vv[:], in_=r_tmp[:], axis=AX.X, op=ALU.add), reads=["r_tmp"], writes=[vk])
        P.op("dve", lambda e, vv=vv, gt=gt: e.tensor_tensor(out=gt[:], in0=gt[:], in1=vv[:], op=ALU.mult), reads=[gk, vk], writes=[gk])
    xs_keys = []
    for t in range(NT):
        for di, dik, w in ((d1i, "d1i", 0), (d2i, "d2i", 1)):
            key = ("Xs", t, w)
            xs_keys.append(key)
            P.dma("pool", lambda e, t=t, di=di: e.indirect_dma_start(out=Xs, out_offset=bass.IndirectOffsetOnAxis(ap=di[:, t:t + 1], axis=0), in_=h2b[:, t, :], in_offset=None, bounds_check=NE * CAP + 127, oob_is_err=False),
                  reads=[("h2b", t), dik] + xz_keys, writes=[key])
    if stage == "dbg":
        dbg_i = nc.dram_tensor("dbg_i", [128, 2 * NT], I32, kind="ExternalOutput").ap()
        dbg_g = nc.dram_tensor("dbg_g", [128, 2 * NT], F32, kind="ExternalOutput").ap()
        dbg_r = nc.dram_tensor("dbg_r", [128, NT * 32], F32, kind="ExternalOutput").ap()
        dbg_l = nc.dram_tensor("dbg_l", [128, NT * 36], F32, kind="ExternalOutput").ap()
        P.dma("sp", lambda e: e.dma_start(out=dbg_i[:, 0:NT], in_=d1i[:]), reads=["d1i"], writes=["dbg1"])
        P.dma("sp", lambda e: e.dma_start(out=dbg_i[:, NT:2 * NT], in_=d2i[:]), reads=["d2i"], writes=["dbg2"])
        P.dma("sp", lambda e: e.dma_start(out=dbg_g[:, 0:NT], in_=gt1[:]), reads=["gt1"], writes=["dbg3"])
        P.dma("sp", lambda e: e.dma_start(out=dbg_g[:, NT:2 * NT], in_=gt2[:]), reads=["gt2"], writes=["dbg4"])
        P.dma("sp", lambda e: e.dma_start(out=dbg_r, in_=r_rank[:].rearrange("p t k -> p (t k)")), reads=["r_rank"], writes=["dbg5"])
        P.dma("sp", lambda e: e.dma_start(out=dbg_l, in_=Lall[:].rearrange("p t k -> p (t k)")), reads=lall_keys, writes=["dbg6"])
        P.finish(["dbg1", "dbg2", "dbg3", "dbg4", "dbg5", "dbg6"])
    rt.close()
    P.barrier()

    ex = ExitStack()
    wg_s = sb(ex, "wg_s", [128, 3, 8, FF], BF16)
    wu_s = sb(ex, "wu_s", [128, 3, 8, FF], BF16)
    wd_s = sb(ex, "wd_s", [128, 3, 4, D], BF16)
    xb = sb(ex, "xb", [128, 2, 2, D], BF16)
    xbT = sb(ex, "xbT", [128, 2, 8, CAP], BF16)
    sil = sb(ex, "sil", [128, 4, CAP], F32)
    hmid = sb(ex, "hmid", [128, 2, 4, CAP], BF16)
    Yo = sb(ex, "Yo", [128, 2, D], BF16)
    fss = sb(ex, "fss", [128, NT], F32)

    def load_w(e_):
        par = e_ % 3
        P.dma("sp", lambda e: e.dma_start(out=wg_s[:, par], in_=Wg_b[e_].rearrange("(kc p) f -> p kc f", p=128)), reads=[("Wb", "g", e_)], writes=[("wg", par)])
        P.dma("sp", lambda e: e.dma_start(out=wu_s[:, par], in_=Wu_b[e_].rearrange("(kc p) f -> p kc f", p=128)), reads=[("Wb", "u", e_)], writes=[("wu", par)])
        P.dma("sp", lambda e: e.dma_start(out=wd_s[:, par], in_=Wd_b[e_].rearrange("(kc p) f -> p kc f", p=128)), reads=[("Wb", "d", e_)], writes=[("wd", par)])

    def load_xb(e_):
        par = e_ % 2
        P.dma("sp", lambda e: e.dma_start(out=xb[:, par], in_=Xs[e_ * CAP:(e_ + 1) * CAP, :].rearrange("(b p) d -> p b d", p=128)), reads=xs_keys, writes=[("xb", par)])

    ys_keys = []
    load_xb(0)
    load_w(0)
    load_w(1)

    def expert(e_):
        par = e_ % 2
        wp = e_ % 3
        if e_ + 1 < NE:
            load_xb(e_ + 1)
        for blk in range(2):
            for half in range(2):
                bt = bank()
                for q in range(4):
                    kc = half * 4 + q
                    P.op("pe", lambda e, blk=blk, kc=kc, q=q, bt=bt: e.matmul(psum[0:128, bt * 512 + q * 128: bt * 512 + (q + 1) * 128], lhsT=xb[:, par, blk, kc * 128:(kc + 1) * 128], rhs=ident_b[:], start=True, stop=True),
                         reads=[("xb", par), "ident_b"], writes=[pk(bt)])
                if half == 0:
                    P.op("act", lambda e, blk=blk, half=half, bt=bt: e.copy(out=xbT[:, par, half * 4:half * 4 + 4, blk * 128:(blk + 1) * 128], in_=psv(bt).rearrange("p (q c) -> p q c", c=128)),
                         reads=[pk(bt)], writes=[("xbT", par)])
                else:
                    P.op("dve", lambda e, blk=blk, half=half, bt=bt: e.tensor_copy(out=xbT[:, par, half * 4:half * 4 + 4, blk * 128:(blk + 1) * 128], in_=psv(bt).rearrange("p (q c) -> p q c", c=128)),
                         reads=[pk(bt)], writes=[("xbT", par)])
        yield
        ba_ = bank(2)
        for fc in range(4):
            for kc in range(8):
                P.op("pe", lambda e, fc=fc, kc=kc: e.matmul(psum[0:128, ba_ * 512 + fc * CAP: ba_ * 512 + (fc + 1) * CAP], lhsT=wg_s[:, wp, kc, fc * 128:(fc + 1) * 128], rhs=xbT[:, par, kc, :], start=(kc == 0), stop=(kc == 7)),
                     reads=[("wg", wp), ("xbT", par)], writes=[pk(ba_), pk(ba_ + 1)])
        P.op("act", lambda e: e.activation(out=sil[:].rearrange("p f c -> p (f c)"), in_=psum[0:128, ba_ * 512:(ba_ + 2) * 512], func=AF.Silu), reads=[pk(ba_), pk(ba_ + 1)], writes=["sil"])
        bb_ = bank(2)
        for fc in range(4):
            for kc in range(8):
                P.op("pe", lambda e, fc=fc, kc=kc: e.matmul(psum[0:128, bb_ * 512 + fc * CAP: bb_ * 512 + (fc + 1) * CAP], lhsT=wu_s[:, wp, kc, fc * 128:(fc + 1) * 128], rhs=xbT[:, par, kc, :], start=(kc == 0), stop=(kc == 7)),
                     reads=[("wu", wp), ("xbT", par)], writes=[pk(bb_), pk(bb_ + 1)])
        P.op("dve", lambda e: e.tensor_tensor(out=hmid[:, par].rearrange("p f c -> p (f c)"), in0=sil[:].rearrange("p f c -> p (f c)"), in1=psum[0:128, bb_ * 512:(bb_ + 2) * 512], op=ALU.mult),
             reads=["sil", pk(bb_), pk(bb_ + 1)], writes=[("hmid", par)])
        yield
        if e_ + 2 < NE:
            load_w(e_ + 2)
        for blk in range(2):
            for dh in range(2):
                bd = bank()
                for fc in range(4):
                    P.op("pe", lambda e, blk=blk, dh=dh, fc=fc, bd=bd: e.matmul(psv(bd), lhsT=hmid[:, par, fc, blk * 128:(blk + 1) * 128], rhs=wd_s[:, wp, fc, dh * 512:(dh + 1) * 512], start=(fc == 0), stop=(fc == 3)),
                         reads=[("hmid", par), ("wd", wp)], writes=[pk(bd)])
                if dh == 0:
                    P.op("act", lambda e, blk=blk, dh=dh, bd=bd: e.copy(out=Yo[:, blk, dh * 512:(dh + 1) * 512], in_=psv(bd)), reads=[pk(bd)], writes=[("Yo", blk, dh)])
                else:
                    P.op("dve", lambda e, blk=blk, dh=dh, bd=bd: e.tensor_copy(out=Yo[:, blk, dh * 512:(dh + 1) * 512], in_=psv(bd)), reads=[pk(bd)], writes=[("Yo", blk, dh)])
        key = ("Ys", e_)
        ys_keys.append(key)
        P.dma("sp", lambda e: e.dma_start(out=Ys[e_ * CAP:(e_ + 1) * CAP, :].rearrange("(b p) d -> p b d", p=128), in_=Yo[:]), reads=[("Yo", b_, d_) for b_ in range(2) for d_ in range(2)], writes=[key])

    gens_ = [expert(e_) for e_ in range(NE)]
    active_ = []
    nxt_ = 0
    while active_ or nxt_ < NE:
        if nxt_ < NE and len(active_) < 2:
            active_.append(gens_[nxt_])
            nxt_ += 1
        for g_ in list(active_):
            try:
                next(g_)
            except StopIteration:
                active_.remove(g_)

    G1 = sb(ex, "G1", [128, 2, D], BF16)
    G2 = sb(ex, "G2", [128, 2, D], BF16)
    fjunk = sb(ex, "fjunk", [128, D], F32)
    P.op("pool", lambda e: e.memset(fss[:], 0.0), writes=[("fss", t) for t in range(NT)])
    def comb_tile(t):
        sl = t % 2
        P.dma("pool", lambda e: e.indirect_dma_start(out=G1[:, sl, :], out_offset=None, in_=Ys, in_offset=bass.IndirectOffsetOnAxis(ap=d1i[:, t:t + 1], axis=0)), reads=ys_keys + ["d1i", ("Yz", 0)], writes=[("G1", sl)])
        P.dma("pool", lambda e: e.indirect_dma_start(out=G2[:, sl, :], out_offset=None, in_=Ys, in_offset=bass.IndirectOffsetOnAxis(ap=d2i[:, t:t + 1], axis=0)), reads=ys_keys + ["d2i", ("Yz", 0)], writes=[("G2", sl)])
        yield
        P.op("dve", lambda e: e.scalar_tensor_tensor(out=x1[:, t, :], in0=G1[:, sl, :], scalar=gt1[:, t:t + 1], in1=x1[:, t, :], op0=ALU.mult, op1=ALU.add), reads=[("G1", sl), "gt1", ("x1", t)], writes=[("x1", t)])
        P.op("dve", lambda e: e.scalar_tensor_tensor(out=x1[:, t, :], in0=G2[:, sl, :], scalar=gt2[:, t:t + 1], in1=x1[:, t, :], op0=ALU.mult, op1=ALU.add), reads=[("G2", sl), "gt2", ("x1", t)], writes=[("x1", t)])
        P.op("act", lambda e: e.activation(out=fjunk[:], in_=x1[:, t, :], func=AF.Square, accum_out=fss[:, t:t + 1]), reads=[("x1", t), ("fss", t)], writes=["fjunk", ("fss", t)])
        P.op("act", lambda e: e.activation(out=fss[:, t:t + 1], in_=fss[:, t:t + 1], func=AF.Sqrt, scale=1.0 / D, bias=EPS), reads=[("fss", t)], writes=[("fss", t)])
        yield
        P.op("dve", lambda e: e.reciprocal(out=fss[:, t:t + 1], in_=fss[:, t:t + 1]), reads=[("fss", t)], writes=[("fss", t)])
        P.op("dve", lambda e: e.scalar_tensor_tensor(out=x1[:, t, :], in0=x1[:, t, :], scalar=fss[:, t:t + 1], in1=g3bc[:], op0=ALU.mult, op1=ALU.mult), reads=[("x1", t), ("fss", t), "g3bc"], writes=[("x1", t)])
        P.dma("sp", lambda e: e.dma_start(out=out[t * 128:(t + 1) * 128, :], in_=x1[:, t, :]), reads=[("x1", t)], writes=[("out", t)])
        yield

    gens_ = [comb_tile(t) for t in range(NT)]
    active_ = []
    nxt_ = 0
    while active_ or nxt_ < NT:
        if nxt_ < NT and len(active_) < 2:
            active_.append(gens_[nxt_])
            nxt_ += 1
        for g_ in list(active_):
            try:
                next(g_)
            except StopIteration:
                active_.remove(g_)
    P.finish([("out", t) for t in range(NT)])
    P.emit()
    return nc


def make_inputs(inp, b):
    x = np.ascontiguousarray(inp["x"][b])
    w_in = inp["w_in"][0]
    m = {
        "x_tok": x,
        "xT": np.ascontiguousarray(x.T),
        "w_in_r": np.ascontiguousarray(w_in[:, :3584].reshape(8, 128, 28, 128).transpose(2, 1, 0, 3)),
        "w_ba": np.ascontiguousarray(w_in[:, 3584:3592].reshape(8, 128, 8).transpose(1, 0, 2)),
        "g1": np.ascontiguousarray(inp["mix_norm_g"][0].reshape(8, 128).T),
        "caw": np.ascontiguousarray(inp["conv_a_w"][0].reshape(3, 4, 128).transpose(2, 1, 0)),
        "gA": np.ascontiguousarray(inp["conv_a_norm_g"][0].reshape(4, 128).T),
        "dcw": np.ascontiguousarray(inp["dn_conv_w"][0].reshape(4, 12, 128).transpose(2, 1, 0)),
        "alog": np.ascontiguousarray(inp["dn_a_log"][0]),
        "dtb": np.ascontiguousarray(inp["dn_dt_bias"][0]),
        "gdn": np.ascontiguousarray(inp["dn_norm_g"][0].reshape(128, 1)),
        "w_out_r": np.ascontiguousarray(inp["w_out"][0].reshape(8, 128, D).transpose(1, 0, 2)),
        "g2": np.ascontiguousarray(inp["ffn_norm_g"][0]),
        "wr_r": np.ascontiguousarray(np.concatenate([inp["router_group_w"][0], inp["router_expert_w"][0]], axis=1).reshape(8, 128, 36).transpose(1, 0, 2)),
        "w_gate": np.ascontiguousarray(inp["w_gate"][0]),
        "w_up": np.ascontiguousarray(inp["w_up"][0]),
        "w_down": np.ascontiguousarray(inp["w_down"][0]),
        "g3": np.ascontiguousarray(inp["final_norm_g"]),
    }
    return m


def kernel(**inputs):
    inp = {k: np.asarray(v) for k, v in inputs.items()}
    nc = build("full")
    shared = make_inputs(inp, 0)
    in_maps = []
    for b in range(8):
        m = dict(shared)
        xb = np.ascontiguousarray(inp["x"][b])
        m["x_tok"] = xb
        m["xT"] = np.ascontiguousarray(xb.T)
        in_maps.append(m)
    res = run_bass_kernel_spmd(nc, in_maps, core_ids=list(range(8)))
    return np.stack([r["out"] for r in res.results], axis=0).astype(np.float32)
```

```python
from contextlib import ExitStack
import numpy as np
import concourse.bass as bass
import concourse.mybir as mybir
from concourse.bass_utils import run_bass_kernel_spmd

F32 = mybir.dt.float32
BF16 = mybir.dt.bfloat16
I32 = mybir.dt.int32
ALU = mybir.AluOpType
AF = mybir.ActivationFunctionType
AX = mybir.AxisListType

S = 2048
D = 1024
NT = 16
NCH = 32
NE = 32
FF = 512
CAP = 256
EPS = 1e-6
DN_DT = F32


class _Eng:
    def __init__(self, eng, sem, is_pe=False):
        self.eng = eng
        self.sem = sem
        self.count = 0
        self.waited = {}
        self.is_pe = is_pe
        self.rec = []


class Prog:
    def __init__(self, nc, n_dma_sems=10):
        self.nc = nc
        self.E = {
            "pe": _Eng(nc.tensor, nc.alloc_semaphore("s_pe"), True),
            "act": _Eng(nc.scalar, nc.alloc_semaphore("s_act")),
            "dve": _Eng(nc.vector, nc.alloc_semaphore("s_dve")),
            "pool": _Eng(nc.gpsimd, nc.alloc_semaphore("s_pool")),
            "sp": _Eng(nc.sync, nc.alloc_semaphore("s_sp")),
        }
        self.dma_sems = {}
        for q in ("sp", "pool", "act"):
            self.dma_sems[q] = [[nc.alloc_semaphore(f"d_{q}{i}"), 0] for i in range(n_dma_sems)]
        self.dma_rr = {"sp": 0, "pool": 0, "act": 0}
        self.rw = {}

    def _deps(self, reads, writes):
        deps = {}

        def add(tok):
            if tok is None:
                return
            s, v = tok
            if deps.get(s.num, (None, -1))[1] < v:
                deps[s.num] = (s, v)

        for k in reads:
            st = self.rw.get(k)
            if st is not None:
                add(st["w"])
        for k in writes:
            st = self.rw.get(k)
            if st is not None:
                add(st["w"])
                for t in st["r"].values():
                    add(t)
        return deps

    def _commit(self, tok, reads, writes):
        for k in writes:
            self.rw[k] = {"w": tok, "r": {}}
        for k in reads:
            st = self.rw.setdefault(k, {"w": None, "r": {}})
            s, v = tok
            if st["r"].get(s.num, (None, -1))[1] < v:
                st["r"][s.num] = tok

    def _wait(self, e, deps, skip_own=False):
        for num, (s, v) in deps.items():
            if skip_own and num == e.sem.num:
                continue
            if e.waited.get(num, 0) < v:
                e.rec.append(("w", s, v))
                e.waited[num] = v

    def op(self, en, fn, reads=(), writes=()):
        e = self.E[en]
        deps = self._deps(reads, writes)
        self._wait(e, deps, skip_own=e.is_pe)
        e.count += 1
        e.rec.append(("i", fn, e.sem, 1))
        tok = (e.sem, e.count)
        self._commit(tok, reads, writes)
        return tok

    def dma(self, q, fn, reads=(), writes=()):
        e = self.E[q]
        deps = self._deps(reads, writes)
        self._wait(e, deps)
        pool = self.dma_sems[q]
        i = self.dma_rr[q]
        self.dma_rr[q] = (i + 1) % len(pool)
        slot = pool[i]
        s, v = slot
        if v > 0 and e.waited.get(s.num, 0) < v:
            e.rec.append(("w", s, v))
            e.waited[s.num] = v
        e.rec.append(("i", fn, s, 16))
        slot[1] = v + 16
        tok = (s, v + 16)
        self._commit(tok, reads, writes)
        return tok

    def barrier(self):
        toks = []
        for e in self.E.values():
            if e.count > 0:
                toks.append((e.sem, e.count))
        for q in self.dma_sems.values():
            for s, v in q:
                if v > 0:
                    toks.append((s, v))
        for e in self.E.values():
            for s, v in toks:
                if s.num == e.sem.num:
                    continue
                if e.waited.get(s.num, 0) < v:
                    e.rec.append(("w", s, v))
                    e.waited[s.num] = v

    def finish(self, keys):
        e = self.E["sp"]
        self._wait(e, self._deps(keys, ()))

    def emit(self):
        nc = self.nc

        def replay(e):
            def f(eng):
                for r in e.rec:
                    if r[0] == "w":
                        eng.wait_ge(r[1], r[2])
                    else:
                        r[1](eng).then_inc(r[2], r[3])
            return f

        with nc.Block() as block:
            block.sync(replay(self.E["sp"]))
            block.scalar(replay(self.E["act"]))
            block.vector(replay(self.E["dve"]))
            block.gpsimd(replay(self.E["pool"]))
            block.tensor(replay(self.E["pe"]))


def interleave(gens):
    gens = list(gens)
    while gens:
        for g in list(gens):
            try:
                next(g)
            except StopIteration:
                gens.remove(g)


def build(stage="full"):
    nc = bass.Bass("TRN2", target_bir_lowering=False)
    P = Prog(nc)

    def din(name, shape, dt=F32):
        return nc.dram_tensor(name, list(shape), dt, kind="ExternalInput")

    x_tok = din("x_tok", [S, D]).ap()
    xT = din("xT", [D, S]).ap()
    w_in_r = din("w_in_r", [28, 128, 8, 128]).ap()
    w_ba = din("w_ba", [128, 8, 8]).ap()
    g1 = din("g1", [128, 8]).ap()
    caw = din("caw", [128, 4, 3]).ap()
    gA = din("gA", [128, 4]).ap()
    dcw = din("dcw", [128, 12, 4]).ap()
    alog_h = din("alog", [4])
    dtb_h = din("dtb", [4])
    gdn = din("gdn", [128, 1]).ap()
    w_out_r = din("w_out_r", [128, 8, D]).ap()
    g2_h = din("g2", [D])
    wr_r = din("wr_r", [128, 8, 36]).ap()
    w_gate = din("w_gate", [NE, D, FF]).ap()
    w_up = din("w_up", [NE, D, FF]).ap()
    w_down = din("w_down", [NE, FF, D]).ap()
    g3_h = din("g3", [D])
    out = nc.dram_tensor("out", [S, D], F32, kind="ExternalOutput").ap()

    DUMP = NE * CAP
    Xs = nc.dram_tensor("Xs_scr", [NE * CAP + 128, D], BF16, kind="Internal").ap()
    Ys = nc.dram_tensor("Ys_scr", [NE * CAP + 128, D], BF16, kind="Internal").ap()
    Wg_b = nc.dram_tensor("Wg_b", [NE, D, FF], BF16, kind="Internal").ap()
    Wu_b = nc.dram_tensor("Wu_b", [NE, D, FF], BF16, kind="Internal").ap()
    Wd_b = nc.dram_tensor("Wd_b", [NE, FF, D], BF16, kind="Internal").ap()
    stack0 = ExitStack()

    def sb(st, name, shape, dt, side=None):
        return st.enter_context(nc.sbuf_tensor(name, list(shape), dt, side=side))

    psum = nc.alloc_psum_tensor("psum", [128, 8 * 512], F32)
    bank_rr = [0]
    bank_lim = [8]

    def bank(n=1):
        b = bank_rr[0]
        if b + n > bank_lim[0]:
            b = 0
        bank_rr[0] = (b + n) % bank_lim[0]
        return b

    def psv(b, parts=128, n=512, nb=1):
        return psum[0:parts, b * 512:b * 512 + n] if nb == 1 else psum[0:parts, b * 512:(b + nb) * 512]

    def pk(b):
        return ("ps", b)

    ident_f = sb(stack0, "ident_f", [128, 128], F32)
    ident_b = sb(stack0, "ident_b", [128, 128], BF16)
    ones_f = sb(stack0, "ones_f", [128, 128], F32)
    ones_b = sb(stack0, "ones_b", [128, 128], BF16)
    bd64_b = sb(stack0, "bd64_b", [128, 128], BF16)
    bd64_f = sb(stack0, "bd64_f", [128, 128], F32)
    u64 = sb(stack0, "u64", [64, 64], F32)
    maskc8 = sb(stack0, "maskc8", [64, 8, 64], F32)
    masks8 = sb(stack0, "masks8", [64, 8, 64], F32)
    eye8 = sb(stack0, "eye8", [64, 8, 64], F32)

    P.op("pool", lambda e: e.memset(ident_f[:], 1.0), writes=["ident_f"])
    P.op("pool", lambda e: e.affine_select(out=ident_f[:], in_=ident_f[:], pattern=[[-1, 128]], compare_op=ALU.is_equal, fill=0.0, base=0, channel_multiplier=1), reads=["ident_f"], writes=["ident_f"])
    P.op("dve", lambda e: e.tensor_copy(out=ident_b[:], in_=ident_f[:]), reads=["ident_f"], writes=["ident_b"])
    P.op("pool", lambda e: e.memset(ones_f[:], 1.0), writes=["ones_f"])
    P.op("pool", lambda e: e.memset(ones_b[:], 1.0), writes=["ones_b"])
    P.op("pool", lambda e: e.memset(bd64_f[:], 1.0), writes=["bd64_f"])
    P.op("pool", lambda e: e.affine_select(out=bd64_f[:, 0:64], in_=bd64_f[:, 0:64], pattern=[[0, 64]], compare_op=ALU.is_ge, fill=0.0, base=63, channel_multiplier=-1), reads=["bd64_f"], writes=["bd64_f"])
    P.op("pool", lambda e: e.affine_select(out=bd64_f[:, 64:128], in_=bd64_f[:, 64:128], pattern=[[0, 64]], compare_op=ALU.is_ge, fill=0.0, base=-64, channel_multiplier=1), reads=["bd64_f"], writes=["bd64_f"])
    P.op("dve", lambda e: e.tensor_copy(out=bd64_b[:], in_=bd64_f[:]), reads=["bd64_f"], writes=["bd64_b"])
    P.op("pool", lambda e: e.memset(u64[:], 1.0), writes=["u64"])
    P.op("pool", lambda e: e.affine_select(out=u64[:], in_=u64[:], pattern=[[1, 64]], compare_op=ALU.is_ge, fill=0.0, base=0, channel_multiplier=-1), reads=["u64"], writes=["u64"])
    for t_, op_, nm in ((maskc8, ALU.is_ge, "maskc8"), (masks8, ALU.is_gt, "masks8"), (eye8, ALU.is_equal, "eye8")):
        P.op("pool", lambda e, t_=t_: e.memset(t_[:], 1.0), writes=[nm])
        P.op("pool", lambda e, t_=t_, op_=op_: e.affine_select(out=t_[:], in_=t_[:], pattern=[[0, 8], [-1, 64]], compare_op=op_, fill=0.0, base=0, channel_multiplier=1), reads=[nm], writes=[nm])

    g1_s = sb(stack0, "g1_s", [128, 8], F32)
    caw_s = sb(stack0, "caw_s", [128, 4, 3], F32)
    gA_s = sb(stack0, "gA_s", [128, 4], F32)
    dcw_s = sb(stack0, "dcw_s", [128, 12, 4], F32)
    gdn_s = sb(stack0, "gdn_s", [128, 1], F32)
    P.dma("sp", lambda e: e.dma_start(out=g1_s[:], in_=g1), writes=["g1_s"])
    P.dma("sp", lambda e: e.dma_start(out=caw_s[:], in_=caw), writes=["caw_s"])
    P.dma("sp", lambda e: e.dma_start(out=gA_s[:], in_=gA), writes=["gA_s"])
    P.dma("sp", lambda e: e.dma_start(out=dcw_s[:], in_=dcw), writes=["dcw_s"])
    P.dma("sp", lambda e: e.dma_start(out=gdn_s[:], in_=gdn), writes=["gdn_s"])

    stackR = ExitStack()
    yT = sb(stackR, "yT", [128, 8, S], BF16, side="right")

    stA = ExitStack()
    hT = sb(stA, "hT", [128, 8, S], BF16)
    wring = sb(stA, "wring", [128, 2, 8, 128], BF16)
    wba_s = sb(stA, "wba_s", [128, 8, 8], BF16)
    pc = sb(stA, "pc", [128, 4, S], F32)
    cvt = sb(stA, "cvt", [128, S], F32)
    sqb = sb(stA, "sqb", [128, 2, S], BF16)
    tb_ba = sb(stA, "tb_ba", [64, NCH, 8], F32)
    tb_alog = sb(stA, "tb_alog", [64, NCH, 4], F32)
    tb_dtb = sb(stA, "tb_dtb", [64, NCH, 4], F32)
    tb_beta = sb(stA, "tb_beta", [64, NCH, 4], F32)
    tb_nbeta = sb(stA, "tb_nbeta", [64, NCH, 4], F32)
    tb_t0 = sb(stA, "tb_t0", [64, NCH, 4], F32)
    tb_t1 = sb(stA, "tb_t1", [64, NCH, 4], F32)
    tb_g = sb(stA, "tb_g", [64, NCH, 4], F32)
    tb_gc = sb(stA, "tb_gc", [64, NCH, 4], F32)
    tb_gl = sb(stA, "tb_gl", [128, NCH, 4], F32)
    tb_egl = sb(stA, "tb_egl", [128, NCH, 4], F32)
    tb_kbe = sb(stA, "tb_kbe", [64, NCH, 4], F32)
    tb_kdec = sb(stA, "tb_kdec", [64, NCH, 4], F32)

    zt = sb(stA, "zt", [128, 2048], BF16)
    P.op("pool", lambda e: e.memset(zt[:], 0.0), writes=["zt"])
    P.dma("pool", lambda e: e.dma_start(out=wba_s[:], in_=w_ba), writes=["wba_s"])
    ab_s = sb(stA, "ab_s", [64, 2, 4], F32)
    P.dma("sp", lambda e: e.dma_start(out=ab_s[:, 0, :], in_=bass.AP(alog_h, 0, [[0, 64], [1, 4]])), writes=["ab_s0"])
    P.dma("sp", lambda e: e.dma_start(out=ab_s[:, 1, :], in_=bass.AP(dtb_h, 0, [[0, 64], [1, 4]])), writes=["ab_s1"])
    P.op("dve", lambda e: e.tensor_copy(out=tb_alog[:], in_=ab_s[:, 0:1, :].to_broadcast([64, NCH, 4])), reads=["ab_s0"], writes=["tb_alog"])
    P.op("dve", lambda e: e.tensor_copy(out=tb_dtb[:], in_=ab_s[:, 1:2, :].to_broadcast([64, NCH, 4])), reads=["ab_s1"], writes=["tb_dtb"])

    stA2 = ExitStack()
    xs = sb(stA2, "xs", [128, 2, S], F32)
    rbc = sb(stA2, "rbc", [128, S], F32)
    for kc in range(8):
        sl = kc % 2
        P.dma("sp", lambda e, kc=kc, sl=sl: e.dma_start(out=xs[:, sl, :], in_=xT[kc * 128:(kc + 1) * 128, :]), writes=[("xs", sl)])
        P.op("act", lambda e, sl=sl: e.activation(out=sqb[:, sl, :], in_=xs[:, sl, :], func=AF.Square), reads=[("xs", sl)], writes=[("sqb", sl)])
        for tb in range(4):
            P.op("pe", lambda e, kc=kc, sl=sl, tb=tb: e.matmul(psv(tb), lhsT=ones_b[:], rhs=sqb[:, sl, tb * 512:(tb + 1) * 512], start=(kc == 0), stop=(kc == 7)),
                 reads=[("sqb", sl), "ones_b"], writes=[pk(tb)])
    for tb in range(4):
        P.op("act", lambda e, tb=tb: e.activation(out=rbc[:, tb * 512:(tb + 1) * 512], in_=psv(tb), func=AF.Ln, scale=1.0 / D, bias=EPS), reads=[pk(tb)], writes=[("rbc", tb)])
        P.op("act", lambda e, tb=tb: e.activation(out=rbc[:, tb * 512:(tb + 1) * 512], in_=rbc[:, tb * 512:(tb + 1) * 512], func=AF.Exp, scale=-0.5), reads=[("rbc", tb)], writes=[("rbc", tb)])
    for kc in range(8):
        sl = kc % 2
        P.dma("sp", lambda e, kc=kc, sl=sl: e.dma_start(out=xs[:, sl, :], in_=xT[kc * 128:(kc + 1) * 128, :]), writes=[("xs", sl)])
        P.op("dve", lambda e, kc=kc, sl=sl: e.scalar_tensor_tensor(out=hT[:, kc, :], in0=xs[:, sl, :], scalar=g1_s[:, kc:kc + 1], in1=rbc[:], op0=ALU.mult, op1=ALU.mult),
             reads=[("xs", sl), "g1_s"] + [("rbc", tb) for tb in range(4)], writes=[("hT", kc)])
    stA2.close()
    hT_keys = [("hT", kc) for kc in range(8)]
    xz_keys = []
    for i in range(NE):
        P.dma("sp", lambda e, i=i: e.dma_start(out=Xs[i * 256:(i + 1) * 256, :].rearrange("(p b) d -> p (b d)", b=2), in_=zt[:]), reads=["zt"], writes=[("Xz", i)])
        xz_keys.append(("Xz", i))
    P.dma("sp", lambda e: e.dma_start(out=Xs[DUMP:DUMP + 128, :], in_=zt[:, 0:D]), reads=["zt"], writes=[("Xz", NE)])
    xz_keys.append(("Xz", NE))
    P.dma("sp", lambda e: e.dma_start(out=Ys[DUMP:DUMP + 128, :], in_=zt[:, 0:D]), reads=["zt"], writes=[("Yz", 0)])

    pre_list = []
    for e_ in range(NE):
        pre_list.append((Wg_b, w_gate, "g", e_))
        pre_list.append((Wu_b, w_up, "u", e_))
        pre_list.append((Wd_b, w_down, "d", e_))
    pre_i = [0]

    def prestage(n):
        for _ in range(n):
            if pre_i[0] >= len(pre_list):
                return
            dst, src, kd, e_ = pre_list[pre_i[0]]
            pre_i[0] += 1
            P.dma("pool", lambda e, dst=dst, src=src, e_=e_: e.dma_start(out=dst[e_], in_=src[e_]), writes=[("Wb", kd, e_)])

    wr_i = [0]

    def proj_chunk(c, slot, dst=None, dname="pc"):
        if dst is None:
            dst = pc
        ws = wr_i[0] % 2
        wr_i[0] += 1
        P.dma("pool", lambda e: e.dma_start(out=wring[:, ws, :, :], in_=w_in_r[c]), writes=[("wring", ws)])
        if c < 12:
            prestage(1)
        for tb in range(4):
            b = bank()
            for kc in range(8):
                P.op("pe", lambda e, kc=kc, tb=tb, b=b: e.matmul(psv(b), lhsT=wring[:, ws, kc, :], rhs=hT[:, kc, tb * 512:(tb + 1) * 512], start=(kc == 0), stop=(kc == 7)),
                     reads=[("wring", ws), ("hT", kc)], writes=[pk(b)])
            P.op("act", lambda e, tb=tb, b=b: e.copy(out=dst[:, slot, tb * 512:(tb + 1) * 512], in_=psv(b)), reads=[pk(b)], writes=[(dname, slot, tb)])

    def pck(slot):
        return [("pc", slot, tb) for tb in range(4)]

    bb = bank()
    for c in range(NCH):
        for kc in range(8):
            P.op("pe", lambda e, c=c, kc=kc: e.matmul(psum[0:64, bb * 512 + c * 8: bb * 512 + c * 8 + 8], lhsT=hT[:, kc, c * 64:(c + 1) * 64], rhs=wba_s[:, kc, :], start=(kc == 0), stop=(kc == 7)),
                 reads=[("hT", kc), "wba_s"], writes=[pk(bb)])
    P.op("act", lambda e: e.copy(out=tb_ba[:].rearrange("p c k -> p (c k)"), in_=psum[0:64, bb * 512: bb * 512 + 256]), reads=[pk(bb)], writes=["tb_ba"])
    P.op("act", lambda e: e.activation(out=tb_beta[:], in_=tb_ba[:, :, 0:4], func=AF.Sigmoid), reads=["tb_ba"], writes=["tb_beta"])
    P.op("dve", lambda e: e.tensor_scalar(out=tb_nbeta[:], in0=tb_beta[:], scalar1=-1.0, scalar2=None, op0=ALU.mult), reads=["tb_beta"], writes=["tb_nbeta"])
    P.op("dve", lambda e: e.tensor_tensor(out=tb_t0[:], in0=tb_ba[:, :, 4:8], in1=tb_dtb[:], op=ALU.add), reads=["tb_ba", "tb_dtb"], writes=["tb_t0"])
    P.op("act", lambda e: e.activation(out=tb_t1[:], in_=tb_t0[:], func=AF.Abs), reads=["tb_t0"], writes=["tb_t1"])
    P.op("act", lambda e: e.activation(out=tb_t1[:], in_=tb_t1[:], func=AF.Exp, scale=-1.0), reads=["tb_t1"], writes=["tb_t1"])
    P.op("act", lambda e: e.activation(out=tb_t1[:], in_=tb_t1[:], func=AF.Ln, bias=1.0), reads=["tb_t1"], writes=["tb_t1"])
    P.op("dve", lambda e: e.tensor_scalar(out=tb_t0[:], in0=tb_t0[:], scalar1=0.0, scalar2=None, op0=ALU.max), reads=["tb_t0"], writes=["tb_t0"])
    P.op("dve", lambda e: e.tensor_tensor(out=tb_t0[:], in0=tb_t0[:], in1=tb_t1[:], op=ALU.add), reads=["tb_t0", "tb_t1"], writes=["tb_t0"])
    P.op("act", lambda e: e.activation(out=tb_alog[:], in_=tb_alog[:], func=AF.Exp), reads=["tb_alog"], writes=["tb_alog"])
    P.op("dve", lambda e: e.scalar_tensor_tensor(out=tb_g[:], in0=tb_t0[:], scalar=-1.0, in1=tb_alog[:], op0=ALU.mult, op1=ALU.mult), reads=["tb_t0", "tb_alog"], writes=["tb_g"])
    gflat = tb_g[:].rearrange("p c k -> p (c k)")
    b1 = bank()
    P.op("pe", lambda e: e.matmul(psum[0:64, b1 * 512:b1 * 512 + 128], lhsT=u64[:], rhs=gflat, start=True, stop=True), reads=["u64", "tb_g"], writes=[pk(b1)])
    P.op("act", lambda e: e.copy(out=tb_gc[:].rearrange("p c k -> p (c k)"), in_=psum[0:64, b1 * 512:b1 * 512 + 128]), reads=[pk(b1)], writes=["tb_gc"])
    b2 = bank()
    P.op("pe", lambda e: e.matmul(psum[0:128, b2 * 512:b2 * 512 + 128], lhsT=ones_f[0:64, :], rhs=gflat, start=True, stop=True), reads=["ones_f", "tb_g"], writes=[pk(b2)])
    P.op("act", lambda e: e.copy(out=tb_gl[:].rearrange("p c k -> p (c k)"), in_=psum[0:128, b2 * 512:b2 * 512 + 128]), reads=[pk(b2)], writes=["tb_gl"])
    P.op("act", lambda e: e.activation(out=tb_egl[:], in_=tb_gl[:], func=AF.Exp), reads=["tb_gl"], writes=["tb_egl"])
    P.op("act", lambda e: e.activation(out=tb_t1[:], in_=tb_gc[:], func=AF.Exp), reads=["tb_gc"], writes=["tb_t1"])
    P.op("dve", lambda e: e.tensor_tensor(out=tb_kbe[:], in0=tb_beta[:], in1=tb_t1[:], op=ALU.mult), reads=["tb_beta", "tb_t1"], writes=["tb_kbe"])
    P.op("dve", lambda e: e.tensor_tensor(out=tb_kdec[:], in0=tb_gl[0:64], in1=tb_gc[:], op=ALU.subtract), reads=["tb_gl", "tb_gc"], writes=["tb_kdec"])
    P.op("act", lambda e: e.activation(out=tb_kdec[:], in_=tb_kdec[:], func=AF.Exp), reads=["tb_kdec"], writes=["tb_kdec"])

    def conv(eng, dst, dkeys, src, skeys, wtile, wkey, widx, K):
        P.op("act", lambda e: e.activation(out=dst, in_=src, func=AF.Copy, scale=wtile[:, widx, K - 1:K]), reads=skeys + [wkey], writes=dkeys)
        for j in range(K - 1):
            sh = K - 1 - j
            P.op("dve", lambda e, j=j, sh=sh: e.scalar_tensor_tensor(out=dst[:, sh:], in0=src[:, 0:S - sh], scalar=wtile[:, widx, j:j + 1], in1=dst[:, sh:], op0=ALU.mult, op1=ALU.add),
                 reads=skeys + [wkey] + dkeys, writes=dkeys)

    sq_i = [0]

    def inv_rms(src, skeys, lhs, lkey, scale):
        sl = sq_i[0] % 2
        sq_i[0] += 1
        P.op("act", lambda e: e.activation(out=sqb[:, sl, :], in_=src, func=AF.Square), reads=skeys, writes=[("sqb", sl)])
        for tb in range(4):
            b = bank()
            P.op("pe", lambda e, tb=tb, b=b: e.matmul(psv(b), lhsT=lhs, rhs=sqb[:, sl, tb * 512:(tb + 1) * 512], start=True, stop=True), reads=[("sqb", sl), lkey], writes=[pk(b)])
            P.op("act", lambda e, tb=tb, b=b: e.activation(out=cvt[:, tb * 512:(tb + 1) * 512], in_=psv(b), func=AF.Ln, scale=scale, bias=EPS), reads=[pk(b)], writes=["cvt"])
        P.op("act", lambda e: e.activation(out=cvt[:], in_=cvt[:], func=AF.Exp, scale=-0.5), reads=["cvt"], writes=["cvt"])

    stMA = ExitStack()
    pcx = sb(stMA, "pcx", [128, 3, S], F32)

    def mixer_unit(j):
        buf, nm = (pc, "pc") if j % 2 == 0 else (pcx, "pcx")

        def kk(i):
            return [(nm, i, tb) for tb in range(4)]

        proj_chunk(j, 0, buf, nm)
        yield
        proj_chunk(8 + j, 2, buf, nm)
        yield
        proj_chunk(4 + j, 1, buf, nm)
        yield
        P.op("dve", lambda e: e.tensor_tensor(out=buf[:, 0, :], in0=buf[:, 0, :], in1=buf[:, 2, :], op=ALU.mult), reads=kk(0) + kk(2), writes=kk(0))
        conv("pool", buf[:, 2, :], kk(2), buf[:, 0, :], kk(0), caw_s, "caw_s", j, 3)
        yield
        P.op("dve", lambda e: e.tensor_tensor(out=buf[:, 2, :], in0=buf[:, 2, :], in1=buf[:, 1, :], op=ALU.mult), reads=kk(1) + kk(2), writes=kk(2))
        inv_rms(buf[:, 2, :], kk(2), bd64_b[:], "bd64_b", 1.0 / 64)
        yield
        P.op("dve", lambda e: e.scalar_tensor_tensor(out=yT[:, j, :], in0=buf[:, 2, :], scalar=gA_s[:, j:j + 1], in1=cvt[:], op0=ALU.mult, op1=ALU.mult),
             reads=kk(2) + ["gA_s", "cvt"], writes=[("yT", j)])

    gens_ = [mixer_unit(j) for j in range(4)]
    active_ = []
    nxt_ = 0
    rnd_ = 0
    while active_ or nxt_ < 4:
        if nxt_ < 4 and len(active_) < 2 and (not active_ or rnd_ >= 3):
            active_.append(gens_[nxt_])
            nxt_ += 1
            rnd_ = 0
        rnd_ += 1
        for g_ in list(active_):
            try:
                next(g_)
            except StopIteration:
                active_.remove(g_)
    stMA.close()
    P.barrier()

    dn = ExitStack()
    GW = 8
    NSET = 2
    qb = sb(dn, "qb", [128, S], BF16)
    kb = sb(dn, "kb", [128, S], BF16)
    vb = sb(dn, "vb", [128, S], BF16)
    NPAR = 3
    ATg = sb(dn, "ATg", [64, NPAR, GW, 64], BF16)
    Kdg = sb(dn, "Kdg", [64, NPAR, GW, 128], BF16)
    Ug = sb(dn, "Ug", [64, NPAR, GW, 128], F32)
    WTg = sb(dn, "WTg", [128, NPAR, GW * 64], BF16)
    qsb = sb(dn, "qsb", [128, S], BF16)
    Sst = sb(dn, "Sst", [128, 2, 128], F32)
    Sb = sb(dn, "Sb", [128, 2, 128], BF16)
    vnew = sb(dn, "vnew", [64, 2, 128], BF16)
    Og = sb(dn, "Og", [64, 4, 128], F32)
    Ogb = sb(dn, "Ogb", [64, 4, 128], BF16)
    Osq = sb(dn, "Osq", [64, 4, 128], F32)
    oss = sb(dn, "oss", [64, 4], F32)

    SC = []
    s0 = {"id": 0}
    s0["GU"] = sb(dn, "s0_GU", [64, GW, 64], F32)[:]
    s0["E"] = sb(dn, "s0_E", [128, GW * 64], F32)[:]
    for nm in ("DS", "DC", "A", "N0", "B0", "N1", "B1", "R0", "R1"):
        s0[nm] = sb(dn, "s0_" + nm, [64, GW, 64], BF16)[:]
    s0["Kbe"] = sb(dn, "s0_Kbe", [64, GW, 128], BF16)[:]
    s0["Vb"] = sb(dn, "s0_Vb", [64, GW, 128], BF16)[:]
    SC.append(s0)

    def bfv(ap, c):
        return ap.bitcast(BF16).rearrange("p (a c) -> p a c", c=c)

    s1 = {"id": 1}
    s1["GU"] = pc[0:64, 1, 0:512].rearrange("p (a c) -> p a c", c=64)
    s1["E"] = pc[:, 1, 512:1024]
    for i_, nm in enumerate(("DS", "DC", "A", "N0")):
        s1[nm] = bfv(pc[0:64, 1, 1024 + 256 * i_:1280 + 256 * i_], 64)
    for i_, nm in enumerate(("B0", "N1", "B1", "R0", "R1")):
        s1[nm] = bfv(pc[0:64, 2, 256 * i_:256 * (i_ + 1)], 64)
    s1["Kbe"] = bfv(pc[0:64, 2, 1280:1792], 128)
    s1["Vb"] = bfv(cvt[0:64, 0:512], 128)
    SC.append(s1)

    def ops512(v):
        return v.rearrange("p a c -> p (a c)")

    def dn_phase1(h, cg, par, Sx):
        sid = Sx["id"]

        def K(nm):
            ks_ = [(nm, sid)]
            if sid == 1:
                ks_.append("alias1")
            return ks_

        def KW(nm):
            return [(nm, sid)]

        qs = pc[:, 0, :]
        c0 = cg * GW
        cols = slice(c0 * 64, (c0 + GW) * 64)
        qk = [("pc", 0, cg)]
        GU, DS, DC, A, E_s, Kbe, Vb = Sx["GU"], Sx["DS"], Sx["DC"], Sx["A"], Sx["E"], Sx["Kbe"], Sx["Vb"]
        al = ["alias1"] if sid == 1 else []
        P.op("pool", lambda e: e.tensor_tensor(out=GU[:], in0=u64[:, None, :].to_broadcast([64, GW, 64]), in1=tb_g[:, c0:c0 + GW, h:h + 1].to_broadcast([64, GW, 64]), op=ALU.mult),
             reads=["u64", "tb_g"] + al, writes=KW("GU"))
        bg = bank()
        P.op("pe", lambda e: e.matmul(psv(bg), lhsT=ones_f[0:64, :], rhs=ops512(GU[:]), start=True, stop=True), reads=["ones_f"] + K("GU"), writes=[pk(bg)])
        P.op("dve", lambda e: e.tensor_tensor(out=GU[:], in0=psv(bg, 64).rearrange("p (a c) -> p a c", c=64), in1=tb_gc[:, c0:c0 + GW, h:h + 1].to_broadcast([64, GW, 64]), op=ALU.subtract),
             reads=[pk(bg), "tb_gc"] + al, writes=KW("GU"))
        P.op("dve", lambda e: e.tensor_scalar(out=GU[:], in0=GU[:], scalar1=0.0, scalar2=None, op0=ALU.max), reads=K("GU"), writes=KW("GU"))
        P.op("act", lambda e: e.activation(out=GU[:], in_=GU[:], func=AF.Exp, scale=-1.0), reads=K("GU"), writes=KW("GU"))
        P.op("act", lambda e: e.activation(out=E_s[:], in_=psv(bg), func=AF.Exp), reads=[pk(bg)] + al, writes=KW("E"))
        yield
        P.op("pool", lambda e: e.tensor_tensor(out=DS[:], in0=GU[:], in1=masks8[:], op=ALU.mult), reads=K("GU") + ["masks8"], writes=KW("DS"))
        P.op("pool", lambda e: e.tensor_tensor(out=DS[:], in0=DS[:], in1=tb_nbeta[:, c0:c0 + GW, h:h + 1].to_broadcast([64, GW, 64]), op=ALU.mult), reads=K("DS") + ["tb_nbeta"], writes=KW("DS"))
        P.op("pool", lambda e: e.tensor_tensor(out=DC[:], in0=GU[:], in1=maskc8[:], op=ALU.mult), reads=K("GU") + ["maskc8"], writes=KW("DC"))
        N0, B0 = Sx["N0"], Sx["B0"]
        bk = bank()
        for a in range(GW):
            cs = slice((c0 + a) * 64, (c0 + a + 1) * 64)
            P.op("pe", lambda e, a=a, cs=cs: e.matmul(psum[0:64, bk * 512 + a * 64: bk * 512 + (a + 1) * 64], lhsT=kb[:, cs], rhs=kb[:, cs], start=True, stop=True), reads=["kb"], writes=[pk(bk)])
        P.op("dve", lambda e: e.tensor_tensor(out=ops512(N0[:]), in0=psv(bk, 64), in1=ops512(DS[:]), op=ALU.mult), reads=[pk(bk)] + K("DS"), writes=KW("N0"))
        yield
        bq = bank()
        for a in range(GW):
            cs = slice((c0 + a) * 64, (c0 + a + 1) * 64)
            P.op("pe", lambda e, a=a, cs=cs: e.matmul(psum[0:64, bq * 512 + a * 64: bq * 512 + (a + 1) * 64], lhsT=qb[:, cs], rhs=kb[:, cs], start=True, stop=True), reads=["qb", "kb"], writes=[pk(bq)])
        P.op("dve", lambda e: e.tensor_tensor(out=ops512(A[:]), in0=psv(bq, 64), in1=ops512(DC[:]), op=ALU.mult), reads=[pk(bq)] + K("DC"), writes=KW("A"))
        P.op("pool", lambda e: e.tensor_tensor(out=qsb[:, cols], in0=qs[:, cols], in1=E_s[:], op=ALU.mult), reads=qk + K("E"), writes=[("qsb", cg)])
        yield
        bt = bank()
        for a in range(GW):
            P.op("pe", lambda e, a=a: e.matmul(psum[0:64, bt * 512 + a * 64: bt * 512 + (a + 1) * 64], lhsT=N0[:, a, :], rhs=ident_b[0:64, 0:64], start=True, stop=True), reads=K("N0") + ["ident_b"], writes=[pk(bt)])
        P.op("act", lambda e: e.copy(out=ops512(B0[:]), in_=psv(bt, 64)), reads=[pk(bt)] + al, writes=KW("B0"))
        yield
        ba_ = bank()
        for a in range(GW):
            P.op("pe", lambda e, a=a: e.matmul(psum[0:64, ba_ * 512 + a * 64: ba_ * 512 + (a + 1) * 64], lhsT=A[:, a, :], rhs=ident_b[0:64, 0:64], start=True, stop=True), reads=K("A") + ["ident_b"], writes=[pk(ba_)])
        P.op("act", lambda e: e.copy(out=ops512(ATg[:, par]), in_=psv(ba_, 64)), reads=[pk(ba_)], writes=[("ATg", par)])
        R = [Sx["R0"], Sx["R1"]]
        Nn = [Sx["N0"], Sx["N1"]]
        Bn = [Sx["B0"], Sx["B1"]]
        P.op("pool", lambda e: e.tensor_tensor(out=R[0][:], in0=B0[:], in1=eye8[:], op=ALU.add), reads=K("B0") + ["eye8"], writes=KW("R0"))
        yield
        cur = 0
        for lvl in range(5):
            nxt = 1 - cur
            nk, bkk = "N%d" % cur, "B%d" % cur
            nk2, bk2 = "N%d" % nxt, "B%d" % nxt
            rk, rk2 = "R%d" % cur, "R%d" % nxt
            pn = bank()
            for a in range(GW):
                P.op("pe", lambda e, a=a, cur=cur, pn=pn: e.matmul(psum[0:64, pn * 512 + a * 64: pn * 512 + (a + 1) * 64], lhsT=Bn[cur][:, a, :], rhs=Nn[cur][:, a, :], start=True, stop=True),
                     reads=K(nk) + K(bkk), writes=[pk(pn)])
            P.op("act", lambda e, nxt=nxt, pn=pn: e.copy(out=ops512(Nn[nxt][:]), in_=psv(pn, 64)), reads=[pk(pn)] + al, writes=KW(nk2))
            yield
            if lvl < 4:
                pb = bank()
                for a in range(GW):
                    P.op("pe", lambda e, a=a, cur=cur, pb=pb: e.matmul(psum[0:64, pb * 512 + a * 64: pb * 512 + (a + 1) * 64], lhsT=Nn[cur][:, a, :], rhs=Bn[cur][:, a, :], start=True, stop=True),
                         reads=K(nk) + K(bkk), writes=[pk(pb)])
                P.op("act", lambda e, nxt=nxt, pb=pb: e.copy(out=ops512(Bn[nxt][:]), in_=psv(pb, 64)), reads=[pk(pb)] + al, writes=KW(bk2))
                yield
            pr = bank()
            for a in range(GW):
                P.op("pe", lambda e, a=a, cur=cur, nxt=nxt, pr=pr: e.matmul(psum[0:64, pr * 512 + a * 64: pr * 512 + (a + 1) * 64], lhsT=Nn[nxt][:, a, :], rhs=R[cur][:, a, :], start=True, stop=True),
                     reads=K(nk2) + K(rk), writes=[pk(pr)])
            P.op("dve", lambda e, cur=cur, nxt=nxt, pr=pr: e.tensor_tensor(out=ops512(R[nxt][:]), in0=psv(pr, 64), in1=ops512(R[cur][:]), op=ALU.add), reads=[pk(pr)] + K(rk), writes=KW(rk2))
            cur = nxt
            yield
        Rf = R[cur]
        rfk = "R%d" % cur
        bkt = bank(2)
        for a in range(GW):
            cs = slice((c0 + a) * 64, (c0 + a + 1) * 64)
            P.op("pe", lambda e, a=a, cs=cs: e.matmul(psum[0:64, bkt * 512 + a * 128: bkt * 512 + (a + 1) * 128], lhsT=kb[:, cs], rhs=ident_b[:], start=True, stop=True), reads=["kb", "ident_b"], writes=[pk(bkt), pk(bkt + 1)])
        kt3 = psum[0:64, bkt * 512:(bkt + 2) * 512].rearrange("p (a c) -> p a c", c=128)
        P.op("dve", lambda e: e.tensor_tensor(out=Kbe[:], in0=kt3, in1=tb_kbe[:, c0:c0 + GW, h:h + 1].to_broadcast([64, GW, 128]), op=ALU.mult), reads=[pk(bkt), pk(bkt + 1), "tb_kbe"] + al, writes=KW("Kbe"))
        P.op("dve", lambda e: e.tensor_tensor(out=Kdg[:, par], in0=kt3, in1=tb_kdec[:, c0:c0 + GW, h:h + 1].to_broadcast([64, GW, 128]), op=ALU.mult), reads=[pk(bkt), pk(bkt + 1), "tb_kdec"], writes=[("Kdg", par)])
        yield
        bvt = bank(2)
        for a in range(GW):
            cs = slice((c0 + a) * 64, (c0 + a + 1) * 64)
            P.op("pe", lambda e, a=a, cs=cs: e.matmul(psum[0:64, bvt * 512 + a * 128: bvt * 512 + (a + 1) * 128], lhsT=vb[:, cs], rhs=ident_b[:], start=True, stop=True), reads=["vb", "ident_b"], writes=[pk(bvt), pk(bvt + 1)])
        vt3 = psum[0:64, bvt * 512:(bvt + 2) * 512].rearrange("p (a c) -> p a c", c=128)
        P.op("dve", lambda e: e.tensor_tensor(out=Vb[:], in0=vt3, in1=tb_beta[:, c0:c0 + GW, h:h + 1].to_broadcast([64, GW, 128]), op=ALU.mult), reads=[pk(bvt), pk(bvt + 1), "tb_beta"] + al, writes=KW("Vb"))
        yield
        bu = bank(2)
        for a in range(GW):
            P.op("pe", lambda e, a=a: e.matmul(psum[0:64, bu * 512 + a * 128: bu * 512 + (a + 1) * 128], lhsT=Rf[:, a, :], rhs=Vb[:, a, :], start=True, stop=True), reads=K(rfk) + K("Vb"), writes=[pk(bu), pk(bu + 1)])
        P.op("act", lambda e: e.copy(out=Ug[:, par].rearrange("p a c -> p (a c)"), in_=psum[0:64, bu * 512:(bu + 2) * 512]), reads=[pk(bu), pk(bu + 1)], writes=[("Ug", par)])
        yield
        bw = bank()
        for a in range(GW):
            P.op("pe", lambda e, a=a: e.matmul(psum[0:128, bw * 512 + a * 64: bw * 512 + (a + 1) * 64], lhsT=Kbe[:, a, :], rhs=Rf[:, a, :], start=True, stop=True), reads=K(rfk) + K("Kbe"), writes=[pk(bw)])
        P.op("act", lambda e: e.copy(out=WTg[:, par, :], in_=psv(bw)), reads=[pk(bw)], writes=[("WTg", par)])
        yield

    s_par = [0]

    def dn_scan(h, cg, par):
        zs = pc[:, 3, :]
        c0 = cg * GW
        for a in range(GW):
            c = c0 + a
            cs = slice(c * 64, (c + 1) * 64)
            sp_, sn_ = s_par[0], 1 - s_par[0]
            vp = a % 2
            bw = bank()
            P.op("pe", lambda e, a=a, sp_=sp_, bw=bw: e.matmul(psum[0:64, bw * 512: bw * 512 + 128], lhsT=WTg[:, par, a * 64:(a + 1) * 64], rhs=Sb[:, sp_, :], start=True, stop=True),
                 reads=[("WTg", par), ("Sb", sp_)], writes=[pk(bw)])
            P.op("dve", lambda e, a=a, vp=vp, bw=bw: e.tensor_tensor(out=vnew[:, vp, :], in0=Ug[:, par, a, :], in1=psum[0:64, bw * 512: bw * 512 + 128], op=ALU.subtract),
                 reads=[("Ug", par), pk(bw)], writes=[("vnew", vp)])
            yield
            bs = bank()
            P.op("pe", lambda e, a=a, vp=vp, bs=bs: e.matmul(psum[0:128, bs * 512: bs * 512 + 128], lhsT=Kdg[:, par, a, :], rhs=vnew[:, vp, :], start=True, stop=True),
                 reads=[("Kdg", par), ("vnew", vp)], writes=[pk(bs)])
            oq = a % 4
            bo = 6 + ((c // 4) % 2)
            P.op("pe", lambda e, cs=cs, sp_=sp_, bo=bo, oq=oq: e.matmul(psum[0:64, bo * 512 + oq * 128: bo * 512 + (oq + 1) * 128], lhsT=qsb[:, cs], rhs=Sb[:, sp_, :], start=True, stop=False),
                 reads=[("qsb", cg), ("Sb", sp_)], writes=[pk(bo)])
            P.op("pe", lambda e, a=a, vp=vp, bo=bo, oq=oq: e.matmul(psum[0:64, bo * 512 + oq * 128: bo * 512 + (oq + 1) * 128], lhsT=ATg[:, par, a, :], rhs=vnew[:, vp, :], start=False, stop=True),
                 reads=[("ATg", par), ("vnew", vp)], writes=[pk(bo)])
            P.op("dve", lambda e, c=c, sp_=sp_, sn_=sn_, bs=bs: e.scalar_tensor_tensor(out=Sb[:, sn_, :], in0=Sst[:, sp_, :], scalar=tb_egl[:, c, h:h + 1], in1=psum[0:128, bs * 512: bs * 512 + 128], op0=ALU.mult, op1=ALU.add),
                 reads=[("S", sp_), "tb_egl", pk(bs)], writes=[("Sb", sn_)])
            P.op("dve", lambda e, c=c, sp_=sp_, sn_=sn_, bs=bs: e.scalar_tensor_tensor(out=Sst[:, sn_, :], in0=Sst[:, sp_, :], scalar=tb_egl[:, c, h:h + 1], in1=psum[0:128, bs * 512: bs * 512 + 128], op0=ALU.mult, op1=ALU.add),
                 reads=[("S", sp_), "tb_egl", pk(bs)], writes=[("S", sn_)])
            s_par[0] = sn_
            if oq == 3:
                c4 = c - 3
                P.op("act", lambda e, bo=bo: e.copy(out=Og[:].rearrange("p a c -> p (a c)"), in_=psv(bo, 64)), reads=[pk(bo)], writes=["Og"])
                P.op("pool", lambda e: e.tensor_tensor(out=Osq[:], in0=Og[:], in1=Og[:], op=ALU.mult), reads=["Og"], writes=["Osq"])
                P.op("dve", lambda e: e.tensor_reduce(out=oss[:], in_=Osq[:], axis=AX.X, op=ALU.add), reads=["Osq"], writes=["oss"])
                P.op("act", lambda e: e.activation(out=oss[:], in_=oss[:], func=AF.Sqrt, scale=1.0 / 128, bias=EPS), reads=["oss"], writes=["oss"])
                P.op("dve", lambda e: e.reciprocal(out=oss[:], in_=oss[:]), reads=["oss"], writes=["oss"])
                P.op("pool", lambda e: e.tensor_tensor(out=Ogb[:], in0=Og[:], in1=oss[:, :, None].to_broadcast([64, 4, 128]), op=ALU.mult), reads=["Og", "oss"], writes=["Ogb"])
                bt = bank()
                for q4 in range(4):
                    P.op("pe", lambda e, q4=q4, bt=bt: e.matmul(psum[0:128, bt * 512 + q4 * 64: bt * 512 + (q4 + 1) * 64], lhsT=Ogb[:, q4, :], rhs=ident_b[0:64, 0:64], start=True, stop=True), reads=["Ogb", "ident_b"], writes=[pk(bt)])
                P.op("dve", lambda e, c4=c4, bt=bt: e.tensor_tensor(out=yT[:, 4 + h, c4 * 64:(c4 + 4) * 64], in0=psum[0:128, bt * 512: bt * 512 + 256], in1=zs[:, c4 * 64:(c4 + 4) * 64], op=ALU.mult),
                     reads=[pk(bt), ("pc", 3, cg)], writes=[("yT", 4 + h)])
            yield

    for h in range(4):
        bank_lim[0] = 8
        proj_chunk(12 + h, 0)
        proj_chunk(16 + h, 1)
        proj_chunk(20 + h, 2)
        proj_chunk(24 + h, 3)
        for s_, m_ in ((0, h), (1, 4 + h), (2, 8 + h)):
            conv("pool", cvt[:], ["cvt"], pc[:, s_, :], pck(s_), dcw_s, "dcw_s", m_, 4)
            if s_ == 2:
                P.op("act", lambda e: e.activation(out=vb[:], in_=cvt[:], func=AF.Silu), reads=["cvt"], writes=["vb"])
            else:
                P.op("act", lambda e, s_=s_: e.activation(out=pc[:, s_, :], in_=cvt[:], func=AF.Silu), reads=["cvt"], writes=pck(s_))
        inv_rms(pc[:, 0, :], pck(0), ones_b[:], "ones_b", 1.0)
        P.op("dve", lambda e: e.scalar_tensor_tensor(out=pc[:, 0, :], in0=pc[:, 0, :], scalar=128 ** -0.5, in1=cvt[:], op0=ALU.mult, op1=ALU.mult), reads=pck(0) + ["cvt"], writes=pck(0))
        P.op("act", lambda e: e.copy(out=qb[:], in_=pc[:, 0, :]), reads=pck(0), writes=["qb"])
        inv_rms(pc[:, 1, :], pck(1), ones_b[:], "ones_b", 1.0)
        P.op("dve", lambda e: e.tensor_tensor(out=kb[:], in0=pc[:, 1, :], in1=cvt[:], op=ALU.mult), reads=pck(1) + ["cvt"], writes=["kb"])
        P.op("act", lambda e: e.activation(out=pc[:, 3, :], in_=pc[:, 3, :], func=AF.Silu), reads=pck(3), writes=pck(3))
        P.op("dve", lambda e: e.tensor_scalar(out=pc[:, 3, :], in0=pc[:, 3, :], scalar1=gdn_s[:, 0:1], scalar2=None, op0=ALU.mult), reads=pck(3) + ["gdn_s"], writes=pck(3))
        P.op("pool", lambda e: e.memset(Sst[:, 0, :], 0.0), writes=[("S", 0)])
        P.op("pool", lambda e: e.memset(Sb[:, 0, :], 0.0), writes=[("Sb", 0)])
        s_par[0] = 0
        ngr = NCH // GW
        bank_lim[0] = 6
        bank_rr[0] = 0
        P.op("pool", lambda e: e.memset(oss[:], 0.0), reads=[], writes=pck(1) + pck(2) + ["cvt", "alias1", "oss"])
        pending = list(range(ngr))
        free_sets = list(range(NSET))
        running = []
        p1_done = set()
        scan_next = 0
        scan_running = False
        rnd = 0
        while pending or running or scan_next < ngr:
            rnd += 1
            if rnd % 4 == 0:
                prestage(1)
            while pending and free_sets and pending[0] - NPAR < scan_next:
                cg = pending.pop(0)
                si = free_sets.pop(0)
                running.append(["p1", cg, dn_phase1(h, cg, cg % NPAR, SC[si]), si])
            if not scan_running and scan_next < ngr and scan_next in p1_done:
                running.append(["scan", scan_next, dn_scan(h, scan_next, scan_next % NPAR), None])
                scan_running = True
            for r in list(running):
                try:
                    next(r[2])
                except StopIteration:
                    running.remove(r)
                    if r[0] == "p1":
                        free_sets.append(r[3])
                        p1_done.add(r[1])
                    else:
                        scan_next += 1
                        scan_running = False
        P.op("pool", lambda e: e.memset(oss[:], 0.0), reads=[], writes=pck(1) + pck(2) + ["cvt", "alias1", "oss"])
    prestage(len(pre_list))
    print("prestage DMAs issued before flush point; total", pre_i[0])
    dn.close()
    stA.close()
    bank_lim[0] = 8
    P.barrier()

    stB = ExitStack()
    x1 = sb(stB, "x1", [128, NT, D], F32)
    stW = ExitStack()
    wout_s = sb(stW, "wout_s", [128, 8, D], BF16)
    for kc in range(8):
        P.dma("pool", lambda e, kc=kc: e.dma_start(out=wout_s[:, kc, :], in_=w_out_r[:, kc, :]), writes=[("wout", kc)])
    for t in range(NT):
        P.dma("sp", lambda e, t=t: e.dma_start(out=x1[:, t, :], in_=x_tok[t * 128:(t + 1) * 128, :]), writes=[("x1", t)])
    for t in range(NT):
        for dh in range(2):
            b = bank()
            for kc in range(8):
                P.op("pe", lambda e, t=t, dh=dh, kc=kc, b=b: e.matmul(psv(b), lhsT=yT[:, kc, t * 128:(t + 1) * 128], rhs=wout_s[:, kc, dh * 512:(dh + 1) * 512], start=(kc == 0), stop=(kc == 7)),
                     reads=[("yT", kc), ("wout", kc)], writes=[pk(b)])
            P.op("dve", lambda e, t=t, dh=dh, b=b: e.tensor_tensor(out=x1[:, t, dh * 512:(dh + 1) * 512], in0=x1[:, t, dh * 512:(dh + 1) * 512], in1=psv(b), op=ALU.add),
                 reads=[pk(b), ("x1", t)], writes=[("x1", t)])
    stW.close()

    if stage == "A":
        for t in range(NT):
            P.dma("sp", lambda e, t=t: e.dma_start(out=out[t * 128:(t + 1) * 128, :], in_=x1[:, t, :]), reads=[("x1", t)], writes=[("out", t)])
        P.finish([("out", t) for t in range(NT)])
        P.emit()
        return nc


    stackR.close()
    P.barrier()

    BIG = 1.0e30

    g2bc = sb(stB, "g2bc", [128, D], F32)
    g3bc = sb(stB, "g3bc", [128, D], F32)
    wr_s = sb(stB, "wr_s", [128, 8, 36], F32)
    ecap = sb(stB, "ecap", [128, NE], F32)
    ecap_i = sb(stB, "ecap_i", [128, NE], I32)
    ustr_b = sb(stB, "ustr_b", [128, 128], BF16)
    d1i = sb(stB, "d1i", [128, NT], I32)
    d2i = sb(stB, "d2i", [128, NT], I32)
    gt1 = sb(stB, "gt1", [128, NT], F32)
    gt2 = sb(stB, "gt2", [128, NT], F32)
    P.dma("sp", lambda e: e.dma_start(out=g2bc[:], in_=bass.AP(g2_h, 0, [[0, 128], [1, D]])), writes=["g2bc"])
    P.dma("sp", lambda e: e.dma_start(out=g3bc[:], in_=bass.AP(g3_h, 0, [[0, 128], [1, D]])), writes=["g3bc"])
    P.dma("sp", lambda e: e.dma_start(out=wr_s[:], in_=wr_r), writes=["wr_s"])
    P.op("pool", lambda e: e.iota(ecap_i[:], [[CAP, NE]], base=0, channel_multiplier=0), writes=["ecap_i"])
    P.op("dve", lambda e: e.tensor_copy(out=ecap[:], in_=ecap_i[:]), reads=["ecap_i"], writes=["ecap"])
    P.op("pool", lambda e: e.memset(ustr_b[:], 1.0), writes=["ustr_b"])
    P.op("pool", lambda e: e.affine_select(out=ustr_b[:], in_=ustr_b[:], pattern=[[1, 128]], compare_op=ALU.is_gt, fill=0.0, base=0, channel_multiplier=-1), reads=["ustr_b"], writes=["ustr_b"])

    rt = ExitStack()
    h2b = sb(rt, "h2b", [128, NT, D], BF16)
    h2f = sb(rt, "h2f", [128, 3, D], F32)
    h2lo = sb(rt, "h2lo", [128, 3, D], BF16)
    hT2 = sb(rt, "hT2", [128, 3, 2, 8, 128], BF16)
    wr_hi = sb(rt, "wr_hi", [128, 8, 36], BF16)
    wr_lo = sb(rt, "wr_lo", [128, 8, 36], BF16)
    ssq = sb(rt, "ssq", [128, NT], F32)
    Lall = sb(rt, "Lall", [128, NT, 36], F32)
    r_gmax = sb(rt, "r_gmax", [128, NT], F32)
    r_gmask = sb(rt, "r_gmask", [128, NT, 4], F32)
    r_ge = sb(rt, "r_ge", [128, NT, 4], F32)
    r_gp = sb(rt, "r_gp", [128, NT], F32)
    r_pen = sb(rt, "r_pen", [128, NT, 4], F32)
    r_el = sb(rt, "r_el", [128, NT, 32], F32)
    r_el2 = sb(rt, "r_el2", [128, NT, 32], F32)
    r_m1 = sb(rt, "r_m1", [128, NT], F32)
    r_m2 = sb(rt, "r_m2", [128, NT], F32)
    r_mask1 = sb(rt, "r_mask1", [128, NT, 32], F32)
    r_mask2 = sb(rt, "r_mask2", [128, NT, 32], F32)
    r_m12b = sb(rt, "r_m12b", [128, NT, 32], BF16)
    r_e21 = sb(rt, "r_e21", [128, NT], F32)
    r_rank = sb(rt, "r_rank", [128, NT, 32], F32)
    r_valid = sb(rt, "r_valid", [128, NT, 32], F32)
    r_slot = sb(rt, "r_slot", [128, NT, 32], F32)
    r_tmp = sb(rt, "r_tmp", [128, NT, 32], F32)
    r_d1f = sb(rt, "r_d1f", [128, NT], F32)
    r_d2f = sb(rt, "r_d2f", [128, NT], F32)
    r_v1 = sb(rt, "r_v1", [128, NT], F32)
    r_v2 = sb(rt, "r_v2", [128, NT], F32)

    ssq_keys = [("ssq", t) for t in range(NT)]
    lall_keys = [("Lall", t) for t in range(NT)]
    P.op("pool", lambda e: e.memset(ssq[:], 0.0), writes=ssq_keys)
    P.op("act", lambda e: e.copy(out=wr_hi[:], in_=wr_s[:]), reads=["wr_s"], writes=["wr_hi"])
    P.op("dve", lambda e: e.tensor_tensor(out=wr_lo[:], in0=wr_s[:], in1=wr_hi[:], op=ALU.subtract), reads=["wr_s", "wr_hi"], writes=["wr_lo"])

    def r1_tile(t):
        sl = t % 3
        P.op("act", lambda e: e.activation(out=h2f[:, sl, :], in_=x1[:, t, :], func=AF.Square, accum_out=ssq[:, t:t + 1]), reads=[("x1", t), ("ssq", t)], writes=[("h2f", sl), ("ssq", t)])
        P.op("act", lambda e: e.activation(out=ssq[:, t:t + 1], in_=ssq[:, t:t + 1], func=AF.Sqrt, scale=1.0 / D, bias=EPS), reads=[("ssq", t)], writes=[("ssq", t)])
        P.op("dve", lambda e: e.reciprocal(out=ssq[:, t:t + 1], in_=ssq[:, t:t + 1]), reads=[("ssq", t)], writes=[("ssq", t)])
        P.op("dve", lambda e: e.scalar_tensor_tensor(out=h2f[:, sl, :], in0=x1[:, t, :], scalar=ssq[:, t:t + 1], in1=g2bc[:], op0=ALU.mult, op1=ALU.mult),
             reads=[("x1", t), ("ssq", t), "g2bc"], writes=[("h2f", sl)])
        P.op("act", lambda e: e.copy(out=h2b[:, t, :], in_=h2f[:, sl, :]), reads=[("h2f", sl)], writes=[("h2b", t)])
        P.op("pool", lambda e: e.tensor_tensor(out=h2lo[:, sl, :], in0=h2f[:, sl, :], in1=h2b[:, t, :], op=ALU.subtract), reads=[("h2f", sl), ("h2b", t)], writes=[("h2lo", sl)])
        yield
        b0 = bank(2)
        for kc in range(8):
            P.op("pe", lambda e, kc=kc: e.matmul(psum[0:128, b0 * 512 + kc * 128: b0 * 512 + (kc + 1) * 128], lhsT=h2b[:, t, kc * 128:(kc + 1) * 128], rhs=ident_b[:], start=True, stop=True),
                 reads=[("h2b", t), "ident_b"], writes=[pk(b0), pk(b0 + 1)])
        P.op("dve", lambda e: e.tensor_copy(out=hT2[:, sl, 0].rearrange("p k c -> p (k c)"), in_=psum[0:128, b0 * 512:(b0 + 2) * 512]), reads=[pk(b0), pk(b0 + 1)], writes=[("hT2", sl, 0)])
        b2 = bank(2)
        for kc in range(8):
            P.op("pe", lambda e, kc=kc: e.matmul(psum[0:128, b2 * 512 + kc * 128: b2 * 512 + (kc + 1) * 128], lhsT=h2lo[:, sl, kc * 128:(kc + 1) * 128], rhs=ident_b[:], start=True, stop=True),
                 reads=[("h2lo", sl), "ident_b"], writes=[pk(b2), pk(b2 + 1)])
        P.op("dve", lambda e: e.tensor_copy(out=hT2[:, sl, 1].rearrange("p k c -> p (k c)"), in_=psum[0:128, b2 * 512:(b2 + 2) * 512]), reads=[pk(b2), pk(b2 + 1)], writes=[("hT2", sl, 1)])
        yield
        bl = bank()
        n_ = 0
        for kc in range(8):
            for hl, wt, wk in ((0, wr_hi, "wr_hi"), (1, wr_hi, "wr_hi"), (0, wr_lo, "wr_lo")):
                P.op("pe", lambda e, kc=kc, hl=hl, wt=wt, n_=n_: e.matmul(psum[0:128, bl * 512: bl * 512 + 36], lhsT=hT2[:, sl, hl, kc, :], rhs=wt[:, kc, :], start=(n_ == 0), stop=(n_ == 23)),
                     reads=[("hT2", sl, hl), wk], writes=[pk(bl)])
                n_ += 1
        P.op("act", lambda e: e.copy(out=Lall[:, t, :], in_=psum[0:128, bl * 512: bl * 512 + 36]), reads=[pk(bl)], writes=[("Lall", t)])
        yield

    gens_ = [r1_tile(t) for t in range(NT)]
    active_ = []
    nxt_ = 0
    while active_ or nxt_ < NT:
        if nxt_ < NT and len(active_) < 3:
            active_.append(gens_[nxt_])
            nxt_ += 1
        for g_ in list(active_):
            try:
                next(g_)
            except StopIteration:
                active_.remove(g_)

    GLv = Lall[:, :, 0:4]
    ELv = Lall[:, :, 4:36]

    def bc(ap, shape):
        return ap.to_broadcast(shape)

    P.op("dve", lambda e: e.tensor_reduce(out=r_gmax[:], in_=GLv, axis=AX.X, op=ALU.max), reads=lall_keys, writes=["r_gmax"])
    P.op("dve", lambda e: e.tensor_tensor(out=r_gmask[:], in0=GLv, in1=bc(r_gmax[:, :, None], [128, NT, 4]), op=ALU.is_equal), reads=lall_keys + ["r_gmax"], writes=["r_gmask"])
    P.op("dve", lambda e: e.tensor_tensor(out=r_ge[:], in0=GLv, in1=bc(r_gmax[:, :, None], [128, NT, 4]), op=ALU.subtract), reads=lall_keys + ["r_gmax"], writes=["r_ge"])
    P.op("act", lambda e: e.activation(out=r_ge[:], in_=r_ge[:], func=AF.Exp), reads=["r_ge"], writes=["r_ge"])
    P.op("dve", lambda e: e.tensor_reduce(out=r_gp[:], in_=r_ge[:], axis=AX.X, op=ALU.add), reads=["r_ge"], writes=["r_gp"])
    P.op("dve", lambda e: e.reciprocal(out=r_gp[:], in_=r_gp[:]), reads=["r_gp"], writes=["r_gp"])
    P.op("dve", lambda e: e.tensor_scalar(out=r_pen[:], in0=r_gmask[:], scalar1=BIG, scalar2=-BIG, op0=ALU.mult, op1=ALU.add), reads=["r_gmask"], writes=["r_pen"])
    P.op("dve", lambda e: e.tensor_tensor(out=r_el[:].rearrange("p t (g k) -> p t g k", k=8), in0=ELv.rearrange("p t (g k) -> p t g k", k=8), in1=bc(r_pen[:, :, :, None], [128, NT, 4, 8]), op=ALU.add),
         reads=lall_keys + ["r_pen"], writes=["r_el"])
    P.op("dve", lambda e: e.tensor_reduce(out=r_m1[:], in_=r_el[:], axis=AX.X, op=ALU.max), reads=["r_el"], writes=["r_m1"])
    P.op("dve", lambda e: e.tensor_tensor(out=r_mask1[:], in0=r_el[:], in1=bc(r_m1[:, :, None], [128, NT, 32]), op=ALU.is_equal), reads=["r_el", "r_m1"], writes=["r_mask1"])
    P.op("dve", lambda e: e.scalar_tensor_tensor(out=r_el2[:], in0=r_mask1[:], scalar=-BIG, in1=r_el[:], op0=ALU.mult, op1=ALU.add), reads=["r_mask1", "r_el"], writes=["r_el2"])
    P.op("dve", lambda e: e.tensor_reduce(out=r_m2[:], in_=r_el2[:], axis=AX.X, op=ALU.max), reads=["r_el2"], writes=["r_m2"])
    P.op("dve", lambda e: e.tensor_tensor(out=r_mask2[:], in0=r_el2[:], in1=bc(r_m2[:, :, None], [128, NT, 32]), op=ALU.is_equal), reads=["r_el2", "r_m2"], writes=["r_mask2"])
    P.op("dve", lambda e: e.tensor_tensor(out=r_m12b[:], in0=r_mask1[:], in1=r_mask2[:], op=ALU.add), reads=["r_mask1", "r_mask2"], writes=["r_m12b"])
    P.op("dve", lambda e: e.tensor_tensor(out=r_e21[:], in0=r_m2[:], in1=r_m1[:], op=ALU.subtract), reads=["r_m1", "r_m2"], writes=["r_e21"])
    P.op("act", lambda e: e.activation(out=r_e21[:], in_=r_e21[:], func=AF.Exp), reads=["r_e21"], writes=["r_e21"])
    P.op("dve", lambda e: e.tensor_scalar(out=gt1[:], in0=r_e21[:], scalar1=1.0, scalar2=None, op0=ALU.add), reads=["r_e21"], writes=["gt1"])
    P.op("dve", lambda e: e.reciprocal(out=gt1[:], in_=gt1[:]), reads=["gt1"], writes=["gt1"])
    P.op("dve", lambda e: e.tensor_tensor(out=gt1[:], in0=gt1[:], in1=r_gp[:], op=ALU.mult), reads=["gt1", "r_gp"], writes=["gt1"])
    P.op("dve", lambda e: e.tensor_tensor(out=gt2[:], in0=gt1[:], in1=r_e21[:], op=ALU.mult), reads=["gt1", "r_e21"], writes=["gt2"])

    br = bank()
    for t in range(NT):
        P.op("pe", lambda e, t=t: e.matmul(psum[0:128, br * 512 + t * 32: br * 512 + (t + 1) * 32], lhsT=ustr_b[:], rhs=r_m12b[:, t, :], start=True, stop=(t == 0)),
             reads=["ustr_b", "r_m12b"], writes=[pk(br)])
        for t2 in range(t):
            P.op("pe", lambda e, t=t, t2=t2: e.matmul(psum[0:128, br * 512 + t * 32: br * 512 + (t + 1) * 32], lhsT=ones_b[:], rhs=r_m12b[:, t2, :], start=False, stop=(t2 == t - 1)),
                 reads=["ones_b", "r_m12b"], writes=[pk(br)])
    P.op("act", lambda e: e.copy(out=r_rank[:].rearrange("p t k -> p (t k)"), in_=psv(br)), reads=[pk(br)], writes=["r_rank"])
    P.op("dve", lambda e: e.tensor_scalar(out=r_valid[:], in0=r_rank[:], scalar1=float(CAP), scalar2=None, op0=ALU.is_lt), reads=["r_rank"], writes=["r_valid"])
    P.op("dve", lambda e: e.tensor_tensor(out=r_slot[:], in0=r_rank[:], in1=bc(ecap[:, None, :], [128, NT, 32]), op=ALU.add), reads=["r_rank", "ecap"], writes=["r_slot"])
    P.op("dve", lambda e: e.tensor_scalar(out=r_slot[:], in0=r_slot[:], scalar1=-float(DUMP), scalar2=None, op0=ALU.add), reads=["r_slot"], writes=["r_slot"])
    P.op("dve", lambda e: e.tensor_tensor(out=r_slot[:], in0=r_slot[:], in1=r_valid[:], op=ALU.mult), reads=["r_slot", "r_valid"], writes=["r_slot"])
    P.op("dve", lambda e: e.tensor_scalar(out=r_slot[:], in0=r_slot[:], scalar1=float(DUMP), scalar2=None, op0=ALU.add), reads=["r_slot"], writes=["r_slot"])
    for msk, mk, df, dk, vv, vk, di, dik, gt, gk in ((r_mask1, "r_mask1", r_d1f, "r_d1f", r_v1, "r_v1", d1i, "d1i", gt1, "gt1"),
                                                     (r_mask2, "r_mask2", r_d2f, "r_d2f", r_v2, "r_v2", d2i, "d2i", gt2, "gt2")):
        P.op("dve", lambda e, msk=msk: e.tensor_tensor(out=r_tmp[:], in0=msk[:], in1=r_slot[:], op=ALU.mult), reads=[mk, "r_slot"], writes=["r_tmp"])
        P.op("dve", lambda e, df=df: e.tensor_reduce(out=df[:], in_=r_tmp[:], axis=AX.X, op=ALU.add), reads=["r_tmp"], writes=[dk])
        P.op("dve", lambda e, df=df, di=di: e.tensor_copy(out=di[:], in_=df[:]), reads=[dk], writes=[dik])
        P.op("dve", lambda e, msk=msk: e.tensor_tensor(out=r_tmp[:], in0=msk[:], in1=r_valid[:], op=ALU.mult), reads=[mk, "r_valid"], writes=["r_tmp"])
        P.op("dve", lambda e, vv=vv: e.tensor_reduce(out=vv[:], in_=r_tmp[:], axis=AX.X, op=ALU.add), reads=["r_tmp"], writes=[vk])
        P.op("dve", lambda e, vv=vv, gt=gt: e.tensor_tensor(out=gt[:], in0=gt[:], in1=vv[:], op=ALU.mult), reads=[gk, vk], writes=[gk])
    xs_keys = []
    for t in range(NT):
        for di, dik, w in ((d1i, "d1i", 0), (d2i, "d2i", 1)):
            key = ("Xs", t, w)
            xs_keys.append(key)
            P.dma("pool", lambda e, t=t, di=di: e.indirect_dma_start(out=Xs, out_offset=bass.IndirectOffsetOnAxis(ap=di[:, t:t + 1], axis=0), in_=h2b[:, t, :], in_offset=None, bounds_check=NE * CAP + 127, oob_is_err=False),
                  reads=[("h2b", t), dik] + xz_keys, writes=[key])
    if stage == "dbg":
        dbg_i = nc.dram_tensor("dbg_i", [128, 2 * NT], I32, kind="ExternalOutput").ap()
        dbg_g = nc.dram_tensor("dbg_g", [128, 2 * NT], F32, kind="ExternalOutput").ap()
        dbg_r = nc.dram_tensor("dbg_r", [128, NT * 32], F32, kind="ExternalOutput").ap()
        dbg_l = nc.dram_tensor("dbg_l", [128, NT * 36], F32, kind="ExternalOutput").ap()
        P.dma("sp", lambda e: e.dma_start(out=dbg_i[:, 0:NT], in_=d1i[:]), reads=["d1i"], writes=["dbg1"])
        P.dma("sp", lambda e: e.dma_start(out=dbg_i[:, NT:2 * NT], in_=d2i[:]), reads=["d2i"], writes=["dbg2"])
        P.dma("sp", lambda e: e.dma_start(out=dbg_g[:, 0:NT], in_=gt1[:]), reads=["gt1"], writes=["dbg3"])
        P.dma("sp", lambda e: e.dma_start(out=dbg_g[:, NT:2 * NT], in_=gt2[:]), reads=["gt2"], writes=["dbg4"])
        P.dma("sp", lambda e: e.dma_start(out=dbg_r, in_=r_rank[:].rearrange("p t k -> p (t k)")), reads=["r_rank"], writes=["dbg5"])
        P.dma("sp", lambda e: e.dma_start(out=dbg_l, in_=Lall[:].rearrange("p t k -> p (t k)")), reads=lall_keys, writes=["dbg6"])
        P.finish(["dbg1", "dbg2", "dbg3", "dbg4", "dbg5", "dbg6"])
    rt.close()
    P.barrier()

    ex = ExitStack()
    wg_s = sb(ex, "wg_s", [128, 3, 8, FF], BF16)
    wu_s = sb(ex, "wu_s", [128, 3, 8, FF], BF16)
    wd_s = sb(ex, "wd_s", [128, 3, 4, D], BF16)
    xb = sb(ex, "xb", [128, 2, 2, D], BF16)
    xbT = sb(ex, "xbT", [128, 2, 8, CAP], BF16)
    sil = sb(ex, "sil", [128, 4, CAP], F32)
    hmid = sb(ex, "hmid", [128, 2, 4, CAP], BF16)
    Yo = sb(ex, "Yo", [128, 2, D], BF16)
    fss = sb(ex, "fss", [128, NT], F32)

    def load_w(e_):
        par = e_ % 3
        P.dma("sp", lambda e: e.dma_start(out=wg_s[:, par], in_=Wg_b[e_].rearrange("(kc p) f -> p kc f", p=128)), reads=[("Wb", "g", e_)], writes=[("wg", par)])
        P.dma("sp", lambda e: e.dma_start(out=wu_s[:, par], in_=Wu_b[e_].rearrange("(kc p) f -> p kc f", p=128)), reads=[("Wb", "u", e_)], writes=[("wu", par)])
        P.dma("sp", lambda e: e.dma_start(out=wd_s[:, par], in_=Wd_b[e_].rearrange("(kc p) f -> p kc f", p=128)), reads=[("Wb", "d", e_)], writes=[("wd", par)])

    def load_xb(e_):
        par = e_ % 2
        P.dma("sp", lambda e: e.dma_start(out=xb[:, par], in_=Xs[e_ * CAP:(e_ + 1) * CAP, :].rearrange("(b p) d -> p b d", p=128)), reads=xs_keys, writes=[("xb", par)])

    ys_keys = []
    load_xb(0)
    load_w(0)
    load_w(1)

    def expert(e_):
        par = e_ % 2
        wp = e_ % 3
        if e_ + 1 < NE:
            load_xb(e_ + 1)
        for blk in range(2):
            for half in range(2):
                bt = bank()
                for q in range(4):
                    kc = half * 4 + q
                    P.op("pe", lambda e, blk=blk, kc=kc, q=q, bt=bt: e.matmul(psum[0:128, bt * 512 + q * 128: bt * 512 + (q + 1) * 128], lhsT=xb[:, par, blk, kc * 128:(kc + 1) * 128], rhs=ident_b[:], start=True, stop=True),
                         reads=[("xb", par), "ident_b"], writes=[pk(bt)])
                if half == 0:
                    P.op("act", lambda e, blk=blk, half=half, bt=bt: e.copy(out=xbT[:, par, half * 4:half * 4 + 4, blk * 128:(blk + 1) * 128], in_=psv(bt).rearrange("p (q c) -> p q c", c=128)),
                         reads=[pk(bt)], writes=[("xbT", par)])
                else:
                    P.op("dve", lambda e, blk=blk, half=half, bt=bt: e.tensor_copy(out=xbT[:, par, half * 4:half * 4 + 4, blk * 128:(blk + 1) * 128], in_=psv(bt).rearrange("p (q c) -> p q c", c=128)),
                         reads=[pk(bt)], writes=[("xbT", par)])
        yield
        ba_ = bank(2)
        for fc in range(4):
            for kc in range(8):
                P.op("pe", lambda e, fc=fc, kc=kc: e.matmul(psum[0:128, ba_ * 512 + fc * CAP: ba_ * 512 + (fc + 1) * CAP], lhsT=wg_s[:, wp, kc, fc * 128:(fc + 1) * 128], rhs=xbT[:, par, kc, :], start=(kc == 0), stop=(kc == 7)),
                     reads=[("wg", wp), ("xbT", par)], writes=[pk(ba_), pk(ba_ + 1)])
        P.op("act", lambda e: e.activation(out=sil[:].rearrange("p f c -> p (f c)"), in_=psum[0:128, ba_ * 512:(ba_ + 2) * 512], func=AF.Silu), reads=[pk(ba_), pk(ba_ + 1)], writes=["sil"])
        bb_ = bank(2)
        for fc in range(4):
            for kc in range(8):
                P.op("pe", lambda e, fc=fc, kc=kc: e.matmul(psum[0:128, bb_ * 512 + fc * CAP: bb_ * 512 + (fc + 1) * CAP], lhsT=wu_s[:, wp, kc, fc * 128:(fc + 1) * 128], rhs=xbT[:, par, kc, :], start=(kc == 0), stop=(kc == 7)),
                     reads=[("wu", wp), ("xbT", par)], writes=[pk(bb_), pk(bb_ + 1)])
        P.op("dve", lambda e: e.tensor_tensor(out=hmid[:, par].rearrange("p f c -> p (f c)"), in0=sil[:].rearrange("p f c -> p (f c)"), in1=psum[0:128, bb_ * 512:(bb_ + 2) * 512], op=ALU.mult),
             reads=["sil", pk(bb_), pk(bb_ + 1)], writes=[("hmid", par)])
        yield
        if e_ + 2 < NE:
            load_w(e_ + 2)
        for blk in range(2):
            for dh in range(2):
                bd = bank()
                for fc in range(4):
                    P.op("pe", lambda e, blk=blk, dh=dh, fc=fc, bd=bd: e.matmul(psv(bd), lhsT=hmid[:, par, fc, blk * 128:(blk + 1) * 128], rhs=wd_s[:, wp, fc, dh * 512:(dh + 1) * 512], start=(fc == 0), stop=(fc == 3)),
                         reads=[("hmid", par), ("wd", wp)], writes=[pk(bd)])
                if dh == 0:
                    P.op("act", lambda e, blk=blk, dh=dh, bd=bd: e.copy(out=Yo[:, blk, dh * 512:(dh + 1) * 512], in_=psv(bd)), reads=[pk(bd)], writes=[("Yo", blk, dh)])
                else:
                    P.op("dve", lambda e, blk=blk, dh=dh, bd=bd: e.tensor_copy(out=Yo[:, blk, dh * 512:(dh + 1) * 512], in_=psv(bd)), reads=[pk(bd)], writes=[("Yo", blk, dh)])
        key = ("Ys", e_)
        ys_keys.append(key)
        P.dma("sp", lambda e: e.dma_start(out=Ys[e_ * CAP:(e_ + 1) * CAP, :].rearrange("(b p) d -> p b d", p=128), in_=Yo[:]), reads=[("Yo", b_, d_) for b_ in range(2) for d_ in range(2)], writes=[key])

    gens_ = [expert(e_) for e_ in range(NE)]
    active_ = []
    nxt_ = 0
    while active_ or nxt_ < NE:
        if nxt_ < NE and len(active_) < 2:
            active_.append(gens_[nxt_])
            nxt_ += 1
        for g_ in list(active_):
            try:
                next(g_)
            except StopIteration:
                active_.remove(g_)

    G1 = sb(ex, "G1", [128, 2, D], BF16)
    G2 = sb(ex, "G2", [128, 2, D], BF16)
    fjunk = sb(ex, "fjunk", [128, D], F32)
    P.op("pool", lambda e: e.memset(fss[:], 0.0), writes=[("fss", t) for t in range(NT)])
    def comb_tile(t):
        sl = t % 2
        P.dma("pool", lambda e: e.indirect_dma_start(out=G1[:, sl, :], out_offset=None, in_=Ys, in_offset=bass.IndirectOffsetOnAxis(ap=d1i[:, t:t + 1], axis=0)), reads=ys_keys + ["d1i", ("Yz", 0)], writes=[("G1", sl)])
        P.dma("pool", lambda e: e.indirect_dma_start(out=G2[:, sl, :], out_offset=None, in_=Ys, in_offset=bass.IndirectOffsetOnAxis(ap=d2i[:, t:t + 1], axis=0)), reads=ys_keys + ["d2i", ("Yz", 0)], writes=[("G2", sl)])
        yield
        P.op("dve", lambda e: e.scalar_tensor_tensor(out=x1[:, t, :], in0=G1[:, sl, :], scalar=gt1[:, t:t + 1], in1=x1[:, t, :], op0=ALU.mult, op1=ALU.add), reads=[("G1", sl), "gt1", ("x1", t)], writes=[("x1", t)])
        P.op("dve", lambda e: e.scalar_tensor_tensor(out=x1[:, t, :], in0=G2[:, sl, :], scalar=gt2[:, t:t + 1], in1=x1[:, t, :], op0=ALU.mult, op1=ALU.add), reads=[("G2", sl), "gt2", ("x1", t)], writes=[("x1", t)])
        P.op("act", lambda e: e.activation(out=fjunk[:], in_=x1[:, t, :], func=AF.Square, accum_out=fss[:, t:t + 1]), reads=[("x1", t), ("fss", t)], writes=["fjunk", ("fss", t)])
        P.op("act", lambda e: e.activation(out=fss[:, t:t + 1], in_=fss[:, t:t + 1], func=AF.Sqrt, scale=1.0 / D, bias=EPS), reads=[("fss", t)], writes=[("fss", t)])
        yield
        P.op("dve", lambda e: e.reciprocal(out=fss[:, t:t + 1], in_=fss[:, t:t + 1]), reads=[("fss", t)], writes=[("fss", t)])
        P.op("dve", lambda e: e.scalar_tensor_tensor(out=x1[:, t, :], in0=x1[:, t, :], scalar=fss[:, t:t + 1], in1=g3bc[:], op0=ALU.mult, op1=ALU.mult), reads=[("x1", t), ("fss", t), "g3bc"], writes=[("x1", t)])
        P.dma("sp", lambda e: e.dma_start(out=out[t * 128:(t + 1) * 128, :], in_=x1[:, t, :]), reads=[("x1", t)], writes=[("out", t)])
        yield

    gens_ = [comb_tile(t) for t in range(NT)]
    active_ = []
    nxt_ = 0
    while active_ or nxt_ < NT:
        if nxt_ < NT and len(active_) < 2:
            active_.append(gens_[nxt_])
            nxt_ += 1
        for g_ in list(active_):
            try:
                next(g_)
            except StopIteration:
                active_.remove(g_)
    P.finish([("out", t) for t in range(NT)])
    P.emit()
    return nc


def make_inputs(inp, b):
    x = np.ascontiguousarray(inp["x"][b])
    w_in = inp["w_in"][0]
    m = {
        "x_tok": x,
        "xT": np.ascontiguousarray(x.T),
        "w_in_r": np.ascontiguousarray(w_in[:, :3584].reshape(8, 128, 28, 128).transpose(2, 1, 0, 3)),
        "w_ba": np.ascontiguousarray(w_in[:, 3584:3592].reshape(8, 128, 8).transpose(1, 0, 2)),
        "g1": np.ascontiguousarray(inp["mix_norm_g"][0].reshape(8, 128).T),
        "caw": np.ascontiguousarray(inp["conv_a_w"][0].reshape(3, 4, 128).transpose(2, 1, 0)),
        "gA": np.ascontiguousarray(inp["conv_a_norm_g"][0].reshape(4, 128).T),
        "dcw": np.ascontiguousarray(inp["dn_conv_w"][0].reshape(4, 12, 128).transpose(2, 1, 0)),
        "alog": np.ascontiguousarray(inp["dn_a_log"][0]),
        "dtb": np.ascontiguousarray(inp["dn_dt_bias"][0]),
        "gdn": np.ascontiguousarray(inp["dn_norm_g"][0].reshape(128, 1)),
        "w_out_r": np.ascontiguousarray(inp["w_out"][0].reshape(8, 128, D).transpose(1, 0, 2)),
        "g2": np.ascontiguousarray(inp["ffn_norm_g"][0]),
        "wr_r": np.ascontiguousarray(np.concatenate([inp["router_group_w"][0], inp["router_expert_w"][0]], axis=1).reshape(8, 128, 36).transpose(1, 0, 2)),
        "w_gate": np.ascontiguousarray(inp["w_gate"][0]),
        "w_up": np.ascontiguousarray(inp["w_up"][0]),
        "w_down": np.ascontiguousarray(inp["w_down"][0]),
        "g3": np.ascontiguousarray(inp["final_norm_g"]),
    }
    return m


def kernel(**inputs):
    inp = {k: np.asarray(v) for k, v in inputs.items()}
    nc = build("full")
    shared = make_inputs(inp, 0)
    in_maps = []
    for b in range(8):
        m = dict(shared)
        xb = np.ascontiguousarray(inp["x"][b])
        m["x_tok"] = xb
        m["xT"] = np.ascontiguousarray(xb.T)
        in_maps.append(m)
    res = run_bass_kernel_spmd(nc, in_maps, core_ids=list(range(8)))
    return np.stack([r["out"] for r in res.results], axis=0).astype(np.float32)
```
